# Optimizing a Trainium2 kernel written in Bass

```python
import jax, jax.numpy as jnp
from jax import lax
import numpy as np

D_MODEL = 1024
BATCH = 8
SEQ = 4096
DEPTH = 4

N_MIXERS = 3
BLOCK = 128
ROPE_THETA = 500000.0
ROPE_FRAC = 4
EPS = 1e-6
NEG_INF = -1e30

A_HEADS = 8
A_QK_DIM = 128
A_ROPE_DIM = A_QK_DIM // ROPE_FRAC
A_NOPE_DIM = A_QK_DIM - A_ROPE_DIM
A_V_DIM = 128
A_KV_RANK = 256
IDX_HEADS = 16
IDX_DIM = 64
TOPK_MAX = 256

B_HEADS = 16
B_KV_HEADS = 4
B_HEAD_DIM = 64
B_WINDOW = 128

C_HEADS = 8
C_HEAD_DIM = 128
C_BRANCHES = ((128, 1), (512, 4), (2048, 16))

MEM_LEN = 256
MEM_HEADS = 4
MEM_HEAD_DIM = 64
MEM_WIDTH = MEM_HEADS * MEM_HEAD_DIM

D_FF = 2816
CONV_WIDTH = 3

MIX_WIDTH = 1024
OUT_IN = MIX_WIDTH + MEM_WIDTH
A_SPLITS = (A_HEADS * A_QK_DIM, A_KV_RANK, A_ROPE_DIM, IDX_HEADS * IDX_DIM, IDX_DIM, IDX_HEADS, MEM_WIDTH)
B_SPLITS = (B_HEADS * B_HEAD_DIM, B_KV_HEADS * B_HEAD_DIM, B_KV_HEADS * B_HEAD_DIM, MEM_WIDTH)
C_SPLITS = (C_HEADS * C_HEAD_DIM, C_HEADS * C_HEAD_DIM, C_HEADS * C_HEAD_DIM, MEM_WIDTH)
N_A = len(range(0, DEPTH, N_MIXERS))
N_B = len(range(1, DEPTH, N_MIXERS))
N_C = len(range(2, DEPTH, N_MIXERS))

kernel_name = 'hybrid_dsa_swa_dilated_memory_convffn'


def rmsnorm(x, g):
    xf = x.astype(jnp.float32)
    y = xf * lax.rsqrt(jnp.mean(xf * xf, axis=-1, keepdims=True) + EPS)
    return (y * g.astype(jnp.float32)).astype(x.dtype)


def split_cols(t, sizes):
    return jnp.split(t, np.cumsum(sizes)[:-1].tolist(), axis=-1)


def rope_tables(positions, rot_dim):
    inv = ROPE_THETA ** (-jnp.arange(0, rot_dim, 2, dtype=jnp.float32) / rot_dim)
    ang = positions.astype(jnp.float32)[..., None] * inv
    return jnp.cos(ang), jnp.sin(ang)


def apply_partial_rope(x, cos, sin):
    half = cos.shape[-1]
    rot = 2 * half
    xf = x[..., :rot].astype(jnp.float32)
    x1, x2 = xf[..., :half], xf[..., half:]
    c, s = cos[:, :, None, :], sin[:, :, None, :]
    xr = jnp.concatenate([x1 * c - x2 * s, x2 * c + x1 * s], axis=-1).astype(x.dtype)
    return jnp.concatenate([xr, x[..., rot:]], axis=-1)


def banded_window_attention(q, k, v, max_dist, scale, sink=None):
    B, n = q.shape[0], q.shape[1]
    nb = -(-n // BLOCK)
    pad = nb * BLOCK - n
    if pad:
        q = jnp.pad(q, ((0, 0), (0, pad), (0, 0), (0, 0), (0, 0)))
        k = jnp.pad(k, ((0, 0), (0, pad), (0, 0), (0, 0)))
        v = jnp.pad(v, ((0, 0), (0, pad), (0, 0), (0, 0)))
    qb = q.reshape(B, nb, BLOCK, *q.shape[2:])
    kb = k.reshape(B, nb, BLOCK, *k.shape[2:])
    vb = v.reshape(B, nb, BLOCK, *v.shape[2:])
    prev = ((0, 0), (1, 0), (0, 0), (0, 0), (0, 0))
    kk = jnp.concatenate([jnp.pad(kb[:, :-1], prev), kb], axis=2)
    vv = jnp.concatenate([jnp.pad(vb[:, :-1], prev), vb], axis=2)
    s = jnp.einsum('bnqhgd,bnkhd->bnhgqk', qb, kk).astype(jnp.float32) * scale
    qi = jnp.arange(BLOCK)[:, None]
    kj = jnp.arange(2 * BLOCK)[None, :]
    dist = BLOCK + qi - kj
    band = (dist >= 0) & (dist <= max_dist)
    blk = jnp.arange(nb)[:, None, None]
    mask = band[None] & ((blk > 0) | (kj >= BLOCK)[None])
    s = jnp.where(mask[None, :, None, None], s, NEG_INF)
    m = jnp.max(s, axis=-1)
    if sink is not None:
        sk = sink.astype(jnp.float32)[None, None, :, :, None]
        m = jnp.maximum(m, sk)
    p = jnp.exp(s - m[..., None])
    l = jnp.sum(p, axis=-1)
    if sink is not None:
        l = l + jnp.exp(sk - m)
    p = p / l[..., None]
    o = jnp.einsum('bnhgqk,bnkhd->bnqhgd', p, vv.astype(jnp.float32))
    o = o.reshape(B, nb * BLOCK, *o.shape[3:])[:, :n].astype(q.dtype)
    lse = (m + jnp.log(l)).transpose(0, 1, 4, 2, 3)
    lse = lse.reshape(B, nb * BLOCK, *lse.shape[3:])[:, :n]
    return o, lse


def dsa_attention(q_nope, q_rope, c_kv, k_rope, q_idx, k_idx, w_idx, w_uk, w_uv):
    B, S = q_nope.shape[0], q_nope.shape[1]
    topk = min(TOPK_MAX, S // 4)
    nb = S // BLOCK
    scale = A_QK_DIM ** -0.5
    q_lat = jnp.einsum('bshn,rhn->bshr', q_nope, w_uk)
    key_pos = jnp.arange(S)

    def to_blocks(t):
        return t.reshape(B, nb, BLOCK, *t.shape[2:]).swapaxes(0, 1)

    def one_block(args):
        ql, qr, qi, wi, t = args
        dots = jnp.einsum('bqhd,bsd->bqhs', qi, k_idx).astype(jnp.float32)
        score = jnp.einsum('bqhs,bqh->bqs', jax.nn.relu(dots), wi.astype(jnp.float32))
        causal = key_pos[None, :] <= t[:, None]
        score = jnp.where(causal[None], score, -jnp.inf)
        _, idx = lax.top_k(score, topk)
        c_sel = jax.vmap(lambda c, i: c[i])(c_kv, idx)
        kr_sel = jax.vmap(lambda c, i: c[i])(k_rope, idx)
        s = (jnp.einsum('bqhr,bqkr->bqhk', ql, c_sel)
             + jnp.einsum('bqhe,bqke->bqhk', qr, kr_sel)).astype(jnp.float32) * scale
        valid = idx <= t[None, :, None]
        s = jnp.where(valid[:, :, None, :], s, NEG_INF)
        p = jax.nn.softmax(s, axis=-1)
        return jnp.einsum('bqhk,bqkr->bqhr', p, c_sel.astype(jnp.float32)).astype(ql.dtype)

    t_ids = jnp.arange(S).reshape(nb, BLOCK)
    o_lat = lax.map(one_block, (to_blocks(q_lat), to_blocks(q_rope), to_blocks(q_idx), to_blocks(w_idx), t_ids))
    o_lat = o_lat.swapaxes(0, 1).reshape(B, S, A_HEADS, A_KV_RANK)
    return jnp.einsum('bshr,rhv->bshv', o_lat, w_uv)


def mixer_a(h, cos32, sin32, cos16, sin16, w_in, kv_norm, w_uk, w_uv):
    B, S, _ = h.shape
    q, c_kv, k_rope, q_idx, k_idx, w_idx, q_mem = split_cols(h @ w_in, A_SPLITS)
    q = apply_partial_rope(q.reshape(B, S, A_HEADS, A_QK_DIM), cos32, sin32)
    q_rope, q_nope = q[..., :A_ROPE_DIM], q[..., A_ROPE_DIM:]
    k_rope = apply_partial_rope(k_rope[:, :, None, :], cos32, sin32)[:, :, 0]
    c_kv = rmsnorm(c_kv, kv_norm)
    q_idx = apply_partial_rope(q_idx.reshape(B, S, IDX_HEADS, IDX_DIM), cos16, sin16)
    k_idx = apply_partial_rope(k_idx[:, :, None, :], cos16, sin16)[:, :, 0]
    w_idx = w_idx * (IDX_HEADS * IDX_DIM) ** -0.5
    o = dsa_attention(q_nope, q_rope, c_kv, k_rope, q_idx, k_idx, w_idx, w_uk, w_uv)
    return o.reshape(B, S, A_HEADS * A_V_DIM), q_mem


def mixer_b(h, cos16, sin16, w_in, sinks):
    B, S, _ = h.shape
    q, k, v, q_mem = split_cols(h @ w_in, B_SPLITS)
    group = B_HEADS // B_KV_HEADS
    q = apply_partial_rope(q.reshape(B, S, B_HEADS, B_HEAD_DIM), cos16, sin16)
    k = apply_partial_rope(k.reshape(B, S, B_KV_HEADS, B_HEAD_DIM), cos16, sin16)
    v = v.reshape(B, S, B_KV_HEADS, B_HEAD_DIM)
    q = q.reshape(B, S, B_KV_HEADS, group, B_HEAD_DIM)
    o, _ = banded_window_attention(q, k, v, B_WINDOW - 1, B_HEAD_DIM ** -0.5,
                                   sink=sinks.reshape(B_KV_HEADS, group))
    return o.reshape(B, S, B_HEADS * B_HEAD_DIM), q_mem


def dilate(t, d):
    B, S = t.shape[0], t.shape[1]
    return t.reshape(B, S // d, d, *t.shape[2:]).swapaxes(1, 2).reshape(B * d, S // d, *t.shape[2:])


def undilate(t, d, B):
    n = t.shape[1]
    return t.reshape(B, d, n, *t.shape[2:]).swapaxes(1, 2).reshape(B, n * d, *t.shape[2:])


def mixer_c(h, cos32, sin32, w_in):
    B, S, _ = h.shape
    q, k, v, q_mem = split_cols(h @ w_in, C_SPLITS)
    q = apply_partial_rope(q.reshape(B, S, C_HEADS, C_HEAD_DIM), cos32, sin32)
    k = apply_partial_rope(k.reshape(B, S, C_HEADS, C_HEAD_DIM), cos32, sin32)
    v = v.reshape(B, S, C_HEADS, C_HEAD_DIM)
    outs, lses = [], []
    for window, d in C_BRANCHES:
        o_g, lse_g = banded_window_attention(dilate(q, d)[:, :, :, None], dilate(k, d), dilate(v, d),
                                             window // d, C_HEAD_DIM ** -0.5)
        outs.append(undilate(o_g[:, :, :, 0], d, B))
        lses.append(undilate(lse_g[..., 0], d, B))
    alpha = jax.nn.softmax(jnp.stack(lses), axis=0)
    o = jnp.sum(alpha[..., None] * jnp.stack(outs).astype(jnp.float32), axis=0).astype(h.dtype)
    return o.reshape(B, S, C_HEADS * C_HEAD_DIM), q_mem


def memory_cross_attention(q_mem, mem_n, w_mem_kv):
    B, S, _ = q_mem.shape
    k, v = jnp.split(mem_n @ w_mem_kv, 2, axis=-1)
    k = k.reshape(B, -1, MEM_HEADS, MEM_HEAD_DIM)
    v = v.reshape(B, -1, MEM_HEADS, MEM_HEAD_DIM)
    q = q_mem.reshape(B, S, MEM_HEADS, MEM_HEAD_DIM)
    s = jnp.einsum('bshd,bmhd->bhsm', q, k).astype(jnp.float32) * MEM_HEAD_DIM ** -0.5
    p = jax.nn.softmax(s, axis=-1)
    o = jnp.einsum('bhsm,bmhd->bshd', p, v.astype(jnp.float32)).astype(q_mem.dtype)
    return o.reshape(B, S, MEM_WIDTH)


def conv_glu_ffn(h, w_up, conv_w, conv_b, w_down):
    S = h.shape[1]
    a, b = jnp.split(h @ w_up, 2, axis=-1)
    a_pad = jnp.pad(a, ((0, 0), (CONV_WIDTH - 1, 0), (0, 0)))
    a = sum(conv_w[j] * a_pad[:, j:j + S] for j in range(CONV_WIDTH)) + conv_b
    return (jax.nn.silu(a) * b) @ w_down


def setup_inputs(seed: int = 0) -> dict:
    key = jax.random.key(seed)
    ks = iter(jax.random.split(key, 40))

    def w(shape, fan_in):
        return jax.random.normal(next(ks), shape, jnp.float32) * fan_in ** -0.5

    def gain(shape):
        return 1.0 + 0.02 * jax.random.normal(next(ks), shape, jnp.float32)

    x = jax.random.normal(next(ks), (BATCH, SEQ, D_MODEL), jnp.float32)
    mem = jax.random.normal(next(ks), (BATCH, MEM_LEN, D_MODEL), jnp.float32)
    offset = jax.random.randint(next(ks), (BATCH, 1), 0, 4096, dtype=jnp.int32)
    positions = offset + jnp.arange(SEQ, dtype=jnp.int32)[None, :]
    return {
        'x': x, 'mem': mem, 'positions': positions,
        'g_mix': gain((DEPTH, D_MODEL)), 'g_ffn': gain((DEPTH, D_MODEL)),
        'g_mem': gain((D_MODEL,)), 'g_final': gain((D_MODEL,)),
        'w_mem_kv': w((DEPTH, D_MODEL, 2 * MEM_WIDTH), D_MODEL),
        'a_w_in': w((N_A, D_MODEL, sum(A_SPLITS)), D_MODEL),
        'a_kv_norm': gain((N_A, A_KV_RANK)),
        'a_w_uk': w((N_A, A_KV_RANK, A_HEADS, A_NOPE_DIM), A_KV_RANK),
        'a_w_uv': w((N_A, A_KV_RANK, A_HEADS, A_V_DIM), A_KV_RANK),
        'a_w_out': w((N_A, OUT_IN, D_MODEL), OUT_IN),
        'b_w_in': w((N_B, D_MODEL, sum(B_SPLITS)), D_MODEL),
        'b_sinks': 0.5 * jax.random.normal(next(ks), (N_B, B_HEADS), jnp.float32),
        'b_w_out': w((N_B, OUT_IN, D_MODEL), OUT_IN),
        'c_w_in': w((N_C, D_MODEL, sum(C_SPLITS)), D_MODEL),
        'c_w_out': w((N_C, OUT_IN, D_MODEL), OUT_IN),
        'f_w_up': w((DEPTH, D_MODEL, 2 * D_FF), D_MODEL),
        'f_conv_w': w((DEPTH, CONV_WIDTH, D_FF), CONV_WIDTH),
        'f_conv_b': 0.02 * jax.random.normal(next(ks), (DEPTH, D_FF), jnp.float32),
        'f_w_down': w((DEPTH, D_FF, D_MODEL), D_FF),
    }


def reference(x, mem, positions, g_mix, g_ffn, g_mem, g_final, w_mem_kv,
              a_w_in, a_kv_norm, a_w_uk, a_w_uv, a_w_out,
              b_w_in, b_sinks, b_w_out, c_w_in, c_w_out,
              f_w_up, f_conv_w, f_conv_b, f_w_down):
    cos32, sin32 = rope_tables(positions, A_QK_DIM // ROPE_FRAC)
    cos16, sin16 = rope_tables(positions, B_HEAD_DIM // ROPE_FRAC)
    mem_n = rmsnorm(mem, g_mem)
    for i in range(DEPTH):
        kind, j = i % N_MIXERS, i // N_MIXERS
        h = rmsnorm(x, g_mix[i])
        if kind == 0:
            mix, q_mem = mixer_a(h, cos32, sin32, cos16, sin16, a_w_in[j], a_kv_norm[j], a_w_uk[j], a_w_uv[j])
            w_out = a_w_out[j]
        elif kind == 1:
            mix, q_mem = mixer_b(h, cos16, sin16, b_w_in[j], b_sinks[j])
            w_out = b_w_out[j]
        else:
            mix, q_mem = mixer_c(h, cos32, sin32, c_w_in[j])
            w_out = c_w_out[j]
        mem_o = memory_cross_attention(q_mem, mem_n, w_mem_kv[i])
        x = x + jnp.concatenate([mix, mem_o], axis=-1) @ w_out
        x = x + conv_glu_ffn(rmsnorm(x, g_ffn[i]), f_w_up[i], f_conv_w[i], f_conv_b[i], f_w_down[i])
    return rmsnorm(x, g_final)
```

```python
import numpy as np
import concourse.bass as bass
import concourse.mybir as mybir
from concourse.bass_utils import run_bass_kernel_spmd
from contextlib import ExitStack

F32 = mybir.dt.float32
BF16 = mybir.dt.bfloat16
I32 = mybir.dt.int32
AF = mybir.ActivationFunctionType
ALU = mybir.AluOpType
AX = mybir.AxisListType

ENGS = ("pe", "act", "dve", "pool", "sp")
SKIP = set()
NDMA = 8
T = 4096
D = 1024
TT = 512
NTT = T // TT
NEG = -30000.0
EPS = 1e-6


class Buf:
    __slots__ = ("name", "base", "parts", "rd")

    def __init__(self, name=""):
        self.name = name
        self.base = []
        self.parts = []
        self.rd = {}


class Prog:
    def __init__(self, nc):
        self.nc = nc
        self.streams = {e: [] for e in ENGS}
        self.cnt = {e: 0 for e in ENGS}
        self.dma_i = {e: 0 for e in ENGS}
        self.dma_val = {}
        self.known = {e: {} for e in ENGS}
        self.last_tok = {e: None for e in ENGS}
        self.dma_latest = {}

    def _wait(self, eng, tok):
        semkey, val, _ = tok
        if self.known[eng].get(semkey, 0) >= val:
            return
        self.known[eng][semkey] = val
        self.streams[eng].append(("wait", semkey, val))

    def _deps(self, eng, reads, writes, is_dma, partial):
        deps = []
        for b in reads:
            deps.extend(b.base)
            deps.extend(b.parts)
        for b in writes:
            deps.extend(b.base)
            if not partial:
                deps.extend(b.parts)
            for e2, t in b.rd.items():
                if e2 == eng and not is_dma:
                    continue
                deps.append(t)
        return [t for t in deps if not (t[2] == "pe" and eng == "pe" and not is_dma)]

    def op(self, eng, fn, reads=(), writes=(), partial=False):
        for t in self._deps(eng, reads, writes, False, partial):
            self._wait(eng, t)
        self.cnt[eng] += 1
        tok = ("E" + eng, self.cnt[eng], eng)
        self.streams[eng].append(("op", fn, tok[0], 1, tok[1]))
        self._commit(eng, tok, reads, writes, partial)
        self.last_tok[eng] = tok
        return tok

    def dma(self, eng, fn, reads=(), writes=(), partial=False):
        i = self.dma_i[eng]
        self.dma_i[eng] += 1
        semkey = "D%s%d" % (eng, i % NDMA)
        prev = self.dma_val.get(semkey, 0)
        if prev:
            self._wait(eng, (semkey, prev, "dma"))
        for t in self._deps(eng, reads, writes, True, partial):
            self._wait(eng, t)
        val = prev + 16
        self.dma_val[semkey] = val
        tok = (semkey, val, "dma")
        self.streams[eng].append(("op", fn, semkey, 16, val))
        self._commit(semkey, tok, reads, writes, partial)
        self.dma_latest[semkey] = tok
        return tok

    def _commit(self, ekey, tok, reads, writes, partial):
        for b in reads:
            b.rd[ekey] = tok
        for b in writes:
            if partial:
                b.parts = [t for t in b.parts if t[0] != tok[0]] + [tok]
            else:
                b.base = [tok]
                b.parts = []
                b.rd = {}

    def barrier(self, engines=ENGS):
        toks = [t for t in self.last_tok.values() if t is not None] + list(self.dma_latest.values())
        for e in engines:
            for t in toks:
                if t[0] == "E" + e:
                    continue
                self._wait(e, t)

    def emit(self):
        nc = self.nc
        semkeys = set()
        waited = set()
        for e in ENGS:
            for it in self.streams[e]:
                if it[0] == "wait":
                    semkeys.add(it[1])
                    waited.add((it[1], it[2]))
        valmap = {}
        incs = {}
        for e in ENGS:
            c = 0
            for k, it in enumerate(self.streams[e]):
                if it[0] == "op" and it[3] == 1:
                    if (it[2], it[4]) in waited:
                        c += 1
                        valmap[(it[2], it[4])] = c
                        incs[(e, k)] = True
                elif it[0] == "op":
                    semkeys.add(it[2])
        with ExitStack() as st:
            sems = {k: st.enter_context(nc.semaphore(k)) for k in sorted(semkeys)}
            block = st.enter_context(nc.Block())

            def run(eobj, ename, items):
                for k, it in enumerate(items):
                    if it[0] == "wait":
                        v = valmap[(it[1], it[2])] if it[1].startswith("E") else it[2]
                        eobj.wait_ge(sems[it[1]], v)
                    elif it[3] == 16:
                        it[1](eobj).then_inc(sems[it[2]], 16)
                    else:
                        ins = it[1](eobj)
                        if (ename, k) in incs:
                            ins.then_inc(sems[it[2]], 1)

            @block.tensor
            def _(e):
                run(e, "pe", self.streams["pe"])

            @block.scalar
            def _(e):
                run(e, "act", self.streams["act"])

            @block.vector
            def _(e):
                run(e, "dve", self.streams["dve"])

            @block.gpsimd
            def _(e):
                run(e, "pool", self.streams["pool"])

            @block.sync
            def _(e):
                run(e, "sp", self.streams["sp"])


def _perm_a():
    cols, groups = [], {}

    def add(name, idx):
        groups[name] = (len(cols), len(idx))
        cols.extend(idx)
    add("QX1", [h * 128 + i for i in range(16) for h in range(8)])
    add("QX2", [h * 128 + 16 + i for i in range(16) for h in range(8)])
    for h in range(8):
        add("QN%d" % h, [h * 128 + 32 + n for n in range(96)])
    add("KR1", [1280 + i for i in range(16)])
    add("KR2", [1280 + 16 + i for i in range(16)])
    add("QIX1", [1312 + h * 64 + i for i in range(8) for h in range(16)])
    add("QIX2", [1312 + h * 64 + 8 + i for i in range(8) for h in range(16)])
    for c in range(6):
        add("QIN%d" % c, [1312 + h * 64 + d for d in range(16 + 8 * c, 24 + 8 * c) for h in range(16)])
    add("KIX1", [2336 + i for i in range(8)])
    add("KIX2", [2336 + 8 + i for i in range(8)])
    add("KIN", [2336 + 16 + i for i in range(48)])
    add("QM0", list(range(2416, 2544)))
    add("QM1", list(range(2544, 2672)))
    add("CKV", list(range(1024, 1280)))
    add("WI", list(range(2400, 2416)))
    return np.array(cols), groups


def _perm_b():
    cols, groups = [], {}

    def add(name, idx):
        groups[name] = (len(cols), len(idx))
        cols.extend(idx)
    add("QX1", [h * 64 + i for i in range(8) for h in range(16)])
    add("QX2", [h * 64 + 8 + i for i in range(8) for h in range(16)])
    for c in range(6):
        add("QN%d" % c, [h * 64 + d for d in range(16 + 8 * c, 24 + 8 * c) for h in range(16)])
    add("KX1", [1024 + h * 64 + i for i in range(8) for h in range(4)])
    add("KX2", [1024 + h * 64 + 8 + i for i in range(8) for h in range(4)])
    add("KN0", [1024 + h * 64 + d for d in range(16, 48) for h in range(4)])
    add("KN1", [1024 + h * 64 + d for d in range(48, 64) for h in range(4)])
    add("QM0", list(range(1536, 1664)))
    add("QM1", list(range(1664, 1792)))
    add("V", list(range(1280, 1536)))
    return np.array(cols), groups


def _perm_c():
    cols, groups = [], {}

    def add(name, idx):
        groups[name] = (len(cols), len(idx))
        cols.extend(idx)
    for pre, base in (("Q", 0), ("K", 1024)):
        add(pre + "X1", [base + h * 128 + i for i in range(16) for h in range(8)])
        add(pre + "X2", [base + h * 128 + 16 + i for i in range(16) for h in range(8)])
        for c in range(6):
            add(pre + "N%d" % c, [base + h * 128 + d for d in range(32 + 16 * c, 48 + 16 * c) for h in range(8)])
    add("QM0", list(range(3072, 3200)))
    add("QM1", list(range(3200, 3328)))
    add("V", list(range(2048, 3072)))
    return np.array(cols), groups


PERMS = {0: _perm_a(), 1: _perm_b(), 2: _perm_c()}


def _consts():
    c = {}
    inv32 = (500000.0 ** (-np.arange(0, 32, 2, dtype=np.float32) / 32)).astype(np.float32)
    inv16 = (500000.0 ** (-np.arange(0, 16, 2, dtype=np.float32) / 16)).astype(np.float32)
    invc = np.zeros((128, 5), np.float32)
    p = np.arange(128)
    invc[:, 0] = inv32[p // 8]
    invc[:, 1] = inv32[p % 16]
    invc[:, 2] = inv16[p // 16]
    invc[:, 3] = inv16[p % 8]
    invc[:, 4] = inv16[(p % 32) // 4]
    c["invc"] = invc
    k = np.arange(128)[:, None]
    q = np.arange(128)[None, :]
    m = np.zeros((128, 4, 128), np.float32)
    m[:, 0] = np.where(k <= q, 0.0, NEG)
    m[:, 1] = np.where(k > q, 0.0, NEG)
    m[:, 2] = np.where(k >= q, 0.0, NEG)
    m[:, 3] = np.eye(128, dtype=np.float32)
    c["cmask"] = m
    sel = np.zeros((128, 16, 128), np.float32)
    for j in range(16):
        for pp in range(128):
            sel[pp, j, 8 * j + pp % 8] = 1.0
    c["sel"] = sel
    zm = np.zeros((128, 128), np.float32)
    for mm in range(128):
        for pp in range(128):
            if mm % 8 == pp % 8:
                zm[mm, pp] = 1.0
    c["zmask"] = zm
    bs = np.zeros((128, 16), np.float32)
    for mm in range(128):
        bs[mm, mm // 8] = 1.0
    c["bsel"] = bs
    c["triq"] = np.where(q.T >= k.T, 0.0, NEG).astype(np.float32) if False else np.where(np.arange(128)[None, :] <= np.arange(128)[:, None], 0.0, NEG).astype(np.float32)
    return c


CONSTS = _consts()


class KB:
    def __init__(self, layers=(0, 1, 2, 3), final=True):
        self.layers = layers
        self.final = final
        nc = self.nc = bass.Bass("TRN2", target_bir_lowering=False)
        self.P = Prog(nc)
        self.inputs = {}
        self.arena = nc.alloc_sbuf_tensor("arena", [128, 206 * 1024 // 4], F32).ap()
        self.cap = 206 * 1024
        self.off = 0
        self.ps = [nc.alloc_psum_tensor("ps%d" % i, [128, 512], F32).ap() for i in range(8)]
        self.psb = [Buf("ps%d" % i) for i in range(8)]
        self.psi = 0

    def din(self, name, shape, dtype=F32):
        ap = self.nc.dram_tensor(name, list(shape), dtype, kind="ExternalInput").ap()
        self.inputs[name] = ap
        return ap

    def dscr(self, name, shape, dtype=BF16):
        return self.nc.dram_tensor(name, list(shape), dtype).ap()

    def tile(self, free, dtype=F32, parts=128):
        if isinstance(free, int):
            free = [free]
        n = int(np.prod(free))
        es = 2 if dtype == BF16 else 4
        nb = (n * es + 31) // 32 * 32
        off = self.off
        self.off += nb
        assert self.off <= self.cap, "SBUF arena overflow %d" % self.off
        v = self.arena[0:parts, off // 4:(off + nb) // 4]
        if dtype != F32:
            v = v.bitcast(dtype)
        v = v[:, 0:n]
        if len(free) == 2:
            v = v.rearrange("p (a b) -> p a b", b=free[1])
        elif len(free) == 3:
            v = v.rearrange("p (a b c) -> p a b c", b=free[1], c=free[2])
        return v

    def bank(self):
        i = self.psi
        self.psi = (self.psi + 1) % 8
        return self.ps[i], self.psb[i]

    def phase_end(self, mark):
        self.P.barrier()
        self.off = mark


def recip(P, out, in_, rbuf, wbuf):
    P.op("dve", lambda e: e.reciprocal(out=out, in_=in_), reads=rbuf, writes=wbuf)


def build_program(layers=(0, 1, 2, 3), final=True, taps=(), ffn=True):
    kb = KB(layers, final)
    nc, P = kb.nc, kb.P
    tile = kb.tile

    xin = kb.din("xT", [D, T])
    memT_d = kb.din("memT", [D, 256])
    pos_d = kb.din("pos", [1, T], I32)
    gv_d = kb.din("gvec", [128, 4 + 4 + 1 + 1, 8])
    invc_d = kb.din("invc", [128, 5])
    cmask_d = kb.din("cmask", [128, 4, 128])
    sel_d = kb.din("sel", [128, 16, 128])
    zmask_d = kb.din("zmask", [128, 128])
    bsel_d = kb.din("bsel", [128, 16])
    triq_d = kb.din("triq", [128, 128])
    w_in_d, w_out_d = {}, {}
    for i in layers:
        w_in_d[i] = kb.din("w_in%d" % i, [D, len(PERMS[i % 3][0])])
        w_out_d[i] = kb.din("w_out%d" % i, [1280, D])
    w_up_d = kb.din("w_up", [4, D, 5632] if ffn else [4, 128, 8])
    w_down_d = kb.din("w_down", [4, 2816, D] if ffn else [4, 128, 8])
    convw_d = kb.din("convw", [4, 128, 22, 3])
    convb_d = kb.din("convb", [4, 128, 22])
    wmem_d = kb.din("wmem", [4, D, 512])
    sinks_d = kb.din("sinks", [128, 8])
    kvn_d = kb.din("kvn", [2, 1, 256])
    wuk_d = kb.din("wukT", [2, 8, 96, 256])
    wuv_d = kb.din("wuv", [2, 8, 256, 128])
    out_d = nc.dram_tensor("outT", [D, T], F32, kind="ExternalOutput").ap()

    XS = [kb.dscr("xs0", [D, T], F32), kb.dscr("xs1", [D, T], F32)]
    MIXd = kb.dscr("mixd", [D, T])
    QMd = kb.dscr("qmd", [256, T])
    Qd = kb.dscr("qd", [D, T])
    Kd = kb.dscr("kd", [D, T])
    Vd = kb.dscr("vd", [T, D])
    ROPE = [(kb.dscr("cos%d" % k, [128, T], F32), kb.dscr("sin%d" % k, [128, T], F32)) for k in range(5)]

    def tsl(tt):
        return slice(tt * TT, (tt + 1) * TT)

    ones_f = tile([128]); b_ones_f = Buf()
    ident = tile([128], BF16); b_ident = Buf()
    ones_b = tile([128], BF16); b_ones_b = Buf()
    onz = tile([192], BF16); b_onz = Buf()
    maskb = tile([3, 512], BF16); b_maskb = Buf()
    gv = tile([10, 8]); b_gv = Buf()
    memn = tile([8, 256], BF16); b_memn = Buf()
    invc = tile([5]); b_invc = Buf()
    epsc = tile([1]); b_eps = Buf()
    P.op("dve", lambda e: e.memset(ones_f, 1.0), writes=[b_ones_f])
    P.op("dve", lambda e: e.memset(epsc, EPS), writes=[b_eps])
    P.op("dve", lambda e: e.memset(ones_b, 1.0), writes=[b_ones_b])
    P.op("dve", lambda e: e.memset(onz, 0.0), writes=[b_onz])
    P.op("dve", lambda e: e.memset(onz[:, 64:128], 1.0), writes=[b_onz])
    P.dma("sp", lambda e: e.dma_start(out=gv, in_=gv_d), writes=[b_gv])
    P.dma("sp", lambda e: e.dma_start(out=invc, in_=invc_d), writes=[b_invc])
    mark0 = kb.off
    cm = tile([4, 128]); b_cm = Buf()
    P.dma("sp", lambda e: e.dma_start(out=cm, in_=cmask_d), writes=[b_cm])
    P.op("dve", lambda e: e.tensor_copy(out=ident, in_=cm[:, 3, :]), reads=[b_cm], writes=[b_ident])
    for k in range(3):
        for r in range(4):
            P.op("dve", lambda e, k=k, r=r: e.tensor_copy(out=maskb[:, k, r * 128:(r + 1) * 128], in_=cm[:, k, :]),
                 reads=[b_cm], writes=[b_maskb], partial=True)

    posi = tile([T], I32); b_posi = Buf()
    posf = tile([T]); b_posf = Buf()
    P.dma("sp", lambda e: e.dma_start(out=posi, in_=pos_d.partition_broadcast(128)), writes=[b_posi])
    P.op("dve", lambda e: e.tensor_copy(out=posf, in_=posi), reads=[b_posi], writes=[b_posf])
    HALF = 2048
    ang = tile([HALF]); b_ang = Buf()
    yk = tile([HALF]); b_yk = Buf()
    ki = tile([HALF], I32); b_ki = Buf()
    rr = tile([HALF]); b_rr = Buf()
    mm_ = tile([HALF]); b_mm = Buf()
    tb = [tile([HALF]), tile([HALF])]; b_tb = [Buf(), Buf()]
    TWO_PI = float(2 * np.pi)
    tbi = 0
    for k in range(5):
        for half in range(2):
            hs = slice(half * HALF, (half + 1) * HALF)
            for which in range(2):
                shift = float(np.pi / 2) if which == 0 else 0.0
                P.op("dve", lambda e, k=k, hs=hs, shift=shift: e.tensor_scalar(out=ang, in0=posf[:, hs], scalar1=invc[:, k:k + 1], scalar2=shift, op0=ALU.mult, op1=ALU.add),
                     reads=[b_posf, b_invc], writes=[b_ang])
                P.op("dve", lambda e: e.tensor_scalar(out=yk, in0=ang, scalar1=1.0 / TWO_PI, scalar2=None, op0=ALU.mult), reads=[b_ang], writes=[b_yk])
                P.op("dve", lambda e: e.tensor_copy(out=ki, in_=yk), reads=[b_yk], writes=[b_ki])
                P.op("dve", lambda e: e.tensor_copy(out=yk, in_=ki), reads=[b_ki], writes=[b_yk])
                P.op("dve", lambda e: e.scalar_tensor_tensor(out=rr, in0=yk, scalar=-TWO_PI, in1=ang, op0=ALU.mult, op1=ALU.add), reads=[b_yk, b_ang], writes=[b_rr])
                P.op("dve", lambda e: e.tensor_scalar(out=mm_, in0=rr, scalar1=float(np.pi), scalar2=-TWO_PI, op0=ALU.is_gt, op1=ALU.mult), reads=[b_rr], writes=[b_mm])
                P.op("dve", lambda e: e.tensor_tensor(out=rr, in0=rr, in1=mm_, op=ALU.add), reads=[b_rr, b_mm], writes=[b_rr])
                P.op("dve", lambda e: e.tensor_scalar(out=mm_, in0=rr, scalar1=-float(np.pi), scalar2=TWO_PI, op0=ALU.is_lt, op1=ALU.mult), reads=[b_rr], writes=[b_mm])
                P.op("dve", lambda e: e.tensor_tensor(out=rr, in0=rr, in1=mm_, op=ALU.add), reads=[b_rr, b_mm], writes=[b_rr])
                P.op("dve", lambda e: e.tensor_scalar(out=rr, in0=rr, scalar1=3.14159, scalar2=-3.14159, op0=ALU.min, op1=ALU.max), reads=[b_rr], writes=[b_rr])
                t_, bt_ = tb[tbi % 2], b_tb[tbi % 2]
                tbi += 1
                P.op("act", lambda e, t_=t_: e.activation(out=t_, in_=rr, func=AF.Sin), reads=[b_rr], writes=[bt_])
                dst = ROPE[k][which]
                P.dma("sp", lambda e, t_=t_, dst=dst, hs=hs: e.dma_start(out=dst[:, hs], in_=t_), reads=[bt_])

    mt = tile([8, 256]); b_mt = Buf()
    msq = tile([8, 256]); b_msq = Buf()
    mrs = tile([256]); b_mrs = Buf()
    P.dma("sp", lambda e: e.dma_start(out=mt, in_=memT_d.rearrange("(kc p) m -> p kc m", p=128)), writes=[b_mt])
    P.op("act", lambda e: e.activation(out=msq, in_=mt, func=AF.Square), reads=[b_mt], writes=[b_msq])
    ps, pb = kb.bank()

    def f_(e, ps=ps):
        for kc in range(8):
            ins = e.matmul(ps[:, 0:256], lhsT=ones_f, rhs=msq[:, kc, :], start=(kc == 0), stop=(kc == 7))
        return ins
    P.op("pe", f_, reads=[b_msq, b_ones_f], writes=[pb])
    P.op("act", lambda e, ps=ps: e.activation(out=mrs, in_=ps[:, 0:256], func=AF.Sqrt, scale=1.0 / D, bias=epsc[:, 0:1]), reads=[pb, b_eps], writes=[b_mrs])
    recip(P, mrs, mrs, [b_mrs], [b_mrs])
    for kc in range(8):
        P.op("dve", lambda e, kc=kc: e.scalar_tensor_tensor(out=memn[:, kc, :], in0=mt[:, kc, :], scalar=gv[:, 8, kc:kc + 1], in1=mrs, op0=ALU.mult, op1=ALU.mult),
             reads=[b_mt, b_gv, b_mrs], writes=[b_memn], partial=True)
    kb.phase_end(mark0)
    PERSIST = kb.off

    def load_w(w_d, c0, ncols, dst, b_dst, kcs=8):
        src = w_d.rearrange("(kc p) m -> p kc m", p=128)
        c = 0
        while c < ncols:
            n = min(512, ncols - c)
            P.dma("pool", lambda e, c=c, n=n: e.dma_start(out=dst[:, :, c:c + n], in_=src[:, :, c0 + c:c0 + c + n]), writes=[b_dst], partial=True)
            c += n

    class NormCtx:
        pass

    def norm_setup():
        n = NormCtx()
        n.xt = [tile([8, TT]), tile([8, TT])]; n.b_xt = [Buf(), Buf()]
        n.sq = tile([8, TT]); n.b_sq = Buf()
        n.rs = tile([TT]); n.b_rs = Buf()
        n.hT = [tile([8, TT], BF16), tile([8, TT], BF16)]; n.b_hT = [Buf(), Buf()]
        return n

    def norm_tile(n, xsrc, tt, gidx, want_x=False):
        s = tt % 2
        xt, bx = n.xt[s], n.b_xt[s]
        P.dma("sp", lambda e: e.dma_start(out=xt, in_=xsrc.rearrange("(kc p) t -> p kc t", p=128)[:, :, tsl(tt)]), writes=[bx])
        P.op("act", lambda e: e.activation(out=n.sq, in_=xt, func=AF.Square), reads=[bx], writes=[n.b_sq])
        ps, pb = kb.bank()

        def f(e):
            for kc in range(8):
                ins = e.matmul(ps, lhsT=ones_f, rhs=n.sq[:, kc, :], start=(kc == 0), stop=(kc == 7))
            return ins
        P.op("pe", f, reads=[n.b_sq, b_ones_f], writes=[pb])
        P.op("act", lambda e: e.activation(out=n.rs, in_=ps, func=AF.Sqrt, scale=1.0 / D, bias=epsc[:, 0:1]), reads=[pb, b_eps], writes=[n.b_rs])
        recip(P, n.rs, n.rs, [n.b_rs], [n.b_rs])
        hT, bh = n.hT[s], n.b_hT[s]
        for kc in range(8):
            P.op("dve", lambda e, kc=kc: e.scalar_tensor_tensor(out=hT[:, kc, :], in0=xt[:, kc, :], scalar=gv[:, gidx, kc:kc + 1], in1=n.rs, op0=ALU.mult, op1=ALU.mult),
                 reads=[bx, b_gv, n.b_rs], writes=[bh], partial=(kc > 0))
        return hT, bh, xt, bx

    def proj(hT, bh, w_sb, b_w, c0, M, n0=0, nn=TT):
        ps, pb = kb.bank()

        def f(e):
            for kc in range(8):
                ins = e.matmul(ps[0:M, 0:nn], lhsT=w_sb[:, kc, c0:c0 + M], rhs=hT[:, kc, n0:n0 + nn], start=(kc == 0), stop=(kc == 7))
            return ins
        P.op("pe", f, reads=[bh, b_w], writes=[pb])
        return ps, pb

    def proj_tok(hT, bh, w_sb, b_w, c0, N, tb):
        ps, pb = kb.bank()

        def f(e):
            for kc in range(8):
                ins = e.matmul(ps[:, 0:N], lhsT=hT[:, kc, tb * 128:(tb + 1) * 128], rhs=w_sb[:, kc, c0:c0 + N], start=(kc == 0), stop=(kc == 7))
            return ins
        P.op("pe", f, reads=[bh, b_w], writes=[pb])
        return ps, pb

    class RopeCtx:
        pass

    def rope_setup():
        r = RopeCtx()
        r.t1 = tile([TT]); r.b1 = Buf()
        r.t2 = tile([TT]); r.b2 = Buf()
        return r

    def rope(r, psA, pbA, psB, pbB, M, cos, sin, b_cs, o1, o2, b_o, partial=True):
        A, B = psA[0:M, :], psB[0:M, :]
        C, S_ = cos[0:M, :], sin[0:M, :]
        t1, t2 = r.t1[0:M, :], r.t2[0:M, :]
        P.op("dve", lambda e: e.tensor_tensor(out=t1, in0=A, in1=C, op=ALU.mult), reads=[pbA, b_cs], writes=[r.b1])
        P.op("dve", lambda e: e.tensor_tensor(out=t2, in0=B, in1=S_, op=ALU.mult), reads=[pbB, b_cs], writes=[r.b2])
        P.op("dve", lambda e: e.tensor_tensor(out=o1, in0=t1, in1=t2, op=ALU.subtract), reads=[r.b1, r.b2], writes=[b_o], partial=partial)
        P.op("dve", lambda e: e.tensor_tensor(out=t1, in0=B, in1=C, op=ALU.mult), reads=[pbB, b_cs], writes=[r.b1])
        P.op("dve", lambda e: e.tensor_tensor(out=t2, in0=A, in1=S_, op=ALU.mult), reads=[pbA, b_cs], writes=[r.b2])
        P.op("dve", lambda e: e.tensor_tensor(out=o2, in0=t1, in1=t2, op=ALU.add), reads=[r.b1, r.b2], writes=[b_o], partial=partial)

    def load_cs(k, tt, cs, b_cs, rows=128):
        P.dma("sp", lambda e: e.dma_start(out=cs[0][0:rows, :], in_=ROPE[k][0][0:rows, tsl(tt)]), writes=[b_cs])
        P.dma("sp", lambda e: e.dma_start(out=cs[1][0:rows, :], in_=ROPE[k][1][0:rows, tsl(tt)]), writes=[b_cs], partial=True)

    def evac(ps, pb, M, out, b_out, partial=True, eng="act", nn=TT):
        if eng == "act":
            P.op("act", lambda e: e.activation(out=out, in_=ps[0:M, 0:nn], func=AF.Copy), reads=[pb], writes=[b_out], partial=partial)
        else:
            P.op("dve", lambda e: e.tensor_copy(out=out, in_=ps[0:M, 0:nn]), reads=[pb], writes=[b_out], partial=partial)

    def phase_out(i, xsrc, xdst):
        mark = kb.off
        wm_sb = tile([8, 512], BF16); b_wm = Buf()
        load_w(wmem_d[i], 0, 512, wm_sb, b_wm)
        wo_sb = tile([10, D], BF16); b_wo = Buf()
        load_w(w_out_d[i], 0, D, wo_sb, b_wo)
        kTm = tile([2, 256], BF16); b_kTm = Buf()
        VM = tile([2, 4, 192], BF16); b_VM = Buf()
        P.op("dve", lambda e: e.memset(VM, 0.0), writes=[b_VM])
        for cc in range(2):
            ps, pb = kb.bank()

            def f(e, ps=ps, cc=cc):
                for kc in range(8):
                    ins = e.matmul(ps[:, 0:256], lhsT=wm_sb[:, kc, cc * 128:(cc + 1) * 128], rhs=memn[:, kc, :], start=(kc == 0), stop=(kc == 7))
                return ins
            P.op("pe", f, reads=[b_wm, b_memn], writes=[pb])
            evac(ps, pb, 128, kTm[:, cc, :], b_kTm, nn=256)
        for mb in range(2):
            ps, pb = kb.bank()

            def f(e, ps=ps, mb=mb):
                for kc in range(8):
                    ins = e.matmul(ps[:, 0:256], lhsT=memn[:, kc, mb * 128:(mb + 1) * 128], rhs=wm_sb[:, kc, 256:512], start=(kc == 0), stop=(kc == 7))
                return ins
            P.op("pe", f, reads=[b_wm, b_memn], writes=[pb])
            P.op("act", lambda e, ps=ps, mb=mb: e.activation(out=VM[:, mb, :, 64:128], in_=ps[:, 0:256].rearrange("p (h c) -> p h c", c=64), func=AF.Copy),
                 reads=[pb], writes=[b_VM], partial=True)
        qm = [tile([2, TT], BF16), tile([2, TT], BF16)]; b_qm = [Buf(), Buf()]
        mix = [tile([8, TT], BF16), tile([8, TT], BF16)]; b_mix = [Buf(), Buf()]
        xt = [tile([8, TT]), tile([8, TT])]; b_xt = [Buf(), Buf()]
        memo = tile([2, TT], BF16); b_memo = Buf()
        PT = [tile([TT], BF16) for _ in range(4)]; b_PT = [Buf() for _ in range(4)]
        rL = tile([TT]); b_rL = Buf()
        pti = 0
        for tt in range(NTT):
            s = tt % 2
            P.dma("sp", lambda e, s=s, tt=tt: e.dma_start(out=qm[s], in_=QMd.rearrange("(c p) t -> p c t", p=128)[:, :, tsl(tt)]), writes=[b_qm[s]])
            P.dma("sp", lambda e, s=s, tt=tt: e.dma_start(out=mix[s], in_=MIXd.rearrange("(c p) t -> p c t", p=128)[:, :, tsl(tt)]), writes=[b_mix[s]])
            P.dma("sp", lambda e, s=s, tt=tt: e.dma_start(out=xt[s], in_=xsrc.rearrange("(c p) t -> p c t", p=128)[:, :, tsl(tt)]), writes=[b_xt[s]])
            for cc in range(2):
                psU, pbU = kb.bank()
                psL, pbL = kb.bank()
                first = True
                for hh in range(2):
                    head = 2 * cc + hh
                    rs_ = slice(64 * hh, 64 * hh + 64)
                    vs_ = slice(64, 192) if hh == 0 else slice(0, 128)
                    for mb in range(2):
                        ps, pb = kb.bank()
                        P.op("pe", lambda e, ps=ps, rs_=rs_, cc=cc, mb=mb, s=s: e.matmul(ps, lhsT=kTm[rs_, cc, mb * 128:(mb + 1) * 128], rhs=qm[s][rs_, cc, :], start=True, stop=True),
                             reads=[b_kTm, b_qm[s]], writes=[pb])
                        pt, bpt = PT[pti % 4], b_PT[pti % 4]
                        pti += 1
                        P.op("act", lambda e, ps=ps, pt=pt: e.activation(out=pt, in_=ps, func=AF.Exp, scale=0.125), reads=[pb], writes=[bpt])
                        last = (hh == 1 and mb == 1)

                        def f(e, pt=pt, mb=mb, head=head, vs_=vs_, first=first, last=last, psU=psU, psL=psL):
                            e.matmul(psU, lhsT=VM[:, mb, head, vs_], rhs=pt, start=first, stop=last, skip_group_check=True)
                            return e.matmul(psL, lhsT=onz[:, vs_], rhs=pt, start=first, stop=last, skip_group_check=True)
                        P.op("pe", f, reads=[bpt, b_VM, b_onz], writes=[pbU, pbL], partial=not first)
                        first = False
                recip(P, rL, psL, [pbL], [b_rL])
                P.op("dve", lambda e, psU=psU, cc=cc: e.tensor_tensor(out=memo[:, cc, :], in0=psU, in1=rL, op=ALU.mult), reads=[pbU, b_rL], writes=[b_memo], partial=(cc > 0))
            for m in range(8):
                ps, pb = kb.bank()

                def f(e, ps=ps, m=m, s=s):
                    for c in range(8):
                        e.matmul(ps, lhsT=wo_sb[:, c, m * 128:(m + 1) * 128], rhs=mix[s][:, c, :], start=(c == 0), stop=False)
                    e.matmul(ps, lhsT=wo_sb[:, 8, m * 128:(m + 1) * 128], rhs=memo[:, 0, :], start=False, stop=False)
                    return e.matmul(ps, lhsT=wo_sb[:, 9, m * 128:(m + 1) * 128], rhs=memo[:, 1, :], start=False, stop=True)
                P.op("pe", f, reads=[b_wo, b_mix[s], b_memo], writes=[pb])
                P.op("dve", lambda e, ps=ps, m=m, s=s: e.tensor_tensor(out=xt[s][:, m, :], in0=xt[s][:, m, :], in1=ps, op=ALU.add), reads=[pb, b_xt[s]], writes=[b_xt[s]], partial=True)
            P.dma("sp", lambda e, s=s, tt=tt: e.dma_start(out=xdst.rearrange("(c p) t -> p c t", p=128)[:, :, tsl(tt)], in_=xt[s]), reads=[b_xt[s]])
        kb.phase_end(mark)

    def phase_ffn(i, xsrc, xdst):
        mark = kb.off
        ST = 2048
        NS = ST // TT
        hA = tile([8, ST], BF16); b_hA = [Buf() for _ in range(NS)]
        g = tile([22, ST], BF16); b_g = [Buf() for _ in range(NS)]
        xt = tile([8, TT]); b_xt = Buf()
        sq = tile([8, TT], BF16); b_sq = Buf()
        rs = tile([TT]); b_rs = Buf()
        wa = [tile([8, 256], BF16), tile([8, 256], BF16)]; b_wa = [Buf(), Buf()]
        wb = [tile([8, 256], BF16), tile([8, 256], BF16)]; b_wb = [Buf(), Buf()]
        wdn = [tile([22, 128], BF16), tile([22, 128], BF16)]; b_wdn = [Buf(), Buf()]
        cw = tile([22, 3]); b_cw = Buf()
        cb = tile([22]); b_cb = Buf()
        halo = tile([22, 2]); b_halo = Buf()
        a_sb = [tile([TT + 2]), tile([TT + 2])]; b_a = [Buf(), Buf()]
        cv = [tile([TT]), tile([TT])]; b_cv = [Buf(), Buf()]
        sg = [tile([TT]), tile([TT])]; b_sg = [Buf(), Buf()]
        xr = [tile([TT]), tile([TT])]; b_xr = [Buf(), Buf()]
        P.dma("sp", lambda e: e.dma_start(out=cw, in_=convw_d[i]), writes=[b_cw])
        P.dma("sp", lambda e: e.dma_start(out=cb, in_=convb_d[i]), writes=[b_cb])
        P.op("dve", lambda e: e.memset(halo, 0.0), writes=[b_halo])
        wupr = w_up_d[i].rearrange("(kc p) m -> p kc m", p=128)
        wdnr = w_down_d[i].rearrange("(c p) m -> p c m", p=128)
        gi = 4 + i
        ai = 0
        for st in range(T // ST):
            for tl in range(NS):
                tt = st * NS + tl
                P.dma("sp", lambda e, tt=tt: e.dma_start(out=xt, in_=xsrc.rearrange("(kc p) t -> p kc t", p=128)[:, :, tsl(tt)]), writes=[b_xt])
                P.op("act", lambda e: e.activation(out=sq, in_=xt, func=AF.Square), reads=[b_xt], writes=[b_sq])
                ps, pb = kb.bank()

                def f(e, ps=ps):
                    for kc in range(8):
                        ins = e.matmul(ps, lhsT=ones_b, rhs=sq[:, kc, :], start=(kc == 0), stop=(kc == 7))
                    return ins
                P.op("pe", f, reads=[b_sq, b_ones_b], writes=[pb])
                P.op("act", lambda e, ps=ps: e.activation(out=rs, in_=ps, func=AF.Sqrt, scale=1.0 / D, bias=epsc[:, 0:1]), reads=[pb, b_eps], writes=[b_rs])
                recip(P, rs, rs, [b_rs], [b_rs])
                for kc in range(8):
                    P.op("dve", lambda e, kc=kc, tl=tl: e.scalar_tensor_tensor(out=hA[:, kc, tl * TT:(tl + 1) * TT], in0=xt[:, kc, :], scalar=gv[:, gi, kc:kc + 1], in1=rs, op0=ALU.mult, op1=ALU.mult),
                         reads=[b_xt, b_gv, b_rs], writes=[b_hA[tl]], partial=(kc > 0))
            for pc in range(11):
                s = pc % 2
                P.dma("pool", lambda e, s=s, pc=pc: e.dma_start(out=wa[s], in_=wupr[:, :, pc * 256:(pc + 1) * 256]), writes=[b_wa[s]])
                P.dma("pool", lambda e, s=s, pc=pc: e.dma_start(out=wb[s], in_=wupr[:, :, 2816 + pc * 256:2816 + (pc + 1) * 256]), writes=[b_wb[s]])
                for cl in range(2):
                    c = pc * 2 + cl
                    for tl in range(NS):
                        tt = st * NS + tl
                        psa, pba = kb.bank()
                        psb, pbb = kb.bank()

                        def f(e, psa=psa, psb=psb, s=s, cl=cl, tl=tl):
                            for kc in range(8):
                                e.matmul(psa, lhsT=wa[s][:, kc, cl * 128:(cl + 1) * 128], rhs=hA[:, kc, tl * TT:(tl + 1) * TT], start=(kc == 0), stop=(kc == 7))
                            for kc in range(8):
                                ins = e.matmul(psb, lhsT=wb[s][:, kc, cl * 128:(cl + 1) * 128], rhs=hA[:, kc, tl * TT:(tl + 1) * TT], start=(kc == 0), stop=(kc == 7))
                            return ins
                        P.op("pe", f, reads=[b_wa[s], b_wb[s], b_hA[tl]], writes=[pba, pbb])
                        a_, ba_ = a_sb[ai % 2], b_a[ai % 2]
                        cv_, bcv_ = cv[ai % 2], b_cv[ai % 2]
                        sg_, bsg_ = sg[ai % 2], b_sg[ai % 2]
                        ai += 1
                        P.op("act", lambda e, a_=a_, psa=psa: e.activation(out=a_[:, 2:TT + 2], in_=psa, func=AF.Copy), reads=[pba], writes=[ba_])
                        P.op("dve", lambda e, a_=a_, c=c: e.tensor_copy(out=a_[:, 0:2], in_=halo[:, c, :]), reads=[b_halo], writes=[ba_], partial=True)
                        P.op("dve", lambda e, a_=a_, cv_=cv_, c=c: e.tensor_scalar(out=cv_, in0=a_[:, 2:TT + 2], scalar1=cw[:, c, 2:3], scalar2=cb[:, c:c + 1], op0=ALU.mult, op1=ALU.add),
                             reads=[ba_, b_cw, b_cb], writes=[bcv_])
                        P.op("dve", lambda e, a_=a_, cv_=cv_, c=c: e.scalar_tensor_tensor(out=cv_, in0=a_[:, 1:TT + 1], scalar=cw[:, c, 1:2], in1=cv_, op0=ALU.mult, op1=ALU.add),
                             reads=[ba_, b_cw, bcv_], writes=[bcv_])
                        P.op("dve", lambda e, a_=a_, cv_=cv_, c=c: e.scalar_tensor_tensor(out=cv_, in0=a_[:, 0:TT], scalar=cw[:, c, 0:1], in1=cv_, op0=ALU.mult, op1=ALU.add),
                             reads=[ba_, b_cw, bcv_], writes=[bcv_])
                        P.op("dve", lambda e, a_=a_, c=c: e.tensor_copy(out=halo[:, c, :], in_=a_[:, TT:TT + 2]), reads=[ba_], writes=[b_halo], partial=True)
                        P.op("act", lambda e, cv_=cv_, sg_=sg_: e.activation(out=sg_, in_=cv_, func=AF.Silu), reads=[bcv_], writes=[bsg_])
                        P.op("dve", lambda e, sg_=sg_, psb=psb, c=c, tl=tl: e.tensor_tensor(out=g[:, c, tl * TT:(tl + 1) * TT], in0=sg_, in1=psb, op=ALU.mult),
                             reads=[bsg_, pbb], writes=[b_g[tl]], partial=True)
            xi = 0
            for m in range(8):
                s = m % 2
                P.dma("pool", lambda e, s=s, m=m: e.dma_start(out=wdn[s], in_=wdnr[:, :, m * 128:(m + 1) * 128]), writes=[b_wdn[s]])
                for tl in range(NS):
                    tt = st * NS + tl
                    xr_, bxr_ = xr[xi % 2], b_xr[xi % 2]
                    xi += 1
                    P.dma("sp", lambda e, xr_=xr_, m=m, tt=tt: e.dma_start(out=xr_, in_=xsrc[m * 128:(m + 1) * 128, tsl(tt)]), writes=[bxr_])
                    ps, pb = kb.bank()

                    def f(e, ps=ps, s=s, tl=tl):
                        for c in range(22):
                            ins = e.matmul(ps, lhsT=wdn[s][:, c, :], rhs=g[:, c, tl * TT:(tl + 1) * TT], start=(c == 0), stop=(c == 21))
                        return ins
                    P.op("pe", f, reads=[b_wdn[s], b_g[tl]], writes=[pb])
                    P.op("dve", lambda e, xr_=xr_, ps=ps: e.tensor_tensor(out=xr_, in0=xr_, in1=ps, op=ALU.add), reads=[pb, bxr_], writes=[bxr_])
                    P.dma("sp", lambda e, xr_=xr_, m=m, tt=tt: e.dma_start(out=xdst[m * 128:(m + 1) * 128, tsl(tt)], in_=xr_), reads=[bxr_])
        kb.phase_end(mark)

    def phase_final(xsrc):
        mark = kb.off
        n = norm_setup()
        ot = [tile([8, TT]), tile([8, TT])]; b_ot = [Buf(), Buf()]
        for tt in range(NTT):
            s = tt % 2
            xt, bx = n.xt[s], n.b_xt[s]
            P.dma("sp", lambda e, xt=xt, tt=tt: e.dma_start(out=xt, in_=xsrc.rearrange("(kc p) t -> p kc t", p=128)[:, :, tsl(tt)]), writes=[bx])
            P.op("act", lambda e, xt=xt: e.activation(out=n.sq, in_=xt, func=AF.Square), reads=[bx], writes=[n.b_sq])
            ps, pb = kb.bank()

            def f(e, ps=ps):
                for kc in range(8):
                    ins = e.matmul(ps, lhsT=ones_f, rhs=n.sq[:, kc, :], start=(kc == 0), stop=(kc == 7))
                return ins
            P.op("pe", f, reads=[n.b_sq, b_ones_f], writes=[pb])
            P.op("act", lambda e, ps=ps: e.activation(out=n.rs, in_=ps, func=AF.Sqrt, scale=1.0 / D, bias=epsc[:, 0:1]), reads=[pb, b_eps], writes=[n.b_rs])
            recip(P, n.rs, n.rs, [n.b_rs], [n.b_rs])
            for kc in range(8):
                P.op("dve", lambda e, kc=kc, xt=xt, s=s: e.scalar_tensor_tensor(out=ot[s][:, kc, :], in0=xt[:, kc, :], scalar=gv[:, 9, kc:kc + 1], in1=n.rs, op0=ALU.mult, op1=ALU.mult),
                     reads=[bx, b_gv, n.b_rs], writes=[b_ot[s]], partial=(kc > 0))
            P.dma("sp", lambda e, s=s, tt=tt: e.dma_start(out=out_d.rearrange("(kc p) t -> p kc t", p=128)[:, :, tsl(tt)], in_=ot[s]), reads=[b_ot[s]])
        kb.phase_end(mark)

    def p1_b(i, xsrc):
        mark = kb.off
        cols, G = PERMS[1]
        NC = len(cols)
        w_sb = tile([8, NC], BF16); b_w = Buf()
        load_w(w_in_d[i], 0, NC, w_sb, b_w)
        n = norm_setup()
        r = rope_setup()
        csq = [tile([TT]), tile([TT])]; b_csq = Buf()
        csk = [tile([TT]), tile([TT])]; b_csk = Buf()
        qst = [tile([8, TT], BF16), tile([8, TT], BF16)]; b_qst = [Buf(), Buf()]
        kst = [tile([4, TT], BF16), tile([4, TT], BF16)]; b_kst = [Buf(), Buf()]
        qmst = [tile([2, TT], BF16), tile([2, TT], BF16)]; b_qmst = [Buf(), Buf()]
        vst = [tile([4, 256], BF16), tile([4, 256], BF16)]; b_vst = [Buf(), Buf()]
        for tt in range(NTT):
            s = tt % 2
            hT, bh, _, _ = norm_tile(n, xsrc, tt, i)
            load_cs(2, tt, csq, b_csq)
            load_cs(4, tt, csk, b_csk, rows=32)
            pA, bA = proj(hT, bh, w_sb, b_w, G["QX1"][0], 128)
            pB, bB = proj(hT, bh, w_sb, b_w, G["QX2"][0], 128)
            rope(r, pA, bA, pB, bB, 128, csq[0], csq[1], b_csq, qst[s][:, 0, :], qst[s][:, 1, :], b_qst[s])
            for c in range(6):
                ps, pb = proj(hT, bh, w_sb, b_w, G["QN%d" % c][0], 128)
                evac(ps, pb, 128, qst[s][:, 2 + c, :], b_qst[s], eng=("act" if c % 2 == 0 else "dve"))
            P.dma("sp", lambda e, s=s, tt=tt: e.dma_start(out=Qd.rearrange("(c p) t -> p c t", p=128)[:, :, tsl(tt)], in_=qst[s]), reads=[b_qst[s]])
            pA, bA = proj(hT, bh, w_sb, b_w, G["KX1"][0], 32)
            pB, bB = proj(hT, bh, w_sb, b_w, G["KX2"][0], 32)
            rope(r, pA, bA, pB, bB, 32, csk[0], csk[1], b_csk, kst[s][0:32, 0, :], kst[s][0:32, 1, :], b_kst[s])
            ps, pb = proj(hT, bh, w_sb, b_w, G["KN0"][0], 128)
            evac(ps, pb, 128, kst[s][:, 2, :], b_kst[s])
            ps, pb = proj(hT, bh, w_sb, b_w, G["KN1"][0], 64)
            evac(ps, pb, 64, kst[s][0:64, 3, :], b_kst[s], eng="dve")
            for (r0, nr, ci) in ((0, 32, 0), (32, 32, 1), (64, 128, 2), (192, 64, 3)):
                P.dma("sp", lambda e, s=s, tt=tt, r0=r0, nr=nr, ci=ci: e.dma_start(out=Kd[r0:r0 + nr, tsl(tt)], in_=kst[s][0:nr, ci, :]), reads=[b_kst[s]])
            for c in range(2):
                ps, pb = proj(hT, bh, w_sb, b_w, G["QM%d" % c][0], 128)
                evac(ps, pb, 128, qmst[s][:, c, :], b_qmst[s], eng=("act" if c == 0 else "dve"))
            P.dma("sp", lambda e, s=s, tt=tt: e.dma_start(out=QMd.rearrange("(c p) t -> p c t", p=128)[:, :, tsl(tt)], in_=qmst[s]), reads=[b_qmst[s]])
            for tb_ in range(4):
                ps, pb = proj_tok(hT, bh, w_sb, b_w, G["V"][0], 256, tb_)
                evac(ps, pb, 128, vst[s][:, tb_, :], b_vst[s], nn=256, eng=("act" if tb_ % 2 == 0 else "dve"))
            P.dma("sp", lambda e, s=s, tt=tt: e.dma_start(out=Vd[:, 0:256].rearrange("(n p) c -> p n c", p=128)[:, tt * 4:tt * 4 + 4, :], in_=vst[s]), reads=[b_vst[s]])
        kb.phase_end(mark)

    def p2_b(i):
        mark = kb.off
        K_sb = tile([4, T], BF16, parts=64); b_K = Buf()
        P.dma("sp", lambda e: e.dma_start(out=K_sb, in_=Kd[0:256, :].rearrange("(d h) t -> d h t", h=4)), writes=[b_K])
        VZ = tile([32, 4, 192], BF16); b_VZ = Buf()
        P.op("dve", lambda e: e.memset(VZ, 0.0), writes=[b_VZ])
        for g in range(4):
            P.dma("sp", lambda e, g=g: e.dma_start(out=VZ[:, :, g, 64:128], in_=Vd[:, g * 64:(g + 1) * 64].rearrange("(n p) c -> p n c", p=128)), writes=[b_VZ], partial=True)
        esk = tile([8]); b_esk = Buf()
        P.dma("sp", lambda e: e.dma_start(out=esk, in_=sinks_d), writes=[b_esk])
        P.op("act", lambda e: e.activation(out=esk, in_=esk, func=AF.Exp), reads=[b_esk], writes=[b_esk])
        Q_sb = [tile([16, TT], BF16, parts=64), tile([16, TT], BF16, parts=64)]; b_Q = [Buf(), Buf()]
        mixst = [tile([8, TT], BF16), tile([8, TT], BF16)]; b_mixst = [Buf(), Buf()]
        NPT = 10
        PT = [tile([TT], BF16) for _ in range(NPT)]; b_PT = [Buf() for _ in range(NPT)]
        Ls = tile([TT]); b_Ls = Buf()
        pti = 0
        for tt in range(NTT):
            s = tt % 2
            P.dma("sp", lambda e, s=s, tt=tt: e.dma_start(out=Q_sb[s], in_=Qd.rearrange("(d h) t -> d h t", h=16)[:, :, tsl(tt)]), writes=[b_Q[s]])
            for g in range(4):
                pts = {}
                for qb in range(4):
                    nb = tt * 4 + qb
                    for w_, kb_ in enumerate((nb - 1, nb)):
                        if kb_ < 0:
                            continue
                        kind = 1 if w_ == 0 else 0
                        ps, pb = kb.bank()

                        def f(e, ps=ps, kind=kind, g=g, kb_=kb_, qb=qb, s=s):
                            e.matmul(ps, lhsT=ident, rhs=maskb[:, kind, :], start=True, stop=False)
                            return e.matmul(ps, lhsT=K_sb[:, g, kb_ * 128:(kb_ + 1) * 128], rhs=Q_sb[s][:, 4 * g:4 * g + 4, qb * 128:(qb + 1) * 128], start=False, stop=True)
                        P.op("pe", f, reads=[b_ident, b_maskb, b_K, b_Q[s]], writes=[pb])
                        pt, bpt = PT[pti % NPT], b_PT[pti % NPT]
                        pti += 1
                        P.op("act", lambda e, ps=ps, pt=pt: e.activation(out=pt, in_=ps, func=AF.Exp, scale=0.125), reads=[pb], writes=[bpt])
                        pts[(qb, w_)] = (pt, bpt, kb_)
                for cc in range(2):
                    psU, pbU = kb.bank()
                    psL, pbL = kb.bank()
                    items = sorted(pts.items())

                    def f(e, items=items, cc=cc, g=g, psU=psU, psL=psL):
                        first = True
                        for (qb, w_), (pt, bpt, kb_) in items:
                            qs = slice(qb * 128, (qb + 1) * 128)
                            for hh in range(2):
                                hq = 2 * cc + hh
                                vs_ = slice(64, 192) if hh == 0 else slice(0, 128)
                                e.matmul(psU[:, qs], lhsT=VZ[:, kb_, g, vs_], rhs=pt[:, hq * 128:(hq + 1) * 128], start=first, stop=False, skip_group_check=True)
                                first = False
                        first = True
                        for (qb, w_), (pt, bpt, kb_) in items:
                            qs = slice(qb * 128, (qb + 1) * 128)
                            for hh in range(2):
                                hq = 2 * cc + hh
                                vs_ = slice(64, 192) if hh == 0 else slice(0, 128)
                                ins = e.matmul(psL[:, qs], lhsT=onz[:, vs_], rhs=pt[:, hq * 128:(hq + 1) * 128], start=first, stop=False, skip_group_check=True)
                                first = False
                        return ins
                    P.op("pe", f, reads=[v[1] for v in pts.values()] + [b_VZ, b_onz], writes=[pbU, pbL])
                    ch = 2 * g + cc
                    P.op("dve", lambda e, psL=psL, ch=ch: e.tensor_scalar(out=Ls, in0=psL, scalar1=esk[:, ch:ch + 1], scalar2=None, op0=ALU.add), reads=[pbL, b_esk], writes=[b_Ls])
                    recip(P, Ls, Ls, [b_Ls], [b_Ls])
                    P.op("dve", lambda e, psU=psU, ch=ch, s=s: e.tensor_tensor(out=mixst[s][:, ch, :], in0=psU, in1=Ls, op=ALU.mult), reads=[pbU, b_Ls], writes=[b_mixst[s]], partial=True)
            P.dma("sp", lambda e, s=s, tt=tt: e.dma_start(out=MIXd.rearrange("(c p) t -> p c t", p=128)[:, :, tsl(tt)], in_=mixst[s]), reads=[b_mixst[s]])
        kb.phase_end(mark)

    def p1_c(i, xsrc):
        mark = kb.off
        cols, G = PERMS[2]
        NC = len(cols)
        w_sb = tile([8, NC], BF16); b_w = Buf()
        load_w(w_in_d[i], 0, NC, w_sb, b_w)
        n = norm_setup()
        r = rope_setup()
        cs = [tile([TT]), tile([TT])]; b_cs = Buf()
        qst = [tile([8, TT], BF16), tile([8, TT], BF16)]; b_qst = [Buf(), Buf()]
        kst = [tile([8, TT], BF16), tile([8, TT], BF16)]; b_kst = [Buf(), Buf()]
        qmst = [tile([2, TT], BF16), tile([2, TT], BF16)]; b_qmst = [Buf(), Buf()]
        vst = [tile([4, 512], BF16), tile([4, 512], BF16)]; b_vst = [Buf(), Buf()]
        vi = 0
        for tt in range(NTT):
            s = tt % 2
            hT, bh, _, _ = norm_tile(n, xsrc, tt, i)
            load_cs(0, tt, cs, b_cs)
            for pre, st_, bst_, dst in (("Q", qst, b_qst, Qd), ("K", kst, b_kst, Kd)):
                pA, bA = proj(hT, bh, w_sb, b_w, G[pre + "X1"][0], 128)
                pB, bB = proj(hT, bh, w_sb, b_w, G[pre + "X2"][0], 128)
                rope(r, pA, bA, pB, bB, 128, cs[0], cs[1], b_cs, st_[s][:, 0, :], st_[s][:, 1, :], bst_[s])
                for c in range(6):
                    ps, pb = proj(hT, bh, w_sb, b_w, G[pre + "N%d" % c][0], 128)
                    evac(ps, pb, 128, st_[s][:, 2 + c, :], bst_[s], eng=("act" if c % 2 == 0 else "dve"))
                P.dma("sp", lambda e, s=s, tt=tt, st_=st_, dst=dst: e.dma_start(out=dst.rearrange("(c p) t -> p c t", p=128)[:, :, tsl(tt)], in_=st_[s]), reads=[bst_[s]])
            for c in range(2):
                ps, pb = proj(hT, bh, w_sb, b_w, G["QM%d" % c][0], 128)
                evac(ps, pb, 128, qmst[s][:, c, :], b_qmst[s], eng=("act" if c == 0 else "dve"))
            P.dma("sp", lambda e, s=s, tt=tt: e.dma_start(out=QMd.rearrange("(c p) t -> p c t", p=128)[:, :, tsl(tt)], in_=qmst[s]), reads=[b_qmst[s]])
            for half in range(2):
                v_, bv_ = vst[vi % 2], b_vst[vi % 2]
                vi += 1
                for tb_ in range(4):
                    ps, pb = proj_tok(hT, bh, w_sb, b_w, G["V"][0] + half * 512, 512, tb_)
                    evac(ps, pb, 128, v_[:, tb_, :], bv_, nn=512, eng=("act" if tb_ % 2 == 0 else "dve"))
                P.dma("sp", lambda e, v_=v_, tt=tt, half=half: e.dma_start(out=Vd[:, half * 512:(half + 1) * 512].rearrange("(n p) c -> p n c", p=128)[:, tt * 4:tt * 4 + 4, :], in_=v_), reads=[bv_])
        kb.phase_end(mark)

    def p2_c(i):
        mark = kb.off
        maskPD = tile([512], BF16); b_mPD = Buf()
        for j in range(4):
            P.op("dve", lambda e, j=j: e.tensor_copy(out=maskPD[:, j * 128:(j + 1) * 128], in_=maskb[:, (2 if j % 2 == 0 else 0), 0:128]), reads=[b_maskb], writes=[b_mPD], partial=True)
        QT = [tile([T], BF16), tile([T], BF16)]; b_QT = [Buf(), Buf()]
        KT = [tile([T], BF16), tile([T], BF16)]; b_KT = [Buf(), Buf()]
        VD = [tile([32, 128], BF16), tile([32, 128], BF16)]; b_VD = [Buf(), Buf()]
        Uacc = tile([T]); b_U = Buf()
        Lacc = tile([T]); b_L = Buf()
        Ob = [tile([T], BF16), tile([T], BF16)]; b_Ob = [Buf(), Buf()]
        NPT = 6
        PT = [tile([512], BF16) for _ in range(NPT)]; b_PT = [Buf() for _ in range(NPT)]
        pti = 0
        vdi = 0
        SC = float(128 ** -0.5)
        for h in range(8):
            s = h % 2
            P.dma("sp", lambda e, s=s, h=h: e.dma_start(out=QT[s], in_=Qd.rearrange("(d h) t -> d h t", h=8)[:, h, :]), writes=[b_QT[s]])
            P.dma("sp", lambda e, s=s, h=h: e.dma_start(out=KT[s], in_=Kd.rearrange("(d h) t -> d h t", h=8)[:, h, :]), writes=[b_KT[s]])
            for dl in (1, 4, 16):
                nblk = 32 // dl
                vd, bvd = VD[vdi % 2], b_VD[vdi % 2]
                vdi += 1
                vsrc = Vd[:, h * 128:(h + 1) * 128].rearrange("(n p r) c -> p r n c", p=128, r=dl)
                for r in range(dl):
                    P.dma("sp", lambda e, vd=vd, vsrc=vsrc, r=r, nblk=nblk: e.dma_start(out=vd[:, r * nblk:(r + 1) * nblk, :], in_=vsrc[:, r, :, :]), writes=[bvd], partial=(r > 0))
                gsz = min(4, nblk)
                for r in range(dl):
                    for nq in range(0, nblk, gsz):
                        slots = {}
                        for jb in range(0, gsz, 2):
                            ps, pb = kb.bank()

                            def f(e, ps=ps, jb=jb, nq=nq, r=r, dl=dl, s=s, gsz=gsz):
                                ins = e.matmul(ps, lhsT=ident, rhs=maskPD, start=True, stop=False)
                                for jj in range(2):
                                    nb = nq + jb + jj
                                    if jb + jj >= gsz:
                                        continue
                                    qsl = slice(r + dl * 128 * nb, r + dl * 128 * nb + dl * 127 + 1, dl)
                                    for w_, kbn in enumerate((nb - 1, nb)):
                                        if kbn < 0:
                                            continue
                                        ksl = slice(r + dl * 128 * kbn, r + dl * 128 * kbn + dl * 127 + 1, dl)
                                        c0 = (2 * jj + w_) * 128
                                        ins = e.matmul(ps[:, c0:c0 + 128], lhsT=KT[s][:, ksl], rhs=QT[s][:, qsl], start=False, stop=False, skip_group_check=True)
                                return ins
                            P.op("pe", f, reads=[b_ident, b_mPD, b_KT[s], b_QT[s]], writes=[pb])
                            pt, bpt = PT[pti % NPT], b_PT[pti % NPT]
                            pti += 1
                            P.op("act", lambda e, ps=ps, pt=pt: e.activation(out=pt, in_=ps, func=AF.Exp, scale=SC), reads=[pb], writes=[bpt])
                            for jj in range(2):
                                if jb + jj < gsz:
                                    slots[jb + jj] = (pt, bpt, jj)
                        psU, pbU = kb.bank()
                        psL, pbL = kb.bank()

                        def f(e, slots=slots, nq=nq, r=r, nblk=nblk, vd=vd, psU=psU, psL=psL, gsz=gsz):
                            for (acc, use_v) in ((psU, True), (psL, False)):
                                first = True
                                for j in range(gsz):
                                    pt, bpt, jj = slots[j]
                                    nb = nq + j
                                    for w_, kbn in enumerate((nb - 1, nb)):
                                        if kbn < 0:
                                            continue
                                        c0 = (2 * jj + w_) * 128
                                        lhs = vd[:, r * nblk + kbn, :] if use_v else ones_b
                                        ins = e.matmul(acc[:, j * 128:(j + 1) * 128], lhsT=lhs, rhs=pt[:, c0:c0 + 128], start=first, stop=False, skip_group_check=True)
                                        first = False
                            return ins
                        P.op("pe", f, reads=[v[1] for v in slots.values()] + [bvd, b_ones_b], writes=[pbU, pbL])
                        asl = slice(r + dl * 128 * nq, r + dl * 128 * nq + dl * (gsz * 128 - 1) + 1, dl)
                        W = gsz * 128
                        if dl == 1:
                            P.op("act", lambda e, psU=psU, asl=asl, W=W: e.activation(out=Uacc[:, asl], in_=psU[:, 0:W], func=AF.Copy), reads=[pbU], writes=[b_U], partial=True)
                            P.op("dve", lambda e, psL=psL, asl=asl, W=W: e.tensor_copy(out=Lacc[:, asl], in_=psL[:, 0:W]), reads=[pbL], writes=[b_L], partial=True)
                        else:
                            P.op("dve", lambda e, psU=psU, asl=asl, W=W: e.tensor_tensor(out=Uacc[:, asl], in0=Uacc[:, asl], in1=psU[:, 0:W], op=ALU.add), reads=[pbU, b_U], writes=[b_U], partial=True)
                            P.op("dve", lambda e, psL=psL, asl=asl, W=W: e.tensor_tensor(out=Lacc[:, asl], in0=Lacc[:, asl], in1=psL[:, 0:W], op=ALU.add), reads=[pbL, b_L], writes=[b_L], partial=True)
            recip(P, Lacc, Lacc, [b_L], [b_L])
            P.op("dve", lambda e, s=s: e.tensor_tensor(out=Ob[s], in0=Uacc, in1=Lacc, op=ALU.mult), reads=[b_U, b_L], writes=[b_Ob[s]])
            P.dma("sp", lambda e, s=s, h=h: e.dma_start(out=MIXd[h * 128:(h + 1) * 128, :], in_=Ob[s]), reads=[b_Ob[s]])
        kb.phase_end(mark)

    QLd = kb.dscr("qld", [2048, T])
    QId = kb.dscr("qid", [D, T])
    WId = kb.dscr("wid", [T, 16], F32)

    def p1_a(i, xsrc):
        mark = kb.off
        j = i // 3
        cols, G = PERMS[0]
        NC = len(cols)
        w_sb = tile([8, NC], BF16); b_w = Buf()
        load_w(w_in_d[i], 0, NC, w_sb, b_w)
        wuk_sb = tile([8, 256], BF16, parts=96); b_wuk = Buf()
        P.dma("pool", lambda e: e.dma_start(out=wuk_sb, in_=wuk_d[j].rearrange("h n r -> n h r")), writes=[b_wuk])
        kvn_sb = tile([256]); b_kvn = Buf()
        P.dma("sp", lambda e: e.dma_start(out=kvn_sb, in_=kvn_d[j].partition_broadcast(128)), writes=[b_kvn])
        n = norm_setup()
        r = rope_setup()
        cs = [[tile([TT]), tile([TT])] for _ in range(4)]; b_cs = [Buf() for _ in range(4)]
        qrst = [tile([2, TT], BF16), tile([2, TT], BF16)]; b_qrst = [Buf(), Buf()]
        qn_sb = [tile([TT], BF16, parts=96), tile([TT], BF16, parts=96)]; b_qn = [Buf(), Buf()]
        qlst = [tile([2, TT], BF16), tile([2, TT], BF16)]; b_qlst = [Buf(), Buf()]
        krst = [tile([2, TT], BF16, parts=16), tile([2, TT], BF16, parts=16)]; b_krst = [Buf(), Buf()]
        qist = [tile([8, TT], BF16), tile([8, TT], BF16)]; b_qist = [Buf(), Buf()]
        kist = [tile([3, TT], BF16, parts=48), tile([3, TT], BF16, parts=48)]; b_kist = [Buf(), Buf()]
        qmst = [tile([2, TT], BF16), tile([2, TT], BF16)]; b_qmst = [Buf(), Buf()]
        ckst = [tile([4, 256], BF16), tile([4, 256], BF16)]; b_ckst = [Buf(), Buf()]
        ckT = [tile([2, TT], BF16), tile([2, TT], BF16)]; b_ckT = [Buf(), Buf()]
        wist = [tile([4, 16]), tile([4, 16])]; b_wist = [Buf(), Buf()]
        junk = tile([256]); b_junk = Buf()
        ssq = tile([1]); b_ssq = Buf()
        qi = 0
        for tt in range(NTT):
            s = tt % 2
            hT, bh, _, _ = norm_tile(n, xsrc, tt, i)
            load_cs(0, tt, cs[0], b_cs[0])
            load_cs(1, tt, cs[1], b_cs[1], rows=16)
            load_cs(2, tt, cs[2], b_cs[2])
            load_cs(3, tt, cs[3], b_cs[3], rows=8)
            pA, bA = proj(hT, bh, w_sb, b_w, G["QX1"][0], 128)
            pB, bB = proj(hT, bh, w_sb, b_w, G["QX2"][0], 128)
            rope(r, pA, bA, pB, bB, 128, cs[0][0], cs[0][1], b_cs[0], qrst[s][:, 0, :], qrst[s][:, 1, :], b_qrst[s])
            P.dma("sp", lambda e, s=s, tt=tt: e.dma_start(out=Qd[0:256, :].rearrange("(c p) t -> p c t", p=128)[:, :, tsl(tt)], in_=qrst[s]), reads=[b_qrst[s]])
            for h in range(8):
                ps, pb = proj(hT, bh, w_sb, b_w, G["QN%d" % h][0], 96)
                qn, bqn = qn_sb[qi % 2], b_qn[qi % 2]
                ql, bql = qlst[qi % 2], b_qlst[qi % 2]
                qi += 1
                evac(ps, pb, 96, qn, bqn, partial=False, eng=("act" if h % 2 == 0 else "dve"))
                for rc in range(2):
                    ps2, pb2 = kb.bank()
                    P.op("pe", lambda e, ps2=ps2, h=h, rc=rc, qn=qn: e.matmul(ps2, lhsT=wuk_sb[:, h, rc * 128:(rc + 1) * 128], rhs=qn, start=True, stop=True), reads=[b_wuk, bqn], writes=[pb2])
                    evac(ps2, pb2, 128, ql[:, rc, :], bql, eng=("act" if rc == 0 else "dve"))
                P.dma("sp", lambda e, ql=ql, h=h, tt=tt: e.dma_start(out=QLd[h * 256:(h + 1) * 256, :].rearrange("(c p) t -> p c t", p=128)[:, :, tsl(tt)], in_=ql), reads=[bql])
            pA, bA = proj(hT, bh, w_sb, b_w, G["KR1"][0], 16)
            pB, bB = proj(hT, bh, w_sb, b_w, G["KR2"][0], 16)
            rope(r, pA, bA, pB, bB, 16, cs[1][0], cs[1][1], b_cs[1], krst[s][:, 0, :], krst[s][:, 1, :], b_krst[s])
            for c in range(2):
                P.dma("sp", lambda e, s=s, tt=tt, c=c: e.dma_start(out=Kd[16 * c:16 * c + 16, tsl(tt)], in_=krst[s][:, c, :]), reads=[b_krst[s]])
            pA, bA = proj(hT, bh, w_sb, b_w, G["QIX1"][0], 128)
            pB, bB = proj(hT, bh, w_sb, b_w, G["QIX2"][0], 128)
            rope(r, pA, bA, pB, bB, 128, cs[2][0], cs[2][1], b_cs[2], qist[s][:, 0, :], qist[s][:, 1, :], b_qist[s])
            for c in range(6):
                ps, pb = proj(hT, bh, w_sb, b_w, G["QIN%d" % c][0], 128)
                evac(ps, pb, 128, qist[s][:, 2 + c, :], b_qist[s], eng=("act" if c % 2 == 0 else "dve"))
            P.dma("sp", lambda e, s=s, tt=tt: e.dma_start(out=QId.rearrange("(c p) t -> p c t", p=128)[:, :, tsl(tt)], in_=qist[s]), reads=[b_qist[s]])
            pA, bA = proj(hT, bh, w_sb, b_w, G["KIX1"][0], 8)
            pB, bB = proj(hT, bh, w_sb, b_w, G["KIX2"][0], 8)
            rope(r, pA, bA, pB, bB, 8, cs[3][0], cs[3][1], b_cs[3], kist[s][0:8, 0, :], kist[s][0:8, 1, :], b_kist[s])
            ps, pb = proj(hT, bh, w_sb, b_w, G["KIN"][0], 48)
            evac(ps, pb, 48, kist[s][:, 2, :], b_kist[s])
            for (r0, nr, ci) in ((64, 8, 0), (72, 8, 1), (80, 48, 2)):
                P.dma("sp", lambda e, s=s, tt=tt, r0=r0, nr=nr, ci=ci: e.dma_start(out=Kd[r0:r0 + nr, tsl(tt)], in_=kist[s][0:nr, ci, :]), reads=[b_kist[s]])
            for c in range(2):
                ps, pb = proj(hT, bh, w_sb, b_w, G["QM%d" % c][0], 128)
                evac(ps, pb, 128, qmst[s][:, c, :], b_qmst[s], eng=("act" if c == 0 else "dve"))
            P.dma("sp", lambda e, s=s, tt=tt: e.dma_start(out=QMd.rearrange("(c p) t -> p c t", p=128)[:, :, tsl(tt)], in_=qmst[s]), reads=[b_qmst[s]])
            for tb_ in range(4):
                ps, pb = proj_tok(hT, bh, w_sb, b_w, G["CKV"][0], 256, tb_)
                P.op("act", lambda e, ps=ps: e.activation(out=junk, in_=ps[:, 0:256], func=AF.Square, accum_out=ssq[:, 0:1]), reads=[pb], writes=[b_junk, b_ssq])
                P.op("act", lambda e: e.activation(out=ssq, in_=ssq, func=AF.Sqrt, scale=1.0 / 256, bias=epsc[:, 0:1]), reads=[b_ssq, b_eps], writes=[b_ssq])
                recip(P, ssq, ssq, [b_ssq], [b_ssq])
                P.op("dve", lambda e, ps=ps, s=s, tb_=tb_: e.scalar_tensor_tensor(out=ckst[s][:, tb_, :], in0=ps[:, 0:256], scalar=ssq[:, 0:1], in1=kvn_sb, op0=ALU.mult, op1=ALU.mult),
                     reads=[pb, b_ssq, b_kvn], writes=[b_ckst[s]], partial=True)
            P.dma("sp", lambda e, s=s, tt=tt: e.dma_start(out=Vd[:, 0:256].rearrange("(n p) c -> p n c", p=128)[:, tt * 4:tt * 4 + 4, :], in_=ckst[s]), reads=[b_ckst[s]])
            for rc in range(2):
                ps, pb = kb.bank()
                psb = ps.bitcast(BF16)

                def f(e, psb=psb, rc=rc, s=s):
                    for tb_ in range(4):
                        ins = e.transpose(psb[:, tb_ * 128:(tb_ + 1) * 128], ckst[s][:, tb_, rc * 128:(rc + 1) * 128], ident)
                    return ins
                P.op("pe", f, reads=[b_ckst[s], b_ident], writes=[pb])
                P.op("act", lambda e, psb=psb, rc=rc, s=s: e.activation(out=ckT[s][:, rc, :], in_=psb[:, 0:512], func=AF.Copy), reads=[pb], writes=[b_ckT[s]], partial=True)
            P.dma("sp", lambda e, s=s, tt=tt: e.dma_start(out=Kd[256:512, :].rearrange("(c p) t -> p c t", p=128)[:, :, tsl(tt)], in_=ckT[s]), reads=[b_ckT[s]])
            for tb_ in range(4):
                ps, pb = proj_tok(hT, bh, w_sb, b_w, G["WI"][0], 16, tb_)
                P.op("act", lambda e, ps=ps, s=s, tb_=tb_: e.activation(out=wist[s][:, tb_, :], in_=ps[:, 0:16], func=AF.Copy, scale=1.0 / 32.0), reads=[pb], writes=[b_wist[s]], partial=True)
            P.dma("sp", lambda e, s=s, tt=tt: e.dma_start(out=WId.rearrange("(n p) c -> p n c", p=128)[:, tt * 4:tt * 4 + 4, :], in_=wist[s]), reads=[b_wist[s]])
        kb.phase_end(mark)

    def p2_a(i, NIT=12):
        mark = kb.off
        j = i // 3
        SC = float(128 ** -0.5)
        KI_sb = tile([T], BF16, parts=64); b_KI = Buf()
        P.dma("sp", lambda e: e.dma_start(out=KI_sb, in_=Kd[64:128, :]), writes=[b_KI])
        CKT_sb = tile([2, T], BF16); b_CKT = Buf()
        P.dma("sp", lambda e: e.dma_start(out=CKT_sb, in_=Kd[256:512, :].rearrange("(c p) t -> p c t", p=128)), writes=[b_CKT])
        CK_sb = tile([32, 256], BF16); b_CK = Buf()
        P.dma("sp", lambda e: e.dma_start(out=CK_sb, in_=Vd[:, 0:256].rearrange("(n p) c -> p n c", p=128)), writes=[b_CK])
        KR_sb = tile([T], BF16, parts=32); b_KR = Buf()
        P.dma("sp", lambda e: e.dma_start(out=KR_sb, in_=Kd[0:32, :]), writes=[b_KR])
        sel_sb = tile([16, 128], BF16); b_sel = Buf()
        P.dma("pool", lambda e: e.dma_start(out=sel_sb, in_=sel_d), writes=[b_sel])
        zmask = tile([128]); b_zm = Buf()
        P.dma("sp", lambda e: e.dma_start(out=zmask, in_=zmask_d), writes=[b_zm])
        bsel = tile([16]); b_bsel = Buf()
        P.dma("sp", lambda e: e.dma_start(out=bsel, in_=bsel_d), writes=[b_bsel])
        triq = tile([128]); b_triq = Buf()
        P.dma("sp", lambda e: e.dma_start(out=triq, in_=triq_d), writes=[b_triq])
        wuv_sb = tile([8, 2, 128], BF16); b_wuv = Buf()
        for h in range(8):
            P.dma("pool", lambda e, h=h: e.dma_start(out=wuv_sb[:, h, :, :], in_=wuv_d[j, h].rearrange("(c p) v -> p c v", p=128)), writes=[b_wuv], partial=True)
        id30k = tile([128], BF16); b_id30k = Buf()
        P.op("dve", lambda e: e.tensor_scalar(out=id30k, in0=ident, scalar1=-NEG, scalar2=None, op0=ALU.mult), reads=[b_ident], writes=[b_id30k])
        halfc = tile([1]); b_half = Buf()
        P.op("dve", lambda e: e.memset(halfc, 0.5), writes=[b_half])
        QI_sb = tile([16, TT], BF16, parts=64); b_QI = Buf()
        QI2 = tile([64, 128], BF16, parts=64); b_QI2 = Buf()
        QL_sb = tile([8, 2, TT], BF16); b_QL = Buf()
        QR_sb = tile([8, TT], BF16, parts=32); b_QR = Buf()
        WI_sb = tile([4, 16]); b_WI = Buf()
        S = tile([T]); b_S = Buf()
        mqb = tile([T], BF16); b_mqb = Buf()
        junk, b_junk = mqb, b_mqb
        MT = tile([32, TT], BF16); b_MT = Buf()
        NR = 6
        Rt = [tile([TT], BF16) for _ in range(NR)]; b_Rt = [Buf() for _ in range(NR)]
        NPT = 4
        PT = [tile([TT], BF16) for _ in range(NPT)]; b_PT = [Buf() for _ in range(NPT)]
        olat = tile([2, TT], BF16); b_olat = Buf()
        rL = tile([TT]); b_rL = Buf()
        mixst1 = tile([8, TT], BF16); b_mixst1 = Buf()
        mixst = [mixst1, mixst1]; b_mixst = [b_mixst1, b_mixst1]
        Z = tile([128]); b_Z = Buf()
        wcol = tile([16]); b_wcol = Buf()
        pw2 = tile([NIT]); b_pw2 = Buf()
        for it in range(NIT):
            P.op("dve", lambda e, it=it: e.memset(pw2[:, it:it + 1], float(2.0 ** -(it + 1))), writes=[b_pw2], partial=True)
        Wst = tile([NIT]); b_Wst = Buf()
        lo = tile([1]); b_lo = Buf()
        hi = tile([1]); b_hi = Buf()
        mid = tile([1]); b_mid = Buf()
        cnt = tile([1]); b_cnt = Buf()
        ge = tile([1]); b_ge = Buf()
        dd = tile([1]); b_dd = Buf()
        ri_ = [0]
        pi_ = [0]
        dbank = [0]

        def bankof(lst, ctr):
            k = lst[ctr[0] % len(lst)]
            ctr[0] += 1
            return kb.ps[k], kb.psb[k]
        sacc = [0]
        tbank = [0]
        for tt in range(NTT):
            P.dma("sp", lambda e, tt=tt: e.dma_start(out=QI_sb, in_=QId.rearrange("(d h) t -> d h t", h=16)[:, :, tsl(tt)]), writes=[b_QI])
            if "qi2" not in SKIP:
              P.op("pool", lambda e: e.tensor_copy(out=QI2.rearrange("d j (h q) -> d j h q", q=8), in_=QI_sb.rearrange("d h (j q) -> d j h q", q=8)), reads=[b_QI], writes=[b_QI2])
            for h in range(8):
                P.dma("sp", lambda e, tt=tt, h=h: e.dma_start(out=QL_sb[:, h, :, :], in_=QLd[h * 256:(h + 1) * 256, :].rearrange("(c p) t -> p c t", p=128)[:, :, tsl(tt)]), writes=[b_QL], partial=(h > 0))
            P.dma("sp", lambda e, tt=tt: e.dma_start(out=QR_sb, in_=Qd[0:256, :].rearrange("(i h) t -> i h t", h=8)[:, :, tsl(tt)]), writes=[b_QR])
            P.dma("sp", lambda e, tt=tt: e.dma_start(out=WI_sb, in_=WId.rearrange("(n p) c -> p n c", p=128)[:, tt * 4:tt * 4 + 4, :]), writes=[b_WI])
            P.op("dve", lambda e, tt=tt: e.memset(MT[:, 4 * tt:4 * tt + 4, :], -1.0), writes=[b_MT])
            for qb in range(4):
                if "idx" in SKIP:
                    continue
                nb = 4 * tt + qb
                nk = (nb + 1) * 128
                if "z" not in SKIP:
                    P.op("dve", lambda e, qb=qb: e.tensor_tensor(out=Z.rearrange("p (h q) -> p h q", q=8), in0=WI_sb[:, qb, :].unsqueeze(2).to_broadcast([128, 16, 8]),
                                                             in1=zmask.rearrange("p (h q) -> p h q", q=8), op=ALU.mult), reads=[b_WI, b_zm], writes=[b_Z])
                ps, pb = bankof([2, 3], tbank)
                if "wcol" not in SKIP:
                    P.op("pe", lambda e, ps=ps: e.matmul(ps[:, 0:16], lhsT=Z, rhs=bsel, start=True, stop=True), reads=[b_Z, b_bsel], writes=[pb])
                    P.op("act", lambda e, ps=ps: e.activation(out=wcol, in_=ps[:, 0:16], func=AF.Copy), reads=[pb], writes=[b_wcol])
                items = [(kc, jj) for kc in range(tt + 1) for jj in range(16)]
                pend = []
                pss_of = {}
                DEPTH = 0
                for idx in range(len(items) + DEPTH):
                    if idx < len(items):
                        kc, jj = items[idx]
                        ncol = 512 if kc < tt else (qb + 1) * 128
                        psd, pbd = bankof([4, 5, 6, 7], dbank)
                        P.op("pe", lambda e, psd=psd, jj=jj, qb=qb, kc=kc, ncol=ncol: e.matmul(psd[:, 0:ncol], lhsT=QI2[:, qb * 16 + jj, :],
                                                                                             rhs=KI_sb[:, kc * 512:kc * 512 + ncol], start=True, stop=True), reads=[b_QI2, b_KI], writes=[pbd])
                        rt, brt = Rt[ri_[0] % NR], b_Rt[ri_[0] % NR]
                        ri_[0] += 1
                        P.op("dve", lambda e, psd=psd, rt=rt, jj=jj, ncol=ncol: e.tensor_scalar(out=rt[:, 0:ncol], in0=psd[:, 0:ncol], scalar1=0.0, scalar2=wcol[:, jj:jj + 1], op0=ALU.max, op1=ALU.mult),
                             reads=[pbd, b_wcol], writes=[brt])
                        pend.append((kc, jj, ncol, rt, brt))
                    if idx >= DEPTH:
                        kc, jj, ncol, rt, brt = pend[idx - DEPTH]
                        if jj == 0:
                            pss_of[kc] = bankof([0, 1], sacc)
                        pss, pbs = pss_of[kc]
                        P.op("pe", lambda e, pss=pss, rt=rt, jj=jj, ncol=ncol: e.matmul(pss[:, 0:ncol], lhsT=sel_sb[:, jj, :], rhs=rt[:, 0:ncol], start=(jj == 0), stop=(jj == 15)),
                             reads=[brt, b_sel], writes=[pbs], partial=(jj > 0))
                        if jj == 15:
                            if kc < tt:
                                P.op("act", lambda e, pss=pss, kc=kc: e.activation(out=S[:, kc * 512:(kc + 1) * 512], in_=pss, func=AF.Copy), reads=[pbs], writes=[b_S], partial=True)
                            else:
                                if qb > 0:
                                    P.op("dve", lambda e, pss=pss, kc=kc, qb=qb: e.tensor_copy(out=S[:, kc * 512:kc * 512 + qb * 128], in_=pss[:, 0:qb * 128]), reads=[pbs], writes=[b_S], partial=True)
                                P.op("dve", lambda e, pss=pss, kc=kc, qb=qb: e.tensor_tensor(out=S[:, kc * 512 + qb * 128:kc * 512 + (qb + 1) * 128], in0=pss[:, qb * 128:(qb + 1) * 128], in1=triq, op=ALU.add),
                                     reads=[pbs, b_triq], writes=[b_S], partial=True)
                if nb >= 2 and "thr" not in SKIP:
                    P.op("dve", lambda e, nb=nb: e.tensor_reduce(out=lo, in_=S[:, 0:nb * 128], axis=AX.X, op=ALU.min), reads=[b_S], writes=[b_lo])
                    P.op("dve", lambda e, nk=nk: e.tensor_reduce(out=hi, in_=S[:, 0:nk], axis=AX.X, op=ALU.max), reads=[b_S], writes=[b_hi])
                    P.op("dve", lambda e: e.scalar_tensor_tensor(out=mid, in0=lo, scalar=hi[:, 0:1], in1=halfc, op0=ALU.add, op1=ALU.mult), reads=[b_lo, b_hi, b_half], writes=[b_mid])
                    P.op("dve", lambda e: e.tensor_tensor(out=dd, in0=hi, in1=lo, op=ALU.subtract), reads=[b_lo, b_hi], writes=[b_dd])
                    P.op("dve", lambda e: e.tensor_scalar(out=Wst, in0=pw2, scalar1=dd[:, 0:1], scalar2=None, op0=ALU.mult), reads=[b_dd, b_pw2], writes=[b_Wst])
                    for it in range(NIT):
                        P.op("dve", lambda e, nk=nk: e.tensor_scalar(out=junk[:, 0:nk], in0=S[:, 0:nk], scalar1=mid[:, 0:1], scalar2=None, op0=ALU.is_ge, op1=ALU.add, accum_out=cnt[:, 0:1]),
                             reads=[b_S, b_mid], writes=[b_junk, b_cnt])
                        P.op("dve", lambda e: e.tensor_scalar(out=ge, in0=cnt, scalar1=255.5, scalar2=0.5, op0=ALU.is_ge, op1=ALU.subtract), reads=[b_cnt], writes=[b_ge])
                        P.op("dve", lambda e, it=it: e.scalar_tensor_tensor(out=mid, in0=ge, scalar=Wst[:, it:it + 1], in1=mid, op0=ALU.mult, op1=ALU.add), reads=[b_ge, b_Wst, b_mid], writes=[b_mid])
                    P.op("dve", lambda e: e.scalar_tensor_tensor(out=lo, in0=Wst[:, NIT - 1:NIT], scalar=-0.5, in1=mid, op0=ALU.mult, op1=ALU.add), reads=[b_Wst, b_mid], writes=[b_lo])
                else:
                    P.op("dve", lambda e: e.memset(lo, -1.0e4), writes=[b_lo])
                P.op("dve", lambda e, nk=nk: e.tensor_scalar(out=mqb[:, 0:nk], in0=S[:, 0:nk], scalar1=lo[:, 0:1], scalar2=1.0, op0=ALU.is_ge, op1=ALU.subtract), reads=[b_S, b_lo], writes=[b_mqb])
                for kb0 in range(0, nb + 1, 4):
                    if "tr" in SKIP:
                        continue
                    gq = min(4, nb + 1 - kb0)
                    ps, pb = bankof([2, 3], tbank)
                    psb_ = ps.bitcast(BF16)

                    def f(e, psb_=psb_, kb0=kb0, gq=gq):
                        for t_i in range(gq):
                            ins = e.transpose(psb_[:, t_i * 128:(t_i + 1) * 128], mqb[:, (kb0 + t_i) * 128:(kb0 + t_i + 1) * 128], ident)
                        return ins
                    P.op("pe", f, reads=[b_mqb, b_ident], writes=[pb])
                    P.op("act", lambda e, psb_=psb_, kb0=kb0, gq=gq, qb=qb: e.activation(out=MT[:, kb0:kb0 + gq, qb * 128:(qb + 1) * 128], in_=psb_[:, 0:gq * 128].rearrange("p (g c) -> p g c", c=128), func=AF.Copy),
                         reads=[pb], writes=[b_MT], partial=True)
            nkb = 4 * (tt + 1)
            sm = tt % 2
            for h in range(8):
                if "att" in SKIP:
                    continue
                psO = [(kb.ps[0], kb.psb[0]), (kb.ps[1], kb.psb[1])]
                psL, pbL = kb.ps[2], kb.psb[2]
                pendp = []
                ADEPTH = 2
                for kidx in range(nkb + ADEPTH):
                    if kidx < nkb:
                        kbk = kidx
                        pss, pbs = bankof([4, 5, 6, 7], dbank)
                        ks = slice(kbk * 128, (kbk + 1) * 128)

                        def f(e, pss=pss, kbk=kbk, ks=ks, h=h):
                            e.matmul(pss, lhsT=id30k, rhs=MT[:, kbk, :], start=True, stop=False)
                            e.matmul(pss, lhsT=CKT_sb[:, 0, ks], rhs=QL_sb[:, h, 0, :], start=False, stop=False)
                            e.matmul(pss, lhsT=CKT_sb[:, 1, ks], rhs=QL_sb[:, h, 1, :], start=False, stop=False)
                            return e.matmul(pss, lhsT=KR_sb[:, ks], rhs=QR_sb[:, h, :], start=False, stop=True)
                        P.op("pe", f, reads=[b_id30k, b_MT, b_CKT, b_QL, b_KR, b_QR], writes=[pbs])
                        pt, bpt = PT[pi_[0] % NPT], b_PT[pi_[0] % NPT]
                        pi_[0] += 1
                        P.op("act", lambda e, pss=pss, pt=pt: e.activation(out=pt, in_=pss, func=AF.Exp, scale=SC), reads=[pbs], writes=[bpt])
                        pendp.append((kbk, pt, bpt))
                    if kidx >= ADEPTH:
                        kbk, pt, bpt = pendp[kidx - ADEPTH]

                        def f2(e, pt=pt, kbk=kbk, first=(kbk == 0), last=(kbk == nkb - 1)):
                            e.matmul(psO[0][0], lhsT=CK_sb[:, kbk, 0:128], rhs=pt, start=first, stop=last)
                            e.matmul(psO[1][0], lhsT=CK_sb[:, kbk, 128:256], rhs=pt, start=first, stop=last)
                            return e.matmul(psL, lhsT=ones_b, rhs=pt, start=first, stop=last)
                        P.op("pe", f2, reads=[bpt, b_CK, b_ones_b], writes=[psO[0][1], psO[1][1], pbL], partial=(kbk > 0))
                recip(P, rL, psL, [pbL], [b_rL])
                for rc in range(2):
                    P.op("dve", lambda e, rc=rc: e.tensor_tensor(out=olat[:, rc, :], in0=psO[rc][0], in1=rL, op=ALU.mult), reads=[psO[rc][1], b_rL], writes=[b_olat], partial=(rc > 0))
                pso, pbo = kb.ps[3], kb.psb[3]

                def f3(e, h=h, pso=pso):
                    e.matmul(pso, lhsT=wuv_sb[:, h, 0, :], rhs=olat[:, 0, :], start=True, stop=False)
                    return e.matmul(pso, lhsT=wuv_sb[:, h, 1, :], rhs=olat[:, 1, :], start=False, stop=True)
                P.op("pe", f3, reads=[b_wuv, b_olat], writes=[pbo])
                P.op("act", lambda e, pso=pso, h=h, sm=sm: e.activation(out=mixst[sm][:, h, :], in_=pso, func=AF.Copy), reads=[pbo], writes=[b_mixst[sm]], partial=True)
            P.dma("sp", lambda e, sm=sm, tt=tt: e.dma_start(out=MIXd.rearrange("(c p) t -> p c t", p=128)[:, :, tsl(tt)], in_=mixst[sm]), reads=[b_mixst[sm]])
        kb.phase_end(mark)

    def run_layers():
        xcur = xin
        for i in layers:
            kind = i % 3
            if kind == 1:
                p1_b(i, xcur)
                p2_b(i)
            elif kind == 2:
                p1_c(i, xcur)
                p2_c(i)
            else:
                p1_a(i, xcur)
                p2_a(i)
            phase_out(i, xcur, XS[0])
            phase_ffn(i, XS[0], XS[1])
            xcur = XS[1]
        if final:
            phase_final(xcur)
        else:
            mark = kb.off
            t_ = [tile([8, TT]), tile([8, TT])]; b_t = [Buf(), Buf()]
            for tt in range(NTT):
                s = tt % 2
                P.dma("sp", lambda e, s=s, tt=tt: e.dma_start(out=t_[s], in_=xcur.rearrange("(kc p) t -> p kc t", p=128)[:, :, tsl(tt)]), writes=[b_t[s]])
                P.dma("sp", lambda e, s=s, tt=tt: e.dma_start(out=out_d.rearrange("(kc p) t -> p kc t", p=128)[:, :, tsl(tt)], in_=t_[s]), reads=[b_t[s]])
            kb.phase_end(mark)
        if taps:
            mark = kb.off
            tp_ = [tile([8, TT], BF16), tile([8, TT], BF16)]; b_tp = [Buf(), Buf()]
            tapo = nc.dram_tensor("tapmix", [D, T], BF16, kind="ExternalOutput").ap()
            for tt in range(NTT):
                s = tt % 2
                P.dma("sp", lambda e, s=s, tt=tt: e.dma_start(out=tp_[s], in_=MIXd.rearrange("(kc p) t -> p kc t", p=128)[:, :, tsl(tt)]), writes=[b_tp[s]])
                P.dma("sp", lambda e, s=s, tt=tt: e.dma_start(out=tapo.rearrange("(kc p) t -> p kc t", p=128)[:, :, tsl(tt)], in_=tp_[s]), reads=[b_tp[s]])
            kb.phase_end(mark)
        P.barrier(engines=("sp",))

    kb_ctx = dict(locals())
    return kb, kb_ctx


def _host_inputs(inp, b, layers):
    f = np.float32
    m = {}
    m["xT"] = np.ascontiguousarray(inp["x"][b].T.astype(f))
    m["memT"] = np.ascontiguousarray(inp["mem"][b].T.astype(f))
    m["pos"] = np.ascontiguousarray(inp["positions"][b][None, :].astype(np.int32))
    gl = [inp["g_mix"][i] for i in range(4)] + [inp["g_ffn"][i] for i in range(4)] + [inp["g_mem"], inp["g_final"]]
    m["gvec"] = np.ascontiguousarray(np.stack([g.reshape(8, 128).T for g in gl], axis=1).astype(f))
    for k in ("invc", "cmask", "sel", "zmask", "bsel", "triq"):
        m[k] = CONSTS[k]
    for i in layers:
        kind, j = i % 3, i // 3
        w = (inp["a_w_in"], inp["b_w_in"], inp["c_w_in"])[kind][j]
        m["w_in%d" % i] = np.ascontiguousarray(w[:, PERMS[kind][0]].astype(f))
        m["w_out%d" % i] = np.ascontiguousarray((inp["a_w_out"], inp["b_w_out"], inp["c_w_out"])[kind][j].astype(f))
    m["w_up"] = np.ascontiguousarray(inp["f_w_up"].astype(f))
    m["w_down"] = np.ascontiguousarray(inp["f_w_down"].astype(f))
    m["convw"] = np.ascontiguousarray(inp["f_conv_w"].reshape(4, 3, 22, 128).transpose(0, 3, 2, 1).astype(f))
    m["convb"] = np.ascontiguousarray(inp["f_conv_b"].reshape(4, 22, 128).transpose(0, 2, 1).astype(f))
    m["wmem"] = np.ascontiguousarray(inp["w_mem_kv"].astype(f))
    sk = inp["b_sinks"][0]
    m["sinks"] = np.ascontiguousarray(np.stack([np.repeat(sk[2 * c:2 * c + 2], 64) for c in range(8)], axis=1).astype(f))
    m["kvn"] = np.ascontiguousarray(inp["a_kv_norm"][:, None, :].astype(f))
    m["wukT"] = np.ascontiguousarray(inp["a_w_uk"].transpose(0, 2, 3, 1).astype(f))
    m["wuv"] = np.ascontiguousarray(inp["a_w_uv"].transpose(0, 2, 1, 3).astype(f))
    return m


_CACHE = {}


def kernel(**inputs):
    inp = {k: np.asarray(v) for k, v in inputs.items()}
    layers = (0, 1, 2, 3)
    if "kb" not in _CACHE:
        kb, ctx = build_program(layers=layers, final=True)
        ctx["run_layers"]()
        kb.P.emit()
        _CACHE["kb"] = kb
    kb = _CACHE["kb"]
    n = 8
    maps = []
    for b in range(n):
        m = _host_inputs(inp, b, layers)
        maps.append({k: m[k] for k in kb.inputs})
    res = run_bass_kernel_spmd(kb.nc, maps, core_ids=list(range(n)))
    out = np.stack([np.ascontiguousarray(res.results[b]["outT"].T) for b in range(n)], axis=0)
    return out.astype(np.float32)
```

```python
import numpy as np
import concourse.bass as bass
import concourse.mybir as mybir
from concourse.bass_utils import run_bass_kernel_spmd
from contextlib import ExitStack

F32 = mybir.dt.float32
BF16 = mybir.dt.bfloat16
I32 = mybir.dt.int32
AF = mybir.ActivationFunctionType
ALU = mybir.AluOpType
AX = mybir.AxisListType

ENGS = ("pe", "act", "dve", "pool", "sp")
SKIP = set()
NDMA = 8
T = 4096
D = 1024
TT = 512
NTT = T // TT
NEG = -30000.0
EPS = 1e-6


class Buf:
    __slots__ = ("name", "base", "parts", "rd")

    def __init__(self, name=""):
        self.name = name
        self.base = []
        self.parts = []
        self.rd = {}


class Prog:
    def __init__(self, nc):
        self.nc = nc
        self.streams = {e: [] for e in ENGS}
        self.cnt = {e: 0 for e in ENGS}
        self.dma_i = {e: 0 for e in ENGS}
        self.dma_val = {}
        self.known = {e: {} for e in ENGS}
        self.last_tok = {e: None for e in ENGS}
        self.dma_latest = {}

    def _wait(self, eng, tok):
        semkey, val, _ = tok
        if self.known[eng].get(semkey, 0) >= val:
            return
        self.known[eng][semkey] = val
        self.streams[eng].append(("wait", semkey, val))

    def _deps(self, eng, reads, writes, is_dma, partial):
        deps = []
        for b in reads:
            deps.extend(b.base)
            deps.extend(b.parts)
        for b in writes:
            deps.extend(b.base)
            if not partial:
                deps.extend(b.parts)
            for e2, t in b.rd.items():
                if e2 == eng and not is_dma:
                    continue
                deps.append(t)
        return [t for t in deps if not (t[2] == "pe" and eng == "pe" and not is_dma)]

    def op(self, eng, fn, reads=(), writes=(), partial=False):
        for t in self._deps(eng, reads, writes, False, partial):
            self._wait(eng, t)
        self.cnt[eng] += 1
        tok = ("E" + eng, self.cnt[eng], eng)
        self.streams[eng].append(("op", fn, tok[0], 1, tok[1]))
        self._commit(eng, tok, reads, writes, partial)
        self.last_tok[eng] = tok
        return tok

    def dma(self, eng, fn, reads=(), writes=(), partial=False):
        i = self.dma_i[eng]
        self.dma_i[eng] += 1
        semkey = "D%s%d" % (eng, i % NDMA)
        prev = self.dma_val.get(semkey, 0)
        if prev:
            self._wait(eng, (semkey, prev, "dma"))
        for t in self._deps(eng, reads, writes, True, partial):
            self._wait(eng, t)
        val = prev + 16
        self.dma_val[semkey] = val
        tok = (semkey, val, "dma")
        self.streams[eng].append(("op", fn, semkey, 16, val))
        self._commit(semkey, tok, reads, writes, partial)
        self.dma_latest[semkey] = tok
        return tok

    def _commit(self, ekey, tok, reads, writes, partial):
        for b in reads:
            b.rd[ekey] = tok
        for b in writes:
            if partial:
                b.parts = [t for t in b.parts if t[0] != tok[0]] + [tok]
            else:
                b.base = [tok]
                b.parts = []
                b.rd = {}

    def barrier(self, engines=ENGS):
        toks = [t for t in self.last_tok.values() if t is not None] + list(self.dma_latest.values())
        for e in engines:
            for t in toks:
                if t[0] == "E" + e:
                    continue
                self._wait(e, t)

    def emit(self):
        nc = self.nc
        semkeys = set()
        waited = set()
        for e in ENGS:
            for it in self.streams[e]:
                if it[0] == "wait":
                    semkeys.add(it[1])
                    waited.add((it[1], it[2]))
        valmap = {}
        incs = {}
        for e in ENGS:
            c = 0
            for k, it in enumerate(self.streams[e]):
                if it[0] == "op" and it[3] == 1:
                    if (it[2], it[4]) in waited:
                        c += 1
                        valmap[(it[2], it[4])] = c
                        incs[(e, k)] = True
                elif it[0] == "op":
                    semkeys.add(it[2])
        with ExitStack() as st:
            sems = {k: st.enter_context(nc.semaphore(k)) for k in sorted(semkeys)}
            block = st.enter_context(nc.Block())

            def run(eobj, ename, items):
                for k, it in enumerate(items):
                    if it[0] == "wait":
                        v = valmap[(it[1], it[2])] if it[1].startswith("E") else it[2]
                        eobj.wait_ge(sems[it[1]], v)
                    elif it[3] == 16:
                        it[1](eobj).then_inc(sems[it[2]], 16)
                    else:
                        ins = it[1](eobj)
                        if (ename, k) in incs:
                            ins.then_inc(sems[it[2]], 1)

            @block.tensor
            def _(e):
                run(e, "pe", self.streams["pe"])

            @block.scalar
            def _(e):
                run(e, "act", self.streams["act"])

            @block.vector
            def _(e):
                run(e, "dve", self.streams["dve"])

            @block.gpsimd
            def _(e):
                run(e, "pool", self.streams["pool"])

            @block.sync
            def _(e):
                run(e, "sp", self.streams["sp"])


def _perm_a():
    cols, groups = [], {}

    def add(name, idx):
        groups[name] = (len(cols), len(idx))
        cols.extend(idx)
    add("QX1", [h * 128 + i for i in range(16) for h in range(8)])
    add("QX2", [h * 128 + 16 + i for i in range(16) for h in range(8)])
    for h in range(8):
        add("QN%d" % h, [h * 128 + 32 + n for n in range(96)])
    add("KR1", [1280 + i for i in range(16)])
    add("KR2", [1280 + 16 + i for i in range(16)])
    add("QIX1", [1312 + h * 64 + i for i in range(8) for h in range(16)])
    add("QIX2", [1312 + h * 64 + 8 + i for i in range(8) for h in range(16)])
    for c in range(6):
        add("QIN%d" % c, [1312 + h * 64 + d for d in range(16 + 8 * c, 24 + 8 * c) for h in range(16)])
    add("KIX1", [2336 + i for i in range(8)])
    add("KIX2", [2336 + 8 + i for i in range(8)])
    add("KIN", [2336 + 16 + i for i in range(48)])
    add("QM0", list(range(2416, 2544)))
    add("QM1", list(range(2544, 2672)))
    add("CKV", list(range(1024, 1280)))
    add("WI", list(range(2400, 2416)))
    return np.array(cols), groups


def _perm_b():
    cols, groups = [], {}

    def add(name, idx):
        groups[name] = (len(cols), len(idx))
        cols.extend(idx)
    add("QX1", [h * 64 + i for i in range(8) for h in range(16)])
    add("QX2", [h * 64 + 8 + i for i in range(8) for h in range(16)])
    for c in range(6):
        add("QN%d" % c, [h * 64 + d for d in range(16 + 8 * c, 24 + 8 * c) for h in range(16)])
    add("KX1", [1024 + h * 64 + i for i in range(8) for h in range(4)])
    add("KX2", [1024 + h * 64 + 8 + i for i in range(8) for h in range(4)])
    add("KN0", [1024 + h * 64 + d for d in range(16, 48) for h in range(4)])
    add("KN1", [1024 + h * 64 + d for d in range(48, 64) for h in range(4)])
    add("QM0", list(range(1536, 1664)))
    add("QM1", list(range(1664, 1792)))
    add("V", list(range(1280, 1536)))
    return np.array(cols), groups


def _perm_c():
    cols, groups = [], {}

    def add(name, idx):
        groups[name] = (len(cols), len(idx))
        cols.extend(idx)
    for pre, base in (("Q", 0), ("K", 1024)):
        add(pre + "X1", [base + h * 128 + i for i in range(16) for h in range(8)])
        add(pre + "X2", [base + h * 128 + 16 + i for i in range(16) for h in range(8)])
        for c in range(6):
            add(pre + "N%d" % c, [base + h * 128 + d for d in range(32 + 16 * c, 48 + 16 * c) for h in range(8)])
    add("QM0", list(range(3072, 3200)))
    add("QM1", list(range(3200, 3328)))
    add("V", list(range(2048, 3072)))
    return np.array(cols), groups


PERMS = {0: _perm_a(), 1: _perm_b(), 2: _perm_c()}


def _consts():
    c = {}
    inv32 = (500000.0 ** (-np.arange(0, 32, 2, dtype=np.float32) / 32)).astype(np.float32)
    inv16 = (500000.0 ** (-np.arange(0, 16, 2, dtype=np.float32) / 16)).astype(np.float32)
    invc = np.zeros((128, 5), np.float32)
    p = np.arange(128)
    invc[:, 0] = inv32[p // 8]
    invc[:, 1] = inv32[p % 16]
    invc[:, 2] = inv16[p // 16]
    invc[:, 3] = inv16[p % 8]
    invc[:, 4] = inv16[(p % 32) // 4]
    c["invc"] = invc
    k = np.arange(128)[:, None]
    q = np.arange(128)[None, :]
    m = np.zeros((128, 4, 128), np.float32)
    m[:, 0] = np.where(k <= q, 0.0, NEG)
    m[:, 1] = np.where(k > q, 0.0, NEG)
    m[:, 2] = np.where(k >= q, 0.0, NEG)
    m[:, 3] = np.eye(128, dtype=np.float32)
    c["cmask"] = m
    sel = np.zeros((128, 16, 128), np.float32)
    for j in range(16):
        for pp in range(128):
            sel[pp, j, 8 * j + pp % 8] = 1.0
    c["sel"] = sel
    zm = np.zeros((128, 128), np.float32)
    for mm in range(128):
        for pp in range(128):
            if mm % 8 == pp % 8:
                zm[mm, pp] = 1.0
    c["zmask"] = zm
    bs = np.zeros((128, 16), np.float32)
    for mm in range(128):
        bs[mm, mm // 8] = 1.0
    c["bsel"] = bs
    c["triq"] = np.where(q.T >= k.T, 0.0, NEG).astype(np.float32) if False else np.where(np.arange(128)[None, :] <= np.arange(128)[:, None], 0.0, NEG).astype(np.float32)
    return c


CONSTS = _consts()


class KB:
    def __init__(self, layers=(0, 1, 2, 3), final=True):
        self.layers = layers
        self.final = final
        nc = self.nc = bass.Bass("TRN2", target_bir_lowering=False)
        self.P = Prog(nc)
        self.inputs = {}
        self.arena = nc.alloc_sbuf_tensor("arena", [128, 206 * 1024 // 4], F32).ap()
        self.cap = 206 * 1024
        self.off = 0
        self.ps = [nc.alloc_psum_tensor("ps%d" % i, [128, 512], F32).ap() for i in range(8)]
        self.psb = [Buf("ps%d" % i) for i in range(8)]
        self.psi = 0

    def din(self, name, shape, dtype=F32):
        ap = self.nc.dram_tensor(name, list(shape), dtype, kind="ExternalInput").ap()
        self.inputs[name] = ap
        return ap

    def dscr(self, name, shape, dtype=BF16):
        return self.nc.dram_tensor(name, list(shape), dtype).ap()

    def tile(self, free, dtype=F32, parts=128):
        if isinstance(free, int):
            free = [free]
        n = int(np.prod(free))
        es = 2 if dtype == BF16 else 4
        nb = (n * es + 31) // 32 * 32
        off = self.off
        self.off += nb
        assert self.off <= self.cap, "SBUF arena overflow %d" % self.off
        v = self.arena[0:parts, off // 4:(off + nb) // 4]
        if dtype != F32:
            v = v.bitcast(dtype)
        v = v[:, 0:n]
        if len(free) == 2:
            v = v.rearrange("p (a b) -> p a b", b=free[1])
        elif len(free) == 3:
            v = v.rearrange("p (a b c) -> p a b c", b=free[1], c=free[2])
        return v

    def bank(self):
        i = self.psi
        self.psi = (self.psi + 1) % 8
        return self.ps[i], self.psb[i]

    def phase_end(self, mark):
        self.P.barrier()
        self.off = mark


def recip(P, out, in_, rbuf, wbuf):
    P.op("dve", lambda e: e.reciprocal(out=out, in_=in_), reads=rbuf, writes=wbuf)


def build_program(layers=(0, 1, 2, 3), final=True, taps=(), ffn=True):
    kb = KB(layers, final)
    nc, P = kb.nc, kb.P
    tile = kb.tile

    xin = kb.din("xT", [D, T])
    memT_d = kb.din("memT", [D, 256])
    pos_d = kb.din("pos", [1, T], I32)
    gv_d = kb.din("gvec", [128, 4 + 4 + 1 + 1, 8])
    invc_d = kb.din("invc", [128, 5])
    cmask_d = kb.din("cmask", [128, 4, 128])
    sel_d = kb.din("sel", [128, 16, 128])
    zmask_d = kb.din("zmask", [128, 128])
    bsel_d = kb.din("bsel", [128, 16])
    triq_d = kb.din("triq", [128, 128])
    w_in_d, w_out_d = {}, {}
    for i in layers:
        w_in_d[i] = kb.din("w_in%d" % i, [D, len(PERMS[i % 3][0])])
        w_out_d[i] = kb.din("w_out%d" % i, [1280, D])
    w_up_d = kb.din("w_up", [4, D, 5632] if ffn else [4, 128, 8])
    w_down_d = kb.din("w_down", [4, 2816, D] if ffn else [4, 128, 8])
    convw_d = kb.din("convw", [4, 128, 22, 3])
    convb_d = kb.din("convb", [4, 128, 22])
    wmem_d = kb.din("wmem", [4, D, 512])
    sinks_d = kb.din("sinks", [128, 8])
    kvn_d = kb.din("kvn", [2, 1, 256])
    wuk_d = kb.din("wukT", [2, 8, 96, 256])
    wuv_d = kb.din("wuv", [2, 8, 256, 128])
    out_d = nc.dram_tensor("outT", [D, T], F32, kind="ExternalOutput").ap()

    XS = [kb.dscr("xs0", [D, T], F32), kb.dscr("xs1", [D, T], F32)]
    MIXd = kb.dscr("mixd", [D, T])
    QMd = kb.dscr("qmd", [256, T])
    Qd = kb.dscr("qd", [D, T])
    Kd = kb.dscr("kd", [D, T])
    Vd = kb.dscr("vd", [T, D])
    ROPE = [(kb.dscr("cos%d" % k, [128, T], F32), kb.dscr("sin%d" % k, [128, T], F32)) for k in range(5)]

    def tsl(tt):
        return slice(tt * TT, (tt + 1) * TT)

    ones_f = tile([128]); b_ones_f = Buf()
    ident = tile([128], BF16); b_ident = Buf()
    ones_b = tile([128], BF16); b_ones_b = Buf()
    onz = tile([192], BF16); b_onz = Buf()
    maskb = tile([3, 512], BF16); b_maskb = Buf()
    gv = tile([10, 8]); b_gv = Buf()
    memn = tile([8, 256], BF16); b_memn = Buf()
    invc = tile([5]); b_invc = Buf()
    epsc = tile([1]); b_eps = Buf()
    P.op("dve", lambda e: e.memset(ones_f, 1.0), writes=[b_ones_f])
    P.op("dve", lambda e: e.memset(epsc, EPS), writes=[b_eps])
    P.op("dve", lambda e: e.memset(ones_b, 1.0), writes=[b_ones_b])
    P.op("dve", lambda e: e.memset(onz, 0.0), writes=[b_onz])
    P.op("dve", lambda e: e.memset(onz[:, 64:128], 1.0), writes=[b_onz])
    P.dma("sp", lambda e: e.dma_start(out=gv, in_=gv_d), writes=[b_gv])
    P.dma("sp", lambda e: e.dma_start(out=invc, in_=invc_d), writes=[b_invc])
    mark0 = kb.off
    cm = tile([4, 128]); b_cm = Buf()
    P.dma("sp", lambda e: e.dma_start(out=cm, in_=cmask_d), writes=[b_cm])
    P.op("dve", lambda e: e.tensor_copy(out=ident, in_=cm[:, 3, :]), reads=[b_cm], writes=[b_ident])
    for k in range(3):
        for r in range(4):
            P.op("dve", lambda e, k=k, r=r: e.tensor_copy(out=maskb[:, k, r * 128:(r + 1) * 128], in_=cm[:, k, :]),
                 reads=[b_cm], writes=[b_maskb], partial=True)

    posi = tile([T], I32); b_posi = Buf()
    posf = tile([T]); b_posf = Buf()
    P.dma("sp", lambda e: e.dma_start(out=posi, in_=pos_d.partition_broadcast(128)), writes=[b_posi])
    P.op("dve", lambda e: e.tensor_copy(out=posf, in_=posi), reads=[b_posi], writes=[b_posf])
    HALF = 2048
    ang = tile([HALF]); b_ang = Buf()
    yk = tile([HALF]); b_yk = Buf()
    ki = tile([HALF], I32); b_ki = Buf()
    rr = tile([HALF]); b_rr = Buf()
    mm_ = tile([HALF]); b_mm = Buf()
    tb = [tile([HALF]), tile([HALF])]; b_tb = [Buf(), Buf()]
    TWO_PI = float(2 * np.pi)
    tbi = 0
    for k in range(5):
        for half in range(2):
            hs = slice(half * HALF, (half + 1) * HALF)
            for which in range(2):
                shift = float(np.pi / 2) if which == 0 else 0.0
                P.op("dve", lambda e, k=k, hs=hs, shift=shift: e.tensor_scalar(out=ang, in0=posf[:, hs], scalar1=invc[:, k:k + 1], scalar2=shift, op0=ALU.mult, op1=ALU.add),
                     reads=[b_posf, b_invc], writes=[b_ang])
                P.op("dve", lambda e: e.tensor_scalar(out=yk, in0=ang, scalar1=1.0 / TWO_PI, scalar2=None, op0=ALU.mult), reads=[b_ang], writes=[b_yk])
                P.op("dve", lambda e: e.tensor_copy(out=ki, in_=yk), reads=[b_yk], writes=[b_ki])
                P.op("dve", lambda e: e.tensor_copy(out=yk, in_=ki), reads=[b_ki], writes=[b_yk])
                P.op("dve", lambda e: e.scalar_tensor_tensor(out=rr, in0=yk, scalar=-TWO_PI, in1=ang, op0=ALU.mult, op1=ALU.add), reads=[b_yk, b_ang], writes=[b_rr])
                P.op("dve", lambda e: e.tensor_scalar(out=mm_, in0=rr, scalar1=float(np.pi), scalar2=-TWO_PI, op0=ALU.is_gt, op1=ALU.mult), reads=[b_rr], writes=[b_mm])
                P.op("dve", lambda e: e.tensor_tensor(out=rr, in0=rr, in1=mm_, op=ALU.add), reads=[b_rr, b_mm], writes=[b_rr])
                P.op("dve", lambda e: e.tensor_scalar(out=mm_, in0=rr, scalar1=-float(np.pi), scalar2=TWO_PI, op0=ALU.is_lt, op1=ALU.mult), reads=[b_rr], writes=[b_mm])
                P.op("dve", lambda e: e.tensor_tensor(out=rr, in0=rr, in1=mm_, op=ALU.add), reads=[b_rr, b_mm], writes=[b_rr])
                P.op("dve", lambda e: e.tensor_scalar(out=rr, in0=rr, scalar1=3.14159, scalar2=-3.14159, op0=ALU.min, op1=ALU.max), reads=[b_rr], writes=[b_rr])
                t_, bt_ = tb[tbi % 2], b_tb[tbi % 2]
                tbi += 1
                P.op("act", lambda e, t_=t_: e.activation(out=t_, in_=rr, func=AF.Sin), reads=[b_rr], writes=[bt_])
                dst = ROPE[k][which]
                P.dma("sp", lambda e, t_=t_, dst=dst, hs=hs: e.dma_start(out=dst[:, hs], in_=t_), reads=[bt_])

    mt = tile([8, 256]); b_mt = Buf()
    msq = tile([8, 256]); b_msq = Buf()
    mrs = tile([256]); b_mrs = Buf()
    P.dma("sp", lambda e: e.dma_start(out=mt, in_=memT_d.rearrange("(kc p) m -> p kc m", p=128)), writes=[b_mt])
    P.op("act", lambda e: e.activation(out=msq, in_=mt, func=AF.Square), reads=[b_mt], writes=[b_msq])
    ps, pb = kb.bank()

    def f_(e, ps=ps):
        for kc in range(8):
            ins = e.matmul(ps[:, 0:256], lhsT=ones_f, rhs=msq[:, kc, :], start=(kc == 0), stop=(kc == 7))
        return ins
    P.op("pe", f_, reads=[b_msq, b_ones_f], writes=[pb])
    P.op("act", lambda e, ps=ps: e.activation(out=mrs, in_=ps[:, 0:256], func=AF.Sqrt, scale=1.0 / D, bias=epsc[:, 0:1]), reads=[pb, b_eps], writes=[b_mrs])
    recip(P, mrs, mrs, [b_mrs], [b_mrs])
    for kc in range(8):
        P.op("dve", lambda e, kc=kc: e.scalar_tensor_tensor(out=memn[:, kc, :], in0=mt[:, kc, :], scalar=gv[:, 8, kc:kc + 1], in1=mrs, op0=ALU.mult, op1=ALU.mult),
             reads=[b_mt, b_gv, b_mrs], writes=[b_memn], partial=True)
    kb.phase_end(mark0)
    PERSIST = kb.off

    def load_w(w_d, c0, ncols, dst, b_dst, kcs=8):
        src = w_d.rearrange("(kc p) m -> p kc m", p=128)
        c = 0
        while c < ncols:
            n = min(512, ncols - c)
            P.dma("pool", lambda e, c=c, n=n: e.dma_start(out=dst[:, :, c:c + n], in_=src[:, :, c0 + c:c0 + c + n]), writes=[b_dst], partial=True)
            c += n

    class NormCtx:
        pass

    def norm_setup():
        n = NormCtx()
        n.xt = [tile([8, TT]), tile([8, TT])]; n.b_xt = [Buf(), Buf()]
        n.sq = tile([8, TT]); n.b_sq = Buf()
        n.rs = tile([TT]); n.b_rs = Buf()
        n.hT = [tile([8, TT], BF16), tile([8, TT], BF16)]; n.b_hT = [Buf(), Buf()]
        return n

    def norm_tile(n, xsrc, tt, gidx, want_x=False):
        s = tt % 2
        xt, bx = n.xt[s], n.b_xt[s]
        P.dma("sp", lambda e: e.dma_start(out=xt, in_=xsrc.rearrange("(kc p) t -> p kc t", p=128)[:, :, tsl(tt)]), writes=[bx])
        P.op("act", lambda e: e.activation(out=n.sq, in_=xt, func=AF.Square), reads=[bx], writes=[n.b_sq])
        ps, pb = kb.bank()

        def f(e):
            for kc in range(8):
                ins = e.matmul(ps, lhsT=ones_f, rhs=n.sq[:, kc, :], start=(kc == 0), stop=(kc == 7))
            return ins
        P.op("pe", f, reads=[n.b_sq, b_ones_f], writes=[pb])
        P.op("act", lambda e: e.activation(out=n.rs, in_=ps, func=AF.Sqrt, scale=1.0 / D, bias=epsc[:, 0:1]), reads=[pb, b_eps], writes=[n.b_rs])
        recip(P, n.rs, n.rs, [n.b_rs], [n.b_rs])
        hT, bh = n.hT[s], n.b_hT[s]
        for kc in range(8):
            P.op("dve", lambda e, kc=kc: e.scalar_tensor_tensor(out=hT[:, kc, :], in0=xt[:, kc, :], scalar=gv[:, gidx, kc:kc + 1], in1=n.rs, op0=ALU.mult, op1=ALU.mult),
                 reads=[bx, b_gv, n.b_rs], writes=[bh], partial=(kc > 0))
        return hT, bh, xt, bx

    def proj(hT, bh, w_sb, b_w, c0, M, n0=0, nn=TT):
        ps, pb = kb.bank()

        def f(e):
            for kc in range(8):
                ins = e.matmul(ps[0:M, 0:nn], lhsT=w_sb[:, kc, c0:c0 + M], rhs=hT[:, kc, n0:n0 + nn], start=(kc == 0), stop=(kc == 7))
            return ins
        P.op("pe", f, reads=[bh, b_w], writes=[pb])
        return ps, pb

    def proj_tok(hT, bh, w_sb, b_w, c0, N, tb):
        ps, pb = kb.bank()

        def f(e):
            for kc in range(8):
                ins = e.matmul(ps[:, 0:N], lhsT=hT[:, kc, tb * 128:(tb + 1) * 128], rhs=w_sb[:, kc, c0:c0 + N], start=(kc == 0), stop=(kc == 7))
            return ins
        P.op("pe", f, reads=[bh, b_w], writes=[pb])
        return ps, pb

    class RopeCtx:
        pass

    def rope_setup():
        r = RopeCtx()
        r.t1 = tile([TT]); r.b1 = Buf()
        r.t2 = tile([TT]); r.b2 = Buf()
        return r

    def rope(r, psA, pbA, psB, pbB, M, cos, sin, b_cs, o1, o2, b_o, partial=True):
        A, B = psA[0:M, :], psB[0:M, :]
        C, S_ = cos[0:M, :], sin[0:M, :]
        t1, t2 = r.t1[0:M, :], r.t2[0:M, :]
        P.op("dve", lambda e: e.tensor_tensor(out=t1, in0=A, in1=C, op=ALU.mult), reads=[pbA, b_cs], writes=[r.b1])
        P.op("dve", lambda e: e.tensor_tensor(out=t2, in0=B, in1=S_, op=ALU.mult), reads=[pbB, b_cs], writes=[r.b2])
        P.op("dve", lambda e: e.tensor_tensor(out=o1, in0=t1, in1=t2, op=ALU.subtract), reads=[r.b1, r.b2], writes=[b_o], partial=partial)
        P.op("dve", lambda e: e.tensor_tensor(out=t1, in0=B, in1=C, op=ALU.mult), reads=[pbB, b_cs], writes=[r.b1])
        P.op("dve", lambda e: e.tensor_tensor(out=t2, in0=A, in1=S_, op=ALU.mult), reads=[pbA, b_cs], writes=[r.b2])
        P.op("dve", lambda e: e.tensor_tensor(out=o2, in0=t1, in1=t2, op=ALU.add), reads=[r.b1, r.b2], writes=[b_o], partial=partial)

    def load_cs(k, tt, cs, b_cs, rows=128):
        P.dma("sp", lambda e: e.dma_start(out=cs[0][0:rows, :], in_=ROPE[k][0][0:rows, tsl(tt)]), writes=[b_cs])
        P.dma("sp", lambda e: e.dma_start(out=cs[1][0:rows, :], in_=ROPE[k][1][0:rows, tsl(tt)]), writes=[b_cs], partial=True)

    def evac(ps, pb, M, out, b_out, partial=True, eng="act", nn=TT):
        if eng == "act":
            P.op("act", lambda e: e.activation(out=out, in_=ps[0:M, 0:nn], func=AF.Copy), reads=[pb], writes=[b_out], partial=partial)
        else:
            P.op("dve", lambda e: e.tensor_copy(out=out, in_=ps[0:M, 0:nn]), reads=[pb], writes=[b_out], partial=partial)

    def phase_out(i, xsrc, xdst):
        mark = kb.off
        wm_sb = tile([8, 512], BF16); b_wm = Buf()
        load_w(wmem_d[i], 0, 512, wm_sb, b_wm)
        wo_sb = tile([10, D], BF16); b_wo = Buf()
        load_w(w_out_d[i], 0, D, wo_sb, b_wo)
        kTm = tile([2, 256], BF16); b_kTm = Buf()
        VM = tile([2, 4, 192], BF16); b_VM = Buf()
        P.op("dve", lambda e: e.memset(VM, 0.0), writes=[b_VM])
        for cc in range(2):
            ps, pb = kb.bank()

            def f(e, ps=ps, cc=cc):
                for kc in range(8):
                    ins = e.matmul(ps[:, 0:256], lhsT=wm_sb[:, kc, cc * 128:(cc + 1) * 128], rhs=memn[:, kc, :], start=(kc == 0), stop=(kc == 7))
                return ins
            P.op("pe", f, reads=[b_wm, b_memn], writes=[pb])
            evac(ps, pb, 128, kTm[:, cc, :], b_kTm, nn=256)
        for mb in range(2):
            ps, pb = kb.bank()

            def f(e, ps=ps, mb=mb):
                for kc in range(8):
                    ins = e.matmul(ps[:, 0:256], lhsT=memn[:, kc, mb * 128:(mb + 1) * 128], rhs=wm_sb[:, kc, 256:512], start=(kc == 0), stop=(kc == 7))
                return ins
            P.op("pe", f, reads=[b_wm, b_memn], writes=[pb])
            P.op("act", lambda e, ps=ps, mb=mb: e.activation(out=VM[:, mb, :, 64:128], in_=ps[:, 0:256].rearrange("p (h c) -> p h c", c=64), func=AF.Copy),
                 reads=[pb], writes=[b_VM], partial=True)
        qm = [tile([2, TT], BF16), tile([2, TT], BF16)]; b_qm = [Buf(), Buf()]
        mix = [tile([8, TT], BF16), tile([8, TT], BF16)]; b_mix = [Buf(), Buf()]
        xt = [tile([8, TT]), tile([8, TT])]; b_xt = [Buf(), Buf()]
        memo = tile([2, TT], BF16); b_memo = Buf()
        PT = [tile([TT], BF16) for _ in range(4)]; b_PT = [Buf() for _ in range(4)]
        rL = tile([TT]); b_rL = Buf()
        pti = 0
        for tt in range(NTT):
            s = tt % 2
            P.dma("sp", lambda e, s=s, tt=tt: e.dma_start(out=qm[s], in_=QMd.rearrange("(c p) t -> p c t", p=128)[:, :, tsl(tt)]), writes=[b_qm[s]])
            P.dma("sp", lambda e, s=s, tt=tt: e.dma_start(out=mix[s], in_=MIXd.rearrange("(c p) t -> p c t", p=128)[:, :, tsl(tt)]), writes=[b_mix[s]])
            P.dma("sp", lambda e, s=s, tt=tt: e.dma_start(out=xt[s], in_=xsrc.rearrange("(c p) t -> p c t", p=128)[:, :, tsl(tt)]), writes=[b_xt[s]])
            for cc in range(2):
                psU, pbU = kb.bank()
                psL, pbL = kb.bank()
                first = True
                for hh in range(2):
                    head = 2 * cc + hh
                    rs_ = slice(64 * hh, 64 * hh + 64)
                    vs_ = slice(64, 192) if hh == 0 else slice(0, 128)
                    for mb in range(2):
                        ps, pb = kb.bank()
                        P.op("pe", lambda e, ps=ps, rs_=rs_, cc=cc, mb=mb, s=s: e.matmul(ps, lhsT=kTm[rs_, cc, mb * 128:(mb + 1) * 128], rhs=qm[s][rs_, cc, :], start=True, stop=True),
                             reads=[b_kTm, b_qm[s]], writes=[pb])
                        pt, bpt = PT[pti % 4], b_PT[pti % 4]
                        pti += 1
                        P.op("act", lambda e, ps=ps, pt=pt: e.activation(out=pt, in_=ps, func=AF.Exp, scale=0.125), reads=[pb], writes=[bpt])
                        last = (hh == 1 and mb == 1)

                        def f(e, pt=pt, mb=mb, head=head, vs_=vs_, first=first, last=last, psU=psU, psL=psL):
                            e.matmul(psU, lhsT=VM[:, mb, head, vs_], rhs=pt, start=first, stop=last, skip_group_check=True)
                            return e.matmul(psL, lhsT=onz[:, vs_], rhs=pt, start=first, stop=last, skip_group_check=True)
                        P.op("pe", f, reads=[bpt, b_VM, b_onz], writes=[pbU, pbL], partial=not first)
                        first = False
                recip(P, rL, psL, [pbL], [b_rL])
                P.op("dve", lambda e, psU=psU, cc=cc: e.tensor_tensor(out=memo[:, cc, :], in0=psU, in1=rL, op=ALU.mult), reads=[pbU, b_rL], writes=[b_memo], partial=(cc > 0))
            for m in range(8):
                ps, pb = kb.bank()

                def f(e, ps=ps, m=m, s=s):
                    for c in range(8):
                        e.matmul(ps, lhsT=wo_sb[:, c, m * 128:(m + 1) * 128], rhs=mix[s][:, c, :], start=(c == 0), stop=False)
                    e.matmul(ps, lhsT=wo_sb[:, 8, m * 128:(m + 1) * 128], rhs=memo[:, 0, :], start=False, stop=False)
                    return e.matmul(ps, lhsT=wo_sb[:, 9, m * 128:(m + 1) * 128], rhs=memo[:, 1, :], start=False, stop=True)
                P.op("pe", f, reads=[b_wo, b_mix[s], b_memo], writes=[pb])
                P.op("dve", lambda e, ps=ps, m=m, s=s: e.tensor_tensor(out=xt[s][:, m, :], in0=xt[s][:, m, :], in1=ps, op=ALU.add), reads=[pb, b_xt[s]], writes=[b_xt[s]], partial=True)
            P.dma("sp", lambda e, s=s, tt=tt: e.dma_start(out=xdst.rearrange("(c p) t -> p c t", p=128)[:, :, tsl(tt)], in_=xt[s]), reads=[b_xt[s]])
        kb.phase_end(mark)

    def phase_ffn(i, xsrc, xdst):
        mark = kb.off
        ST = 2048
        NS = ST // TT
        hA = tile([8, ST], BF16); b_hA = [Buf() for _ in range(NS)]
        g = tile([22, ST], BF16); b_g = [Buf() for _ in range(NS)]
        xt = tile([8, TT]); b_xt = Buf()
        sq = tile([8, TT], BF16); b_sq = Buf()
        rs = tile([TT]); b_rs = Buf()
        wa = [tile([8, 256], BF16), tile([8, 256], BF16)]; b_wa = [Buf(), Buf()]
        wb = [tile([8, 256], BF16), tile([8, 256], BF16)]; b_wb = [Buf(), Buf()]
        wdn = [tile([22, 128], BF16), tile([22, 128], BF16)]; b_wdn = [Buf(), Buf()]
        cw = tile([22, 3]); b_cw = Buf()
        cb = tile([22]); b_cb = Buf()
        halo = tile([22, 2]); b_halo = Buf()
        a_sb = [tile([TT + 2]), tile([TT + 2])]; b_a = [Buf(), Buf()]
        cv = [tile([TT]), tile([TT])]; b_cv = [Buf(), Buf()]
        sg = [tile([TT]), tile([TT])]; b_sg = [Buf(), Buf()]
        xr = [tile([TT]), tile([TT])]; b_xr = [Buf(), Buf()]
        P.dma("sp", lambda e: e.dma_start(out=cw, in_=convw_d[i]), writes=[b_cw])
        P.dma("sp", lambda e: e.dma_start(out=cb, in_=convb_d[i]), writes=[b_cb])
        P.op("dve", lambda e: e.memset(halo, 0.0), writes=[b_halo])
        wupr = w_up_d[i].rearrange("(kc p) m -> p kc m", p=128)
        wdnr = w_down_d[i].rearrange("(c p) m -> p c m", p=128)
        gi = 4 + i
        ai = 0
        for st in range(T // ST):
            for tl in range(NS):
                tt = st * NS + tl
                P.dma("sp", lambda e, tt=tt: e.dma_start(out=xt, in_=xsrc.rearrange("(kc p) t -> p kc t", p=128)[:, :, tsl(tt)]), writes=[b_xt])
                P.op("act", lambda e: e.activation(out=sq, in_=xt, func=AF.Square), reads=[b_xt], writes=[b_sq])
                ps, pb = kb.bank()

                def f(e, ps=ps):
                    for kc in range(8):
                        ins = e.matmul(ps, lhsT=ones_b, rhs=sq[:, kc, :], start=(kc == 0), stop=(kc == 7))
                    return ins
                P.op("pe", f, reads=[b_sq, b_ones_b], writes=[pb])
                P.op("act", lambda e, ps=ps: e.activation(out=rs, in_=ps, func=AF.Sqrt, scale=1.0 / D, bias=epsc[:, 0:1]), reads=[pb, b_eps], writes=[b_rs])
                recip(P, rs, rs, [b_rs], [b_rs])
                for kc in range(8):
                    P.op("dve", lambda e, kc=kc, tl=tl: e.scalar_tensor_tensor(out=hA[:, kc, tl * TT:(tl + 1) * TT], in0=xt[:, kc, :], scalar=gv[:, gi, kc:kc + 1], in1=rs, op0=ALU.mult, op1=ALU.mult),
                         reads=[b_xt, b_gv, b_rs], writes=[b_hA[tl]], partial=(kc > 0))
            for pc in range(11):
                s = pc % 2
                P.dma("pool", lambda e, s=s, pc=pc: e.dma_start(out=wa[s], in_=wupr[:, :, pc * 256:(pc + 1) * 256]), writes=[b_wa[s]])
                P.dma("pool", lambda e, s=s, pc=pc: e.dma_start(out=wb[s], in_=wupr[:, :, 2816 + pc * 256:2816 + (pc + 1) * 256]), writes=[b_wb[s]])
                for cl in range(2):
                    c = pc * 2 + cl
                    for tl in range(NS):
                        tt = st * NS + tl
                        psa, pba = kb.bank()
                        psb, pbb = kb.bank()

                        def f(e, psa=psa, psb=psb, s=s, cl=cl, tl=tl):
                            for kc in range(8):
                                e.matmul(psa, lhsT=wa[s][:, kc, cl * 128:(cl + 1) * 128], rhs=hA[:, kc, tl * TT:(tl + 1) * TT], start=(kc == 0), stop=(kc == 7))
                            for kc in range(8):
                                ins = e.matmul(psb, lhsT=wb[s][:, kc, cl * 128:(cl + 1) * 128], rhs=hA[:, kc, tl * TT:(tl + 1) * TT], start=(kc == 0), stop=(kc == 7))
                            return ins
                        P.op("pe", f, reads=[b_wa[s], b_wb[s], b_hA[tl]], writes=[pba, pbb])
                        a_, ba_ = a_sb[ai % 2], b_a[ai % 2]
                        cv_, bcv_ = cv[ai % 2], b_cv[ai % 2]
                        sg_, bsg_ = sg[ai % 2], b_sg[ai % 2]
                        ai += 1
                        P.op("act", lambda e, a_=a_, psa=psa: e.activation(out=a_[:, 2:TT + 2], in_=psa, func=AF.Copy), reads=[pba], writes=[ba_])
                        P.op("dve", lambda e, a_=a_, c=c: e.tensor_copy(out=a_[:, 0:2], in_=halo[:, c, :]), reads=[b_halo], writes=[ba_], partial=True)
                        P.op("dve", lambda e, a_=a_, cv_=cv_, c=c: e.tensor_scalar(out=cv_, in0=a_[:, 2:TT + 2], scalar1=cw[:, c, 2:3], scalar2=cb[:, c:c + 1], op0=ALU.mult, op1=ALU.add),
                             reads=[ba_, b_cw, b_cb], writes=[bcv_])
                        P.op("dve", lambda e, a_=a_, cv_=cv_, c=c: e.scalar_tensor_tensor(out=cv_, in0=a_[:, 1:TT + 1], scalar=cw[:, c, 1:2], in1=cv_, op0=ALU.mult, op1=ALU.add),
                             reads=[ba_, b_cw, bcv_], writes=[bcv_])
                        P.op("dve", lambda e, a_=a_, cv_=cv_, c=c: e.scalar_tensor_tensor(out=cv_, in0=a_[:, 0:TT], scalar=cw[:, c, 0:1], in1=cv_, op0=ALU.mult, op1=ALU.add),
                             reads=[ba_, b_cw, bcv_], writes=[bcv_])
                        P.op("dve", lambda e, a_=a_, c=c: e.tensor_copy(out=halo[:, c, :], in_=a_[:, TT:TT + 2]), reads=[ba_], writes=[b_halo], partial=True)
                        P.op("act", lambda e, cv_=cv_, sg_=sg_: e.activation(out=sg_, in_=cv_, func=AF.Silu), reads=[bcv_], writes=[bsg_])
                        P.op("dve", lambda e, sg_=sg_, psb=psb, c=c, tl=tl: e.tensor_tensor(out=g[:, c, tl * TT:(tl + 1) * TT], in0=sg_, in1=psb, op=ALU.mult),
                             reads=[bsg_, pbb], writes=[b_g[tl]], partial=True)
            xi = 0
            for m in range(8):
                s = m % 2
                P.dma("pool", lambda e, s=s, m=m: e.dma_start(out=wdn[s], in_=wdnr[:, :, m * 128:(m + 1) * 128]), writes=[b_wdn[s]])
                for tl in range(NS):
                    tt = st * NS + tl
                    xr_, bxr_ = xr[xi % 2], b_xr[xi % 2]
                    xi += 1
                    P.dma("sp", lambda e, xr_=xr_, m=m, tt=tt: e.dma_start(out=xr_, in_=xsrc[m * 128:(m + 1) * 128, tsl(tt)]), writes=[bxr_])
                    ps, pb = kb.bank()

                    def f(e, ps=ps, s=s, tl=tl):
                        for c in range(22):
                            ins = e.matmul(ps, lhsT=wdn[s][:, c, :], rhs=g[:, c, tl * TT:(tl + 1) * TT], start=(c == 0), stop=(c == 21))
                        return ins
                    P.op("pe", f, reads=[b_wdn[s], b_g[tl]], writes=[pb])
                    P.op("dve", lambda e, xr_=xr_, ps=ps: e.tensor_tensor(out=xr_, in0=xr_, in1=ps, op=ALU.add), reads=[pb, bxr_], writes=[bxr_])
                    P.dma("sp", lambda e, xr_=xr_, m=m, tt=tt: e.dma_start(out=xdst[m * 128:(m + 1) * 128, tsl(tt)], in_=xr_), reads=[bxr_])
        kb.phase_end(mark)

    def phase_final(xsrc):
        mark = kb.off
        n = norm_setup()
        ot = [tile([8, TT]), tile([8, TT])]; b_ot = [Buf(), Buf()]
        for tt in range(NTT):
            s = tt % 2
            xt, bx = n.xt[s], n.b_xt[s]
            P.dma("sp", lambda e, xt=xt, tt=tt: e.dma_start(out=xt, in_=xsrc.rearrange("(kc p) t -> p kc t", p=128)[:, :, tsl(tt)]), writes=[bx])
            P.op("act", lambda e, xt=xt: e.activation(out=n.sq, in_=xt, func=AF.Square), reads=[bx], writes=[n.b_sq])
            ps, pb = kb.bank()

            def f(e, ps=ps):
                for kc in range(8):
                    ins = e.matmul(ps, lhsT=ones_f, rhs=n.sq[:, kc, :], start=(kc == 0), stop=(kc == 7))
                return ins
            P.op("pe", f, reads=[n.b_sq, b_ones_f], writes=[pb])
            P.op("act", lambda e, ps=ps: e.activation(out=n.rs, in_=ps, func=AF.Sqrt, scale=1.0 / D, bias=epsc[:, 0:1]), reads=[pb, b_eps], writes=[n.b_rs])
            recip(P, n.rs, n.rs, [n.b_rs], [n.b_rs])
            for kc in range(8):
                P.op("dve", lambda e, kc=kc, xt=xt, s=s: e.scalar_tensor_tensor(out=ot[s][:, kc, :], in0=xt[:, kc, :], scalar=gv[:, 9, kc:kc + 1], in1=n.rs, op0=ALU.mult, op1=ALU.mult),
                     reads=[bx, b_gv, n.b_rs], writes=[b_ot[s]], partial=(kc > 0))
            P.dma("sp", lambda e, s=s, tt=tt: e.dma_start(out=out_d.rearrange("(kc p) t -> p kc t", p=128)[:, :, tsl(tt)], in_=ot[s]), reads=[b_ot[s]])
        kb.phase_end(mark)

    def p1_b(i, xsrc):
        mark = kb.off
        cols, G = PERMS[1]
        NC = len(cols)
        w_sb = tile([8, NC], BF16); b_w = Buf()
        load_w(w_in_d[i], 0, NC, w_sb, b_w)
        n = norm_setup()
        r = rope_setup()
        csq = [tile([TT]), tile([TT])]; b_csq = Buf()
        csk = [tile([TT]), tile([TT])]; b_csk = Buf()
        qst = [tile([8, TT], BF16), tile([8, TT], BF16)]; b_qst = [Buf(), Buf()]
        kst = [tile([4, TT], BF16), tile([4, TT], BF16)]; b_kst = [Buf(), Buf()]
        qmst = [tile([2, TT], BF16), tile([2, TT], BF16)]; b_qmst = [Buf(), Buf()]
        vst = [tile([4, 256], BF16), tile([4, 256], BF16)]; b_vst = [Buf(), Buf()]
        for tt in range(NTT):
            s = tt % 2
            hT, bh, _, _ = norm_tile(n, xsrc, tt, i)
            load_cs(2, tt, csq, b_csq)
            load_cs(4, tt, csk, b_csk, rows=32)
            pA, bA = proj(hT, bh, w_sb, b_w, G["QX1"][0], 128)
            pB, bB = proj(hT, bh, w_sb, b_w, G["QX2"][0], 128)
            rope(r, pA, bA, pB, bB, 128, csq[0], csq[1], b_csq, qst[s][:, 0, :], qst[s][:, 1, :], b_qst[s])
            for c in range(6):
                ps, pb = proj(hT, bh, w_sb, b_w, G["QN%d" % c][0], 128)
                evac(ps, pb, 128, qst[s][:, 2 + c, :], b_qst[s], eng=("act" if c % 2 == 0 else "dve"))
            P.dma("sp", lambda e, s=s, tt=tt: e.dma_start(out=Qd.rearrange("(c p) t -> p c t", p=128)[:, :, tsl(tt)], in_=qst[s]), reads=[b_qst[s]])
            pA, bA = proj(hT, bh, w_sb, b_w, G["KX1"][0], 32)
            pB, bB = proj(hT, bh, w_sb, b_w, G["KX2"][0], 32)
            rope(r, pA, bA, pB, bB, 32, csk[0], csk[1], b_csk, kst[s][0:32, 0, :], kst[s][0:32, 1, :], b_kst[s])
            ps, pb = proj(hT, bh, w_sb, b_w, G["KN0"][0], 128)
            evac(ps, pb, 128, kst[s][:, 2, :], b_kst[s])
            ps, pb = proj(hT, bh, w_sb, b_w, G["KN1"][0], 64)
            evac(ps, pb, 64, kst[s][0:64, 3, :], b_kst[s], eng="dve")
            for (r0, nr, ci) in ((0, 32, 0), (32, 32, 1), (64, 128, 2), (192, 64, 3)):
                P.dma("sp", lambda e, s=s, tt=tt, r0=r0, nr=nr, ci=ci: e.dma_start(out=Kd[r0:r0 + nr, tsl(tt)], in_=kst[s][0:nr, ci, :]), reads=[b_kst[s]])
            for c in range(2):
                ps, pb = proj(hT, bh, w_sb, b_w, G["QM%d" % c][0], 128)
                evac(ps, pb, 128, qmst[s][:, c, :], b_qmst[s], eng=("act" if c == 0 else "dve"))
            P.dma("sp", lambda e, s=s, tt=tt: e.dma_start(out=QMd.rearrange("(c p) t -> p c t", p=128)[:, :, tsl(tt)], in_=qmst[s]), reads=[b_qmst[s]])
            for tb_ in range(4):
                ps, pb = proj_tok(hT, bh, w_sb, b_w, G["V"][0], 256, tb_)
                evac(ps, pb, 128, vst[s][:, tb_, :], b_vst[s], nn=256, eng=("act" if tb_ % 2 == 0 else "dve"))
            P.dma("sp", lambda e, s=s, tt=tt: e.dma_start(out=Vd[:, 0:256].rearrange("(n p) c -> p n c", p=128)[:, tt * 4:tt * 4 + 4, :], in_=vst[s]), reads=[b_vst[s]])
        kb.phase_end(mark)

    def p2_b(i):
        mark = kb.off
        K_sb = tile([4, T], BF16, parts=64); b_K = Buf()
        P.dma("sp", lambda e: e.dma_start(out=K_sb, in_=Kd[0:256, :].rearrange("(d h) t -> d h t", h=4)), writes=[b_K])
        VZ = tile([32, 4, 192], BF16); b_VZ = Buf()
        P.op("dve", lambda e: e.memset(VZ, 0.0), writes=[b_VZ])
        for g in range(4):
            P.dma("sp", lambda e, g=g: e.dma_start(out=VZ[:, :, g, 64:128], in_=Vd[:, g * 64:(g + 1) * 64].rearrange("(n p) c -> p n c", p=128)), writes=[b_VZ], partial=True)
        esk = tile([8]); b_esk = Buf()
        P.dma("sp", lambda e: e.dma_start(out=esk, in_=sinks_d), writes=[b_esk])
        P.op("act", lambda e: e.activation(out=esk, in_=esk, func=AF.Exp), reads=[b_esk], writes=[b_esk])
        Q_sb = [tile([16, TT], BF16, parts=64), tile([16, TT], BF16, parts=64)]; b_Q = [Buf(), Buf()]
        mixst = [tile([8, TT], BF16), tile([8, TT], BF16)]; b_mixst = [Buf(), Buf()]
        NPT = 10
        PT = [tile([TT], BF16) for _ in range(NPT)]; b_PT = [Buf() for _ in range(NPT)]
        Ls = tile([TT]); b_Ls = Buf()
        pti = 0
        for tt in range(NTT):
            s = tt % 2
            P.dma("sp", lambda e, s=s, tt=tt: e.dma_start(out=Q_sb[s], in_=Qd.rearrange("(d h) t -> d h t", h=16)[:, :, tsl(tt)]), writes=[b_Q[s]])
            for g in range(4):
                pts = {}
                for qb in range(4):
                    nb = tt * 4 + qb
                    for w_, kb_ in enumerate((nb - 1, nb)):
                        if kb_ < 0:
                            continue
                        kind = 1 if w_ == 0 else 0
                        ps, pb = kb.bank()

                        def f(e, ps=ps, kind=kind, g=g, kb_=kb_, qb=qb, s=s):
                            e.matmul(ps, lhsT=ident, rhs=maskb[:, kind, :], start=True, stop=False)
                            return e.matmul(ps, lhsT=K_sb[:, g, kb_ * 128:(kb_ + 1) * 128], rhs=Q_sb[s][:, 4 * g:4 * g + 4, qb * 128:(qb + 1) * 128], start=False, stop=True)
                        P.op("pe", f, reads=[b_ident, b_maskb, b_K, b_Q[s]], writes=[pb])
                        pt, bpt = PT[pti % NPT], b_PT[pti % NPT]
                        pti += 1
                        P.op("act", lambda e, ps=ps, pt=pt: e.activation(out=pt, in_=ps, func=AF.Exp, scale=0.125), reads=[pb], writes=[bpt])
                        pts[(qb, w_)] = (pt, bpt, kb_)
                for cc in range(2):
                    psU, pbU = kb.bank()
                    psL, pbL = kb.bank()
                    items = sorted(pts.items())

                    def f(e, items=items, cc=cc, g=g, psU=psU, psL=psL):
                        first = True
                        for (qb, w_), (pt, bpt, kb_) in items:
                            qs = slice(qb * 128, (qb + 1) * 128)
                            for hh in range(2):
                                hq = 2 * cc + hh
                                vs_ = slice(64, 192) if hh == 0 else slice(0, 128)
                                e.matmul(psU[:, qs], lhsT=VZ[:, kb_, g, vs_], rhs=pt[:, hq * 128:(hq + 1) * 128], start=first, stop=False, skip_group_check=True)
                                first = False
                        first = True
                        for (qb, w_), (pt, bpt, kb_) in items:
                            qs = slice(qb * 128, (qb + 1) * 128)
                            for hh in range(2):
                                hq = 2 * cc + hh
                                vs_ = slice(64, 192) if hh == 0 else slice(0, 128)
                                ins = e.matmul(psL[:, qs], lhsT=onz[:, vs_], rhs=pt[:, hq * 128:(hq + 1) * 128], start=first, stop=False, skip_group_check=True)
                                first = False
                        return ins
                    P.op("pe", f, reads=[v[1] for v in pts.values()] + [b_VZ, b_onz], writes=[pbU, pbL])
                    ch = 2 * g + cc
                    P.op("dve", lambda e, psL=psL, ch=ch: e.tensor_scalar(out=Ls, in0=psL, scalar1=esk[:, ch:ch + 1], scalar2=None, op0=ALU.add), reads=[pbL, b_esk], writes=[b_Ls])
                    recip(P, Ls, Ls, [b_Ls], [b_Ls])
                    P.op("dve", lambda e, psU=psU, ch=ch, s=s: e.tensor_tensor(out=mixst[s][:, ch, :], in0=psU, in1=Ls, op=ALU.mult), reads=[pbU, b_Ls], writes=[b_mixst[s]], partial=True)
            P.dma("sp", lambda e, s=s, tt=tt: e.dma_start(out=MIXd.rearrange("(c p) t -> p c t", p=128)[:, :, tsl(tt)], in_=mixst[s]), reads=[b_mixst[s]])
        kb.phase_end(mark)

    def p1_c(i, xsrc):
        mark = kb.off
        cols, G = PERMS[2]
        NC = len(cols)
        w_sb = tile([8, NC], BF16); b_w = Buf()
        load_w(w_in_d[i], 0, NC, w_sb, b_w)
        n = norm_setup()
        r = rope_setup()
        cs = [tile([TT]), tile([TT])]; b_cs = Buf()
        qst = [tile([8, TT], BF16), tile([8, TT], BF16)]; b_qst = [Buf(), Buf()]
        kst = [tile([8, TT], BF16), tile([8, TT], BF16)]; b_kst = [Buf(), Buf()]
        qmst = [tile([2, TT], BF16), tile([2, TT], BF16)]; b_qmst = [Buf(), Buf()]
        vst = [tile([4, 512], BF16), tile([4, 512], BF16)]; b_vst = [Buf(), Buf()]
        vi = 0
        for tt in range(NTT):
            s = tt % 2
            hT, bh, _, _ = norm_tile(n, xsrc, tt, i)
            load_cs(0, tt, cs, b_cs)
            for pre, st_, bst_, dst in (("Q", qst, b_qst, Qd), ("K", kst, b_kst, Kd)):
                pA, bA = proj(hT, bh, w_sb, b_w, G[pre + "X1"][0], 128)
                pB, bB = proj(hT, bh, w_sb, b_w, G[pre + "X2"][0], 128)
                rope(r, pA, bA, pB, bB, 128, cs[0], cs[1], b_cs, st_[s][:, 0, :], st_[s][:, 1, :], bst_[s])
                for c in range(6):
                    ps, pb = proj(hT, bh, w_sb, b_w, G[pre + "N%d" % c][0], 128)
                    evac(ps, pb, 128, st_[s][:, 2 + c, :], bst_[s], eng=("act" if c % 2 == 0 else "dve"))
                P.dma("sp", lambda e, s=s, tt=tt, st_=st_, dst=dst: e.dma_start(out=dst.rearrange("(c p) t -> p c t", p=128)[:, :, tsl(tt)], in_=st_[s]), reads=[bst_[s]])
            for c in range(2):
                ps, pb = proj(hT, bh, w_sb, b_w, G["QM%d" % c][0], 128)
                evac(ps, pb, 128, qmst[s][:, c, :], b_qmst[s], eng=("act" if c == 0 else "dve"))
            P.dma("sp", lambda e, s=s, tt=tt: e.dma_start(out=QMd.rearrange("(c p) t -> p c t", p=128)[:, :, tsl(tt)], in_=qmst[s]), reads=[b_qmst[s]])
            for half in range(2):
                v_, bv_ = vst[vi % 2], b_vst[vi % 2]
                vi += 1
                for tb_ in range(4):
                    ps, pb = proj_tok(hT, bh, w_sb, b_w, G["V"][0] + half * 512, 512, tb_)
                    evac(ps, pb, 128, v_[:, tb_, :], bv_, nn=512, eng=("act" if tb_ % 2 == 0 else "dve"))
                P.dma("sp", lambda e, v_=v_, tt=tt, half=half: e.dma_start(out=Vd[:, half * 512:(half + 1) * 512].rearrange("(n p) c -> p n c", p=128)[:, tt * 4:tt * 4 + 4, :], in_=v_), reads=[bv_])
        kb.phase_end(mark)

    def p2_c(i):
        mark = kb.off
        maskPD = tile([512], BF16); b_mPD = Buf()
        for j in range(4):
            P.op("dve", lambda e, j=j: e.tensor_copy(out=maskPD[:, j * 128:(j + 1) * 128], in_=maskb[:, (2 if j % 2 == 0 else 0), 0:128]), reads=[b_maskb], writes=[b_mPD], partial=True)
        QT = [tile([T], BF16), tile([T], BF16)]; b_QT = [Buf(), Buf()]
        KT = [tile([T], BF16), tile([T], BF16)]; b_KT = [Buf(), Buf()]
        VD = [tile([32, 128], BF16), tile([32, 128], BF16)]; b_VD = [Buf(), Buf()]
        Uacc = tile([T]); b_U = Buf()
        Lacc = tile([T]); b_L = Buf()
        Ob = [tile([T], BF16), tile([T], BF16)]; b_Ob = [Buf(), Buf()]
        NPT = 6
        PT = [tile([512], BF16) for _ in range(NPT)]; b_PT = [Buf() for _ in range(NPT)]
        pti = 0
        vdi = 0
        SC = float(128 ** -0.5)
        for h in range(8):
            s = h % 2
            P.dma("sp", lambda e, s=s, h=h: e.dma_start(out=QT[s], in_=Qd.rearrange("(d h) t -> d h t", h=8)[:, h, :]), writes=[b_QT[s]])
            P.dma("sp", lambda e, s=s, h=h: e.dma_start(out=KT[s], in_=Kd.rearrange("(d h) t -> d h t", h=8)[:, h, :]), writes=[b_KT[s]])
            for dl in (1, 4, 16):
                nblk = 32 // dl
                vd, bvd = VD[vdi % 2], b_VD[vdi % 2]
                vdi += 1
                vsrc = Vd[:, h * 128:(h + 1) * 128].rearrange("(n p r) c -> p r n c", p=128, r=dl)
                for r in range(dl):
                    P.dma("sp", lambda e, vd=vd, vsrc=vsrc, r=r, nblk=nblk: e.dma_start(out=vd[:, r * nblk:(r + 1) * nblk, :], in_=vsrc[:, r, :, :]), writes=[bvd], partial=(r > 0))
                gsz = min(4, nblk)
                for r in range(dl):
                    for nq in range(0, nblk, gsz):
                        slots = {}
                        for jb in range(0, gsz, 2):
                            ps, pb = kb.bank()

                            def f(e, ps=ps, jb=jb, nq=nq, r=r, dl=dl, s=s, gsz=gsz):
                                ins = e.matmul(ps, lhsT=ident, rhs=maskPD, start=True, stop=False)
                                for jj in range(2):
                                    nb = nq + jb + jj
                                    if jb + jj >= gsz:
                                        continue
                                    qsl = slice(r + dl * 128 * nb, r + dl * 128 * nb + dl * 127 + 1, dl)
                                    for w_, kbn in enumerate((nb - 1, nb)):
                                        if kbn < 0:
                                            continue
                                        ksl = slice(r + dl * 128 * kbn, r + dl * 128 * kbn + dl * 127 + 1, dl)
                                        c0 = (2 * jj + w_) * 128
                                        ins = e.matmul(ps[:, c0:c0 + 128], lhsT=KT[s][:, ksl], rhs=QT[s][:, qsl], start=False, stop=False, skip_group_check=True)
                                return ins
                            P.op("pe", f, reads=[b_ident, b_mPD, b_KT[s], b_QT[s]], writes=[pb])
                            pt, bpt = PT[pti % NPT], b_PT[pti % NPT]
                            pti += 1
                            P.op("act", lambda e, ps=ps, pt=pt: e.activation(out=pt, in_=ps, func=AF.Exp, scale=SC), reads=[pb], writes=[bpt])
                            for jj in range(2):
                                if jb + jj < gsz:
                                    slots[jb + jj] = (pt, bpt, jj)
                        psU, pbU = kb.bank()
                        psL, pbL = kb.bank()

                        def f(e, slots=slots, nq=nq, r=r, nblk=nblk, vd=vd, psU=psU, psL=psL, gsz=gsz):
                            for (acc, use_v) in ((psU, True), (psL, False)):
                                first = True
                                for j in range(gsz):
                                    pt, bpt, jj = slots[j]
                                    nb = nq + j
                                    for w_, kbn in enumerate((nb - 1, nb)):
                                        if kbn < 0:
                                            continue
                                        c0 = (2 * jj + w_) * 128
                                        lhs = vd[:, r * nblk + kbn, :] if use_v else ones_b
                                        ins = e.matmul(acc[:, j * 128:(j + 1) * 128], lhsT=lhs, rhs=pt[:, c0:c0 + 128], start=first, stop=False, skip_group_check=True)
                                        first = False
                            return ins
                        P.op("pe", f, reads=[v[1] for v in slots.values()] + [bvd, b_ones_b], writes=[pbU, pbL])
                        asl = slice(r + dl * 128 * nq, r + dl * 128 * nq + dl * (gsz * 128 - 1) + 1, dl)
                        W = gsz * 128
                        if dl == 1:
                            P.op("act", lambda e, psU=psU, asl=asl, W=W: e.activation(out=Uacc[:, asl], in_=psU[:, 0:W], func=AF.Copy), reads=[pbU], writes=[b_U], partial=True)
                            P.op("dve", lambda e, psL=psL, asl=asl, W=W: e.tensor_copy(out=Lacc[:, asl], in_=psL[:, 0:W]), reads=[pbL], writes=[b_L], partial=True)
                        else:
                            P.op("dve", lambda e, psU=psU, asl=asl, W=W: e.tensor_tensor(out=Uacc[:, asl], in0=Uacc[:, asl], in1=psU[:, 0:W], op=ALU.add), reads=[pbU, b_U], writes=[b_U], partial=True)
                            P.op("dve", lambda e, psL=psL, asl=asl, W=W: e.tensor_tensor(out=Lacc[:, asl], in0=Lacc[:, asl], in1=psL[:, 0:W], op=ALU.add), reads=[pbL, b_L], writes=[b_L], partial=True)
            recip(P, Lacc, Lacc, [b_L], [b_L])
            P.op("dve", lambda e, s=s: e.tensor_tensor(out=Ob[s], in0=Uacc, in1=Lacc, op=ALU.mult), reads=[b_U, b_L], writes=[b_Ob[s]])
            P.dma("sp", lambda e, s=s, h=h: e.dma_start(out=MIXd[h * 128:(h + 1) * 128, :], in_=Ob[s]), reads=[b_Ob[s]])
        kb.phase_end(mark)

    QLd = kb.dscr("qld", [2048, T])
    QId = kb.dscr("qid", [D, T])
    WId = kb.dscr("wid", [T, 16], F32)

    def p1_a(i, xsrc):
        mark = kb.off
        j = i // 3
        cols, G = PERMS[0]
        NC = len(cols)
        w_sb = tile([8, NC], BF16); b_w = Buf()
        load_w(w_in_d[i], 0, NC, w_sb, b_w)
        wuk_sb = tile([8, 256], BF16, parts=96); b_wuk = Buf()
        P.dma("pool", lambda e: e.dma_start(out=wuk_sb, in_=wuk_d[j].rearrange("h n r -> n h r")), writes=[b_wuk])
        kvn_sb = tile([256]); b_kvn = Buf()
        P.dma("sp", lambda e: e.dma_start(out=kvn_sb, in_=kvn_d[j].partition_broadcast(128)), writes=[b_kvn])
        n = norm_setup()
        r = rope_setup()
        cs = [[tile([TT]), tile([TT])] for _ in range(4)]; b_cs = [Buf() for _ in range(4)]
        qrst = [tile([2, TT], BF16), tile([2, TT], BF16)]; b_qrst = [Buf(), Buf()]
        qn_sb = [tile([TT], BF16, parts=96), tile([TT], BF16, parts=96)]; b_qn = [Buf(), Buf()]
        qlst = [tile([2, TT], BF16), tile([2, TT], BF16)]; b_qlst = [Buf(), Buf()]
        krst = [tile([2, TT], BF16, parts=16), tile([2, TT], BF16, parts=16)]; b_krst = [Buf(), Buf()]
        qist = [tile([8, TT], BF16), tile([8, TT], BF16)]; b_qist = [Buf(), Buf()]
        kist = [tile([3, TT], BF16, parts=48), tile([3, TT], BF16, parts=48)]; b_kist = [Buf(), Buf()]
        qmst = [tile([2, TT], BF16), tile([2, TT], BF16)]; b_qmst = [Buf(), Buf()]
        ckst = [tile([4, 256], BF16), tile([4, 256], BF16)]; b_ckst = [Buf(), Buf()]
        ckT = [tile([2, TT], BF16), tile([2, TT], BF16)]; b_ckT = [Buf(), Buf()]
        wist = [tile([4, 16]), tile([4, 16])]; b_wist = [Buf(), Buf()]
        junk = tile([256]); b_junk = Buf()
        ssq = tile([1]); b_ssq = Buf()
        qi = 0
        for tt in range(NTT):
            s = tt % 2
            hT, bh, _, _ = norm_tile(n, xsrc, tt, i)
            load_cs(0, tt, cs[0], b_cs[0])
            load_cs(1, tt, cs[1], b_cs[1], rows=16)
            load_cs(2, tt, cs[2], b_cs[2])
            load_cs(3, tt, cs[3], b_cs[3], rows=8)
            pA, bA = proj(hT, bh, w_sb, b_w, G["QX1"][0], 128)
            pB, bB = proj(hT, bh, w_sb, b_w, G["QX2"][0], 128)
            rope(r, pA, bA, pB, bB, 128, cs[0][0], cs[0][1], b_cs[0], qrst[s][:, 0, :], qrst[s][:, 1, :], b_qrst[s])
            P.dma("sp", lambda e, s=s, tt=tt: e.dma_start(out=Qd[0:256, :].rearrange("(c p) t -> p c t", p=128)[:, :, tsl(tt)], in_=qrst[s]), reads=[b_qrst[s]])
            for h in range(8):
                ps, pb = proj(hT, bh, w_sb, b_w, G["QN%d" % h][0], 96)
                qn, bqn = qn_sb[qi % 2], b_qn[qi % 2]
                ql, bql = qlst[qi % 2], b_qlst[qi % 2]
                qi += 1
                evac(ps, pb, 96, qn, bqn, partial=False, eng=("act" if h % 2 == 0 else "dve"))
                for rc in range(2):
                    ps2, pb2 = kb.bank()
                    P.op("pe", lambda e, ps2=ps2, h=h, rc=rc, qn=qn: e.matmul(ps2, lhsT=wuk_sb[:, h, rc * 128:(rc + 1) * 128], rhs=qn, start=True, stop=True), reads=[b_wuk, bqn], writes=[pb2])
                    evac(ps2, pb2, 128, ql[:, rc, :], bql, eng=("act" if rc == 0 else "dve"))
                P.dma("sp", lambda e, ql=ql, h=h, tt=tt: e.dma_start(out=QLd[h * 256:(h + 1) * 256, :].rearrange("(c p) t -> p c t", p=128)[:, :, tsl(tt)], in_=ql), reads=[bql])
            pA, bA = proj(hT, bh, w_sb, b_w, G["KR1"][0], 16)
            pB, bB = proj(hT, bh, w_sb, b_w, G["KR2"][0], 16)
            rope(r, pA, bA, pB, bB, 16, cs[1][0], cs[1][1], b_cs[1], krst[s][:, 0, :], krst[s][:, 1, :], b_krst[s])
            for c in range(2):
                P.dma("sp", lambda e, s=s, tt=tt, c=c: e.dma_start(out=Kd[16 * c:16 * c + 16, tsl(tt)], in_=krst[s][:, c, :]), reads=[b_krst[s]])
            pA, bA = proj(hT, bh, w_sb, b_w, G["QIX1"][0], 128)
            pB, bB = proj(hT, bh, w_sb, b_w, G["QIX2"][0], 128)
            rope(r, pA, bA, pB, bB, 128, cs[2][0], cs[2][1], b_cs[2], qist[s][:, 0, :], qist[s][:, 1, :], b_qist[s])
            for c in range(6):
                ps, pb = proj(hT, bh, w_sb, b_w, G["QIN%d" % c][0], 128)
                evac(ps, pb, 128, qist[s][:, 2 + c, :], b_qist[s], eng=("act" if c % 2 == 0 else "dve"))
            P.dma("sp", lambda e, s=s, tt=tt: e.dma_start(out=QId.rearrange("(c p) t -> p c t", p=128)[:, :, tsl(tt)], in_=qist[s]), reads=[b_qist[s]])
            pA, bA = proj(hT, bh, w_sb, b_w, G["KIX1"][0], 8)
            pB, bB = proj(hT, bh, w_sb, b_w, G["KIX2"][0], 8)
            rope(r, pA, bA, pB, bB, 8, cs[3][0], cs[3][1], b_cs[3], kist[s][0:8, 0, :], kist[s][0:8, 1, :], b_kist[s])
            ps, pb = proj(hT, bh, w_sb, b_w, G["KIN"][0], 48)
            evac(ps, pb, 48, kist[s][:, 2, :], b_kist[s])
            for (r0, nr, ci) in ((64, 8, 0), (72, 8, 1), (80, 48, 2)):
                P.dma("sp", lambda e, s=s, tt=tt, r0=r0, nr=nr, ci=ci: e.dma_start(out=Kd[r0:r0 + nr, tsl(tt)], in_=kist[s][0:nr, ci, :]), reads=[b_kist[s]])
            for c in range(2):
                ps, pb = proj(hT, bh, w_sb, b_w, G["QM%d" % c][0], 128)
                evac(ps, pb, 128, qmst[s][:, c, :], b_qmst[s], eng=("act" if c == 0 else "dve"))
            P.dma("sp", lambda e, s=s, tt=tt: e.dma_start(out=QMd.rearrange("(c p) t -> p c t", p=128)[:, :, tsl(tt)], in_=qmst[s]), reads=[b_qmst[s]])
            for tb_ in range(4):
                ps, pb = proj_tok(hT, bh, w_sb, b_w, G["CKV"][0], 256, tb_)
                P.op("act", lambda e, ps=ps: e.activation(out=junk, in_=ps[:, 0:256], func=AF.Square, accum_out=ssq[:, 0:1]), reads=[pb], writes=[b_junk, b_ssq])
                P.op("act", lambda e: e.activation(out=ssq, in_=ssq, func=AF.Sqrt, scale=1.0 / 256, bias=epsc[:, 0:1]), reads=[b_ssq, b_eps], writes=[b_ssq])
                recip(P, ssq, ssq, [b_ssq], [b_ssq])
                P.op("dve", lambda e, ps=ps, s=s, tb_=tb_: e.scalar_tensor_tensor(out=ckst[s][:, tb_, :], in0=ps[:, 0:256], scalar=ssq[:, 0:1], in1=kvn_sb, op0=ALU.mult, op1=ALU.mult),
                     reads=[pb, b_ssq, b_kvn], writes=[b_ckst[s]], partial=True)
            P.dma("sp", lambda e, s=s, tt=tt: e.dma_start(out=Vd[:, 0:256].rearrange("(n p) c -> p n c", p=128)[:, tt * 4:tt * 4 + 4, :], in_=ckst[s]), reads=[b_ckst[s]])
            for rc in range(2):
                ps, pb = kb.bank()
                psb = ps.bitcast(BF16)

                def f(e, psb=psb, rc=rc, s=s):
                    for tb_ in range(4):
                        ins = e.transpose(psb[:, tb_ * 128:(tb_ + 1) * 128], ckst[s][:, tb_, rc * 128:(rc + 1) * 128], ident)
                    return ins
                P.op("pe", f, reads=[b_ckst[s], b_ident], writes=[pb])
                P.op("act", lambda e, psb=psb, rc=rc, s=s: e.activation(out=ckT[s][:, rc, :], in_=psb[:, 0:512], func=AF.Copy), reads=[pb], writes=[b_ckT[s]], partial=True)
            P.dma("sp", lambda e, s=s, tt=tt: e.dma_start(out=Kd[256:512, :].rearrange("(c p) t -> p c t", p=128)[:, :, tsl(tt)], in_=ckT[s]), reads=[b_ckT[s]])
            for tb_ in range(4):
                ps, pb = proj_tok(hT, bh, w_sb, b_w, G["WI"][0], 16, tb_)
                P.op("act", lambda e, ps=ps, s=s, tb_=tb_: e.activation(out=wist[s][:, tb_, :], in_=ps[:, 0:16], func=AF.Copy, scale=1.0 / 32.0), reads=[pb], writes=[b_wist[s]], partial=True)
            P.dma("sp", lambda e, s=s, tt=tt: e.dma_start(out=WId.rearrange("(n p) c -> p n c", p=128)[:, tt * 4:tt * 4 + 4, :], in_=wist[s]), reads=[b_wist[s]])
        kb.phase_end(mark)

    def p2_a(i, NIT=12):
        mark = kb.off
        j = i // 3
        SC = float(128 ** -0.5)
        KI_sb = tile([T], BF16, parts=64); b_KI = Buf()
        P.dma("sp", lambda e: e.dma_start(out=KI_sb, in_=Kd[64:128, :]), writes=[b_KI])
        CKT_sb = tile([2, T], BF16); b_CKT = Buf()
        P.dma("sp", lambda e: e.dma_start(out=CKT_sb, in_=Kd[256:512, :].rearrange("(c p) t -> p c t", p=128)), writes=[b_CKT])
        CK_sb = tile([32, 256], BF16); b_CK = Buf()
        P.dma("sp", lambda e: e.dma_start(out=CK_sb, in_=Vd[:, 0:256].rearrange("(n p) c -> p n c", p=128)), writes=[b_CK])
        KR_sb = tile([T], BF16, parts=32); b_KR = Buf()
        P.dma("sp", lambda e: e.dma_start(out=KR_sb, in_=Kd[0:32, :]), writes=[b_KR])
        sel_sb = tile([16, 128], BF16); b_sel = Buf()
        P.dma("pool", lambda e: e.dma_start(out=sel_sb, in_=sel_d), writes=[b_sel])
        zmask = tile([128]); b_zm = Buf()
        P.dma("sp", lambda e: e.dma_start(out=zmask, in_=zmask_d), writes=[b_zm])
        bsel = tile([16]); b_bsel = Buf()
        P.dma("sp", lambda e: e.dma_start(out=bsel, in_=bsel_d), writes=[b_bsel])
        triq = tile([128]); b_triq = Buf()
        P.dma("sp", lambda e: e.dma_start(out=triq, in_=triq_d), writes=[b_triq])
        wuv_sb = tile([8, 2, 128], BF16); b_wuv = Buf()
        for h in range(8):
            P.dma("pool", lambda e, h=h: e.dma_start(out=wuv_sb[:, h, :, :], in_=wuv_d[j, h].rearrange("(c p) v -> p c v", p=128)), writes=[b_wuv], partial=True)
        id30k = tile([128], BF16); b_id30k = Buf()
        P.op("dve", lambda e: e.tensor_scalar(out=id30k, in0=ident, scalar1=-NEG, scalar2=None, op0=ALU.mult), reads=[b_ident], writes=[b_id30k])
        halfc = tile([1]); b_half = Buf()
        P.op("dve", lambda e: e.memset(halfc, 0.5), writes=[b_half])
        QI_sb = tile([16, TT], BF16, parts=64); b_QI = Buf()
        QI2 = tile([64, 128], BF16, parts=64); b_QI2 = Buf()
        QL_sb = tile([8, 2, TT], BF16); b_QL = Buf()
        QR_sb = tile([8, TT], BF16, parts=32); b_QR = Buf()
        WI_sb = tile([4, 16]); b_WI = Buf()
        S = tile([T]); b_S = Buf()
        mqb = tile([T], BF16); b_mqb = Buf()
        junk, b_junk = mqb, b_mqb
        MT = tile([32, TT], BF16); b_MT = Buf()
        NR = 9
        Rt = [tile([TT], BF16) for _ in range(NR)]; b_Rt = [Buf() for _ in range(NR)]
        NPT = 4
        PT = [tile([TT], BF16) for _ in range(NPT)]; b_PT = [Buf() for _ in range(NPT)]
        olat = tile([2, TT], BF16); b_olat = Buf()
        rL = tile([TT]); b_rL = Buf()
        mixst1 = tile([8, TT], BF16); b_mixst1 = Buf()
        mixst = [mixst1, mixst1]; b_mixst = [b_mixst1, b_mixst1]
        Z = tile([128]); b_Z = Buf()
        wcol = tile([16]); b_wcol = Buf()
        pw2 = tile([NIT]); b_pw2 = Buf()
        for it in range(NIT):
            P.op("dve", lambda e, it=it: e.memset(pw2[:, it:it + 1], float(2.0 ** -(it + 1))), writes=[b_pw2], partial=True)
        Wst = tile([NIT]); b_Wst = Buf()
        lo = tile([1]); b_lo = Buf()
        hi = tile([1]); b_hi = Buf()
        mid = tile([1]); b_mid = Buf()
        cnt = tile([1]); b_cnt = Buf()
        ge = tile([1]); b_ge = Buf()
        dd = tile([1]); b_dd = Buf()
        ri_ = [0]
        pi_ = [0]
        dbank = [0]

        def bankof(lst, ctr):
            k = lst[ctr[0] % len(lst)]
            ctr[0] += 1
            return kb.ps[k], kb.psb[k]
        sacc = [0]
        tbank = [0]
        gcount = [0]
        for tt in range(NTT):
            P.dma("sp", lambda e, tt=tt: e.dma_start(out=QI_sb, in_=QId.rearrange("(d h) t -> d h t", h=16)[:, :, tsl(tt)]), writes=[b_QI])
            if "qi2" not in SKIP:
              P.op("pool", lambda e: e.tensor_copy(out=QI2.rearrange("d j (h q) -> d j h q", q=8), in_=QI_sb.rearrange("d h (j q) -> d j h q", q=8)), reads=[b_QI], writes=[b_QI2])
            for h in range(8):
                P.dma("sp", lambda e, tt=tt, h=h: e.dma_start(out=QL_sb[:, h, :, :], in_=QLd[h * 256:(h + 1) * 256, :].rearrange("(c p) t -> p c t", p=128)[:, :, tsl(tt)]), writes=[b_QL], partial=(h > 0))
            P.dma("sp", lambda e, tt=tt: e.dma_start(out=QR_sb, in_=Qd[0:256, :].rearrange("(i h) t -> i h t", h=8)[:, :, tsl(tt)]), writes=[b_QR])
            P.dma("sp", lambda e, tt=tt: e.dma_start(out=WI_sb, in_=WId.rearrange("(n p) c -> p n c", p=128)[:, tt * 4:tt * 4 + 4, :]), writes=[b_WI])
            P.op("dve", lambda e, tt=tt: e.memset(MT[:, 4 * tt:4 * tt + 4, :], -1.0), writes=[b_MT])
            for qb in range(4):
                if "idx" in SKIP:
                    continue
                nb = 4 * tt + qb
                nk = (nb + 1) * 128
                if "z" not in SKIP:
                    P.op("dve", lambda e, qb=qb: e.tensor_tensor(out=Z.rearrange("p (h q) -> p h q", q=8), in0=WI_sb[:, qb, :].unsqueeze(2).to_broadcast([128, 16, 8]),
                                                             in1=zmask.rearrange("p (h q) -> p h q", q=8), op=ALU.mult), reads=[b_WI, b_zm], writes=[b_Z])
                ps, pb = bankof([2, 3], tbank)
                if "wcol" not in SKIP:
                    P.op("pe", lambda e, ps=ps: e.matmul(ps[:, 0:16], lhsT=Z, rhs=bsel, start=True, stop=True), reads=[b_Z, b_bsel], writes=[pb])
                    P.op("act", lambda e, ps=ps: e.activation(out=wcol, in_=ps[:, 0:16], func=AF.Copy), reads=[pb], writes=[b_wcol])
                items = [(kc, jj) for kc in range(tt + 1) for jj in range(16)]
                groups = [items[k:k + 3] for k in range(0, len(items), 3)]
                pss_of = {}
                gpend = []
                for gidx in range(len(groups) + 1):
                    if gidx < len(groups):
                        grp = groups[gidx]
                        bset = [2, 3, 4] if (gcount[0] % 2 == 0) else [5, 6, 7]
                        gcount[0] += 1
                        recs = []
                        for ii, (kc, jj) in enumerate(grp):
                            ncol = 512 if kc < tt else (qb + 1) * 128
                            rt, brt = Rt[ri_[0] % NR], b_Rt[ri_[0] % NR]
                            ri_[0] += 1
                            recs.append((kc, jj, ncol, kb.ps[bset[ii]], kb.psb[bset[ii]], rt, brt))

                        def fd(e, recs=recs, qb=qb):
                            for (kc, jj, ncol, psd, pbd, rt, brt) in recs:
                                ins = e.matmul(psd[:, 0:ncol], lhsT=QI2[:, qb * 16 + jj, :], rhs=KI_sb[:, kc * 512:kc * 512 + ncol], start=True, stop=True)
                            return ins
                        P.op("pe", fd, reads=[b_QI2, b_KI], writes=[r_[4] for r_ in recs])
                        for (kc, jj, ncol, psd, pbd, rt, brt) in recs:
                            P.op("dve", lambda e, psd=psd, rt=rt, jj=jj, ncol=ncol: e.tensor_scalar(out=rt[:, 0:ncol], in0=psd[:, 0:ncol], scalar1=0.0, scalar2=wcol[:, jj:jj + 1], op0=ALU.max, op1=ALU.mult),
                                 reads=[pbd, b_wcol], writes=[brt])
                        gpend.append(recs)
                    if gidx >= 1:
                        recs = gpend[gidx - 1]
                        for (kc, jj, ncol, psd, pbd, rt, brt) in recs:
                            if jj == 0:
                                pss_of[kc] = bankof([0, 1], sacc)
                        wr = []
                        for (kc, jj, ncol, psd, pbd, rt, brt) in recs:
                            if pss_of[kc][1] not in wr:
                                wr.append(pss_of[kc][1])

                        def fs(e, recs=recs, pss_of=dict(pss_of)):
                            for (kc, jj, ncol, psd, pbd, rt, brt) in recs:
                                ins = e.matmul(pss_of[kc][0][:, 0:ncol], lhsT=sel_sb[:, jj, :], rhs=rt[:, 0:ncol], start=(jj == 0), stop=(jj == 15))
                            return ins
                        all_first = all(r_[1] == 0 for r_ in recs)
                        P.op("pe", fs, reads=[r_[6] for r_ in reversed(recs)] + [b_sel], writes=wr, partial=True)
                        for (kc, jj, ncol, psd, pbd, rt, brt) in recs:
                            if jj != 15:
                                continue
                            pss, pbs = pss_of[kc]
                            if kc < tt:
                                P.op("act", lambda e, pss=pss, kc=kc: e.activation(out=S[:, kc * 512:(kc + 1) * 512], in_=pss, func=AF.Copy), reads=[pbs], writes=[b_S], partial=True)
                            else:
                                if qb > 0:
                                    P.op("dve", lambda e, pss=pss, kc=kc, qb=qb: e.tensor_copy(out=S[:, kc * 512:kc * 512 + qb * 128], in_=pss[:, 0:qb * 128]), reads=[pbs], writes=[b_S], partial=True)
                                P.op("dve", lambda e, pss=pss, kc=kc, qb=qb: e.tensor_tensor(out=S[:, kc * 512 + qb * 128:kc * 512 + (qb + 1) * 128], in0=pss[:, qb * 128:(qb + 1) * 128], in1=triq, op=ALU.add),
                                     reads=[pbs, b_triq], writes=[b_S], partial=True)
                if nb >= 2 and "thr" not in SKIP:
                    P.op("dve", lambda e, nb=nb: e.tensor_reduce(out=lo, in_=S[:, 0:nb * 128], axis=AX.X, op=ALU.min), reads=[b_S], writes=[b_lo])
                    P.op("dve", lambda e, nk=nk: e.tensor_reduce(out=hi, in_=S[:, 0:nk], axis=AX.X, op=ALU.max), reads=[b_S], writes=[b_hi])
                    P.op("dve", lambda e: e.scalar_tensor_tensor(out=mid, in0=lo, scalar=hi[:, 0:1], in1=halfc, op0=ALU.add, op1=ALU.mult), reads=[b_lo, b_hi, b_half], writes=[b_mid])
                    P.op("dve", lambda e: e.tensor_tensor(out=dd, in0=hi, in1=lo, op=ALU.subtract), reads=[b_lo, b_hi], writes=[b_dd])
                    P.op("dve", lambda e: e.tensor_scalar(out=Wst, in0=pw2, scalar1=dd[:, 0:1], scalar2=None, op0=ALU.mult), reads=[b_dd, b_pw2], writes=[b_Wst])
                    for it in range(NIT):
                        P.op("dve", lambda e, nk=nk: e.tensor_scalar(out=junk[:, 0:nk], in0=S[:, 0:nk], scalar1=mid[:, 0:1], scalar2=None, op0=ALU.is_ge, op1=ALU.add, accum_out=cnt[:, 0:1]),
                             reads=[b_S, b_mid], writes=[b_junk, b_cnt])
                        P.op("dve", lambda e: e.tensor_scalar(out=ge, in0=cnt, scalar1=255.5, scalar2=0.5, op0=ALU.is_ge, op1=ALU.subtract), reads=[b_cnt], writes=[b_ge])
                        P.op("dve", lambda e, it=it: e.scalar_tensor_tensor(out=mid, in0=ge, scalar=Wst[:, it:it + 1], in1=mid, op0=ALU.mult, op1=ALU.add), reads=[b_ge, b_Wst, b_mid], writes=[b_mid])
                    P.op("dve", lambda e: e.scalar_tensor_tensor(out=lo, in0=Wst[:, NIT - 1:NIT], scalar=-0.5, in1=mid, op0=ALU.mult, op1=ALU.add), reads=[b_Wst, b_mid], writes=[b_lo])
                else:
                    P.op("dve", lambda e: e.memset(lo, -1.0e4), writes=[b_lo])
                P.op("dve", lambda e, nk=nk: e.tensor_scalar(out=mqb[:, 0:nk], in0=S[:, 0:nk], scalar1=lo[:, 0:1], scalar2=1.0, op0=ALU.is_ge, op1=ALU.subtract), reads=[b_S, b_lo], writes=[b_mqb])
                for kb0 in range(0, nb + 1, 4):
                    if "tr" in SKIP:
                        continue
                    gq = min(4, nb + 1 - kb0)
                    ps, pb = bankof([2, 3], tbank)
                    psb_ = ps.bitcast(BF16)

                    def f(e, psb_=psb_, kb0=kb0, gq=gq):
                        for t_i in range(gq):
                            ins = e.transpose(psb_[:, t_i * 128:(t_i + 1) * 128], mqb[:, (kb0 + t_i) * 128:(kb0 + t_i + 1) * 128], ident)
                        return ins
                    P.op("pe", f, reads=[b_mqb, b_ident], writes=[pb])
                    P.op("act", lambda e, psb_=psb_, kb0=kb0, gq=gq, qb=qb: e.activation(out=MT[:, kb0:kb0 + gq, qb * 128:(qb + 1) * 128], in_=psb_[:, 0:gq * 128].rearrange("p (g c) -> p g c", c=128), func=AF.Copy),
                         reads=[pb], writes=[b_MT], partial=True)
            nkb = 4 * (tt + 1)
            sm = tt % 2
            for h in range(8):
                if "att" in SKIP:
                    continue
                psO = [(kb.ps[0], kb.psb[0]), (kb.ps[1], kb.psb[1])]
                psL, pbL = kb.ps[2], kb.psb[2]
                pendp = []
                ADEPTH = 2
                for kidx in range(nkb + ADEPTH):
                    if kidx < nkb:
                        kbk = kidx
                        pss, pbs = bankof([4, 5, 6, 7], dbank)
                        ks = slice(kbk * 128, (kbk + 1) * 128)

                        def f(e, pss=pss, kbk=kbk, ks=ks, h=h):
                            e.matmul(pss, lhsT=id30k, rhs=MT[:, kbk, :], start=True, stop=False)
                            e.matmul(pss, lhsT=CKT_sb[:, 0, ks], rhs=QL_sb[:, h, 0, :], start=False, stop=False)
                            e.matmul(pss, lhsT=CKT_sb[:, 1, ks], rhs=QL_sb[:, h, 1, :], start=False, stop=False)
                            return e.matmul(pss, lhsT=KR_sb[:, ks], rhs=QR_sb[:, h, :], start=False, stop=True)
                        P.op("pe", f, reads=[b_id30k, b_MT, b_CKT, b_QL, b_KR, b_QR], writes=[pbs])
                        pt, bpt = PT[pi_[0] % NPT], b_PT[pi_[0] % NPT]
                        pi_[0] += 1
                        P.op("act", lambda e, pss=pss, pt=pt: e.activation(out=pt, in_=pss, func=AF.Exp, scale=SC), reads=[pbs], writes=[bpt])
                        pendp.append((kbk, pt, bpt))
                    if kidx >= ADEPTH:
                        kbk, pt, bpt = pendp[kidx - ADEPTH]

                        def f2(e, pt=pt, kbk=kbk, first=(kbk == 0), last=(kbk == nkb - 1)):
                            e.matmul(psO[0][0], lhsT=CK_sb[:, kbk, 0:128], rhs=pt, start=first, stop=last)
                            e.matmul(psO[1][0], lhsT=CK_sb[:, kbk, 128:256], rhs=pt, start=first, stop=last)
                            return e.matmul(psL, lhsT=ones_b, rhs=pt, start=first, stop=last)
                        P.op("pe", f2, reads=[bpt, b_CK, b_ones_b], writes=[psO[0][1], psO[1][1], pbL], partial=(kbk > 0))
                recip(P, rL, psL, [pbL], [b_rL])
                for rc in range(2):
                    P.op("dve", lambda e, rc=rc: e.tensor_tensor(out=olat[:, rc, :], in0=psO[rc][0], in1=rL, op=ALU.mult), reads=[psO[rc][1], b_rL], writes=[b_olat], partial=(rc > 0))
                pso, pbo = kb.ps[3], kb.psb[3]

                def f3(e, h=h, pso=pso):
                    e.matmul(pso, lhsT=wuv_sb[:, h, 0, :], rhs=olat[:, 0, :], start=True, stop=False)
                    return e.matmul(pso, lhsT=wuv_sb[:, h, 1, :], rhs=olat[:, 1, :], start=False, stop=True)
                P.op("pe", f3, reads=[b_wuv, b_olat], writes=[pbo])
                P.op("act", lambda e, pso=pso, h=h, sm=sm: e.activation(out=mixst[sm][:, h, :], in_=pso, func=AF.Copy), reads=[pbo], writes=[b_mixst[sm]], partial=True)
            P.dma("sp", lambda e, sm=sm, tt=tt: e.dma_start(out=MIXd.rearrange("(c p) t -> p c t", p=128)[:, :, tsl(tt)], in_=mixst[sm]), reads=[b_mixst[sm]])
        kb.phase_end(mark)

    def run_layers():
        xcur = xin
        for i in layers:
            kind = i % 3
            if kind == 1:
                p1_b(i, xcur)
                p2_b(i)
            elif kind == 2:
                p1_c(i, xcur)
                p2_c(i)
            else:
                p1_a(i, xcur)
                p2_a(i)
            phase_out(i, xcur, XS[0])
            phase_ffn(i, XS[0], XS[1])
            xcur = XS[1]
        if final:
            phase_final(xcur)
        else:
            mark = kb.off
            t_ = [tile([8, TT]), tile([8, TT])]; b_t = [Buf(), Buf()]
            for tt in range(NTT):
                s = tt % 2
                P.dma("sp", lambda e, s=s, tt=tt: e.dma_start(out=t_[s], in_=xcur.rearrange("(kc p) t -> p kc t", p=128)[:, :, tsl(tt)]), writes=[b_t[s]])
                P.dma("sp", lambda e, s=s, tt=tt: e.dma_start(out=out_d.rearrange("(kc p) t -> p kc t", p=128)[:, :, tsl(tt)], in_=t_[s]), reads=[b_t[s]])
            kb.phase_end(mark)
        if taps:
            mark = kb.off
            tp_ = [tile([8, TT], BF16), tile([8, TT], BF16)]; b_tp = [Buf(), Buf()]
            tapo = nc.dram_tensor("tapmix", [D, T], BF16, kind="ExternalOutput").ap()
            for tt in range(NTT):
                s = tt % 2
                P.dma("sp", lambda e, s=s, tt=tt: e.dma_start(out=tp_[s], in_=MIXd.rearrange("(kc p) t -> p kc t", p=128)[:, :, tsl(tt)]), writes=[b_tp[s]])
                P.dma("sp", lambda e, s=s, tt=tt: e.dma_start(out=tapo.rearrange("(kc p) t -> p kc t", p=128)[:, :, tsl(tt)], in_=tp_[s]), reads=[b_tp[s]])
            kb.phase_end(mark)
        P.barrier(engines=("sp",))

    kb_ctx = dict(locals())
    return kb, kb_ctx


def _host_inputs(inp, b, layers):
    f = np.float32
    m = {}
    m["xT"] = np.ascontiguousarray(inp["x"][b].T.astype(f))
    m["memT"] = np.ascontiguousarray(inp["mem"][b].T.astype(f))
    m["pos"] = np.ascontiguousarray(inp["positions"][b][None, :].astype(np.int32))
    gl = [inp["g_mix"][i] for i in range(4)] + [inp["g_ffn"][i] for i in range(4)] + [inp["g_mem"], inp["g_final"]]
    m["gvec"] = np.ascontiguousarray(np.stack([g.reshape(8, 128).T for g in gl], axis=1).astype(f))
    for k in ("invc", "cmask", "sel", "zmask", "bsel", "triq"):
        m[k] = CONSTS[k]
    for i in layers:
        kind, j = i % 3, i // 3
        w = (inp["a_w_in"], inp["b_w_in"], inp["c_w_in"])[kind][j]
        m["w_in%d" % i] = np.ascontiguousarray(w[:, PERMS[kind][0]].astype(f))
        m["w_out%d" % i] = np.ascontiguousarray((inp["a_w_out"], inp["b_w_out"], inp["c_w_out"])[kind][j].astype(f))
    m["w_up"] = np.ascontiguousarray(inp["f_w_up"].astype(f))
    m["w_down"] = np.ascontiguousarray(inp["f_w_down"].astype(f))
    m["convw"] = np.ascontiguousarray(inp["f_conv_w"].reshape(4, 3, 22, 128).transpose(0, 3, 2, 1).astype(f))
    m["convb"] = np.ascontiguousarray(inp["f_conv_b"].reshape(4, 22, 128).transpose(0, 2, 1).astype(f))
    m["wmem"] = np.ascontiguousarray(inp["w_mem_kv"].astype(f))
    sk = inp["b_sinks"][0]
    m["sinks"] = np.ascontiguousarray(np.stack([np.repeat(sk[2 * c:2 * c + 2], 64) for c in range(8)], axis=1).astype(f))
    m["kvn"] = np.ascontiguousarray(inp["a_kv_norm"][:, None, :].astype(f))
    m["wukT"] = np.ascontiguousarray(inp["a_w_uk"].transpose(0, 2, 3, 1).astype(f))
    m["wuv"] = np.ascontiguousarray(inp["a_w_uv"].transpose(0, 2, 1, 3).astype(f))
    return m


_CACHE = {}


def kernel(**inputs):
    inp = {k: np.asarray(v) for k, v in inputs.items()}
    layers = (0, 1, 2, 3)
    if "kb" not in _CACHE:
        kb, ctx = build_program(layers=layers, final=True)
        ctx["run_layers"]()
        kb.P.emit()
        _CACHE["kb"] = kb
    kb = _CACHE["kb"]
    n = 8
    maps = []
    for b in range(n):
        m = _host_inputs(inp, b, layers)
        maps.append({k: m[k] for k in kb.inputs})
    res = run_bass_kernel_spmd(kb.nc, maps, core_ids=list(range(n)))
    out = np.stack([np.ascontiguousarray(res.results[b]["outT"].T) for b in range(n)], axis=0)
    return out.astype(np.float32)
```

```python
import numpy as np
import concourse.bass as bass
import concourse.mybir as mybir
from concourse.bass_utils import run_bass_kernel_spmd
from contextlib import ExitStack

F32 = mybir.dt.float32
BF16 = mybir.dt.bfloat16
I32 = mybir.dt.int32
AF = mybir.ActivationFunctionType
ALU = mybir.AluOpType
AX = mybir.AxisListType

ENGS = ("pe", "act", "dve", "pool", "sp")
SKIP = set()
NDMA = 8
T = 4096
D = 1024
TT = 512
NTT = T // TT
NEG = -30000.0
EPS = 1e-6


class Buf:
    __slots__ = ("name", "base", "parts", "rd")

    def __init__(self, name=""):
        self.name = name
        self.base = []
        self.parts = []
        self.rd = {}


class Prog:
    def __init__(self, nc):
        self.nc = nc
        self.streams = {e: [] for e in ENGS}
        self.cnt = {e: 0 for e in ENGS}
        self.dma_i = {e: 0 for e in ENGS}
        self.dma_val = {}
        self.known = {e: {} for e in ENGS}
        self.last_tok = {e: None for e in ENGS}
        self.dma_latest = {}

    def _wait(self, eng, tok):
        semkey, val, _ = tok
        if self.known[eng].get(semkey, 0) >= val:
            return
        self.known[eng][semkey] = val
        self.streams[eng].append(("wait", semkey, val))

    def _deps(self, eng, reads, writes, is_dma, partial):
        deps = []
        for b in reads:
            deps.extend(b.base)
            deps.extend(b.parts)
        for b in writes:
            deps.extend(b.base)
            if not partial:
                deps.extend(b.parts)
            for e2, t in b.rd.items():
                if e2 == eng and not is_dma:
                    continue
                deps.append(t)
        return [t for t in deps if not (t[2] == "pe" and eng == "pe" and not is_dma)]

    def op(self, eng, fn, reads=(), writes=(), partial=False):
        for t in self._deps(eng, reads, writes, False, partial):
            self._wait(eng, t)
        self.cnt[eng] += 1
        tok = ("E" + eng, self.cnt[eng], eng)
        self.streams[eng].append(("op", fn, tok[0], 1, tok[1]))
        self._commit(eng, tok, reads, writes, partial)
        self.last_tok[eng] = tok
        return tok

    def dma(self, eng, fn, reads=(), writes=(), partial=False):
        i = self.dma_i[eng]
        self.dma_i[eng] += 1
        semkey = "D%s%d" % (eng, i % NDMA)
        prev = self.dma_val.get(semkey, 0)
        if prev:
            self._wait(eng, (semkey, prev, "dma"))
        for t in self._deps(eng, reads, writes, True, partial):
            self._wait(eng, t)
        val = prev + 16
        self.dma_val[semkey] = val
        tok = (semkey, val, "dma")
        self.streams[eng].append(("op", fn, semkey, 16, val))
        self._commit(semkey, tok, reads, writes, partial)
        self.dma_latest[semkey] = tok
        return tok

    def _commit(self, ekey, tok, reads, writes, partial):
        for b in reads:
            b.rd[ekey] = tok
        for b in writes:
            if partial:
                b.parts = [t for t in b.parts if t[0] != tok[0]] + [tok]
            else:
                b.base = [tok]
                b.parts = []
                b.rd = {}

    def barrier(self, engines=ENGS):
        toks = [t for t in self.last_tok.values() if t is not None] + list(self.dma_latest.values())
        for e in engines:
            for t in toks:
                if t[0] == "E" + e:
                    continue
                self._wait(e, t)

    def emit(self):
        nc = self.nc
        semkeys = set()
        waited = set()
        for e in ENGS:
            for it in self.streams[e]:
                if it[0] == "wait":
                    semkeys.add(it[1])
                    waited.add((it[1], it[2]))
        valmap = {}
        incs = {}
        for e in ENGS:
            c = 0
            for k, it in enumerate(self.streams[e]):
                if it[0] == "op" and it[3] == 1:
                    if (it[2], it[4]) in waited:
                        c += 1
                        valmap[(it[2], it[4])] = c
                        incs[(e, k)] = True
                elif it[0] == "op":
                    semkeys.add(it[2])
        with ExitStack() as st:
            sems = {k: st.enter_context(nc.semaphore(k)) for k in sorted(semkeys)}
            block = st.enter_context(nc.Block())

            def run(eobj, ename, items):
                for k, it in enumerate(items):
                    if it[0] == "wait":
                        v = valmap[(it[1], it[2])] if it[1].startswith("E") else it[2]
                        eobj.wait_ge(sems[it[1]], v)
                    elif it[3] == 16:
                        it[1](eobj).then_inc(sems[it[2]], 16)
                    else:
                        ins = it[1](eobj)
                        if (ename, k) in incs:
                            ins.then_inc(sems[it[2]], 1)

            @block.tensor
            def _(e):
                run(e, "pe", self.streams["pe"])

            @block.scalar
            def _(e):
                run(e, "act", self.streams["act"])

            @block.vector
            def _(e):
                run(e, "dve", self.streams["dve"])

            @block.gpsimd
            def _(e):
                run(e, "pool", self.streams["pool"])

            @block.sync
            def _(e):
                run(e, "sp", self.streams["sp"])


def _perm_a():
    cols, groups = [], {}

    def add(name, idx):
        groups[name] = (len(cols), len(idx))
        cols.extend(idx)
    add("QX1", [h * 128 + i for i in range(16) for h in range(8)])
    add("QX2", [h * 128 + 16 + i for i in range(16) for h in range(8)])
    for h in range(8):
        add("QN%d" % h, [h * 128 + 32 + n for n in range(96)])
    add("KR1", [1280 + i for i in range(16)])
    add("KR2", [1280 + 16 + i for i in range(16)])
    add("QIX1", [1312 + h * 64 + i for i in range(8) for h in range(16)])
    add("QIX2", [1312 + h * 64 + 8 + i for i in range(8) for h in range(16)])
    for c in range(6):
        add("QIN%d" % c, [1312 + h * 64 + d for d in range(16 + 8 * c, 24 + 8 * c) for h in range(16)])
    add("KIX1", [2336 + i for i in range(8)])
    add("KIX2", [2336 + 8 + i for i in range(8)])
    add("KIN", [2336 + 16 + i for i in range(48)])
    add("QM0", list(range(2416, 2544)))
    add("QM1", list(range(2544, 2672)))
    add("CKV", list(range(1024, 1280)))
    add("WI", list(range(2400, 2416)))
    return np.array(cols), groups


def _perm_b():
    cols, groups = [], {}

    def add(name, idx):
        groups[name] = (len(cols), len(idx))
        cols.extend(idx)
    add("QX1", [h * 64 + i for i in range(8) for h in range(16)])
    add("QX2", [h * 64 + 8 + i for i in range(8) for h in range(16)])
    for c in range(6):
        add("QN%d" % c, [h * 64 + d for d in range(16 + 8 * c, 24 + 8 * c) for h in range(16)])
    add("KX1", [1024 + h * 64 + i for i in range(8) for h in range(4)])
    add("KX2", [1024 + h * 64 + 8 + i for i in range(8) for h in range(4)])
    add("KN0", [1024 + h * 64 + d for d in range(16, 48) for h in range(4)])
    add("KN1", [1024 + h * 64 + d for d in range(48, 64) for h in range(4)])
    add("QM0", list(range(1536, 1664)))
    add("QM1", list(range(1664, 1792)))
    add("V", list(range(1280, 1536)))
    return np.array(cols), groups


def _perm_c():
    cols, groups = [], {}

    def add(name, idx):
        groups[name] = (len(cols), len(idx))
        cols.extend(idx)
    for pre, base in (("Q", 0), ("K", 1024)):
        add(pre + "X1", [base + h * 128 + i for i in range(16) for h in range(8)])
        add(pre + "X2", [base + h * 128 + 16 + i for i in range(16) for h in range(8)])
        for c in range(6):
            add(pre + "N%d" % c, [base + h * 128 + d for d in range(32 + 16 * c, 48 + 16 * c) for h in range(8)])
    add("QM0", list(range(3072, 3200)))
    add("QM1", list(range(3200, 3328)))
    add("V", list(range(2048, 3072)))
    return np.array(cols), groups


PERMS = {0: _perm_a(), 1: _perm_b(), 2: _perm_c()}


def _consts():
    c = {}
    inv32 = (500000.0 ** (-np.arange(0, 32, 2, dtype=np.float32) / 32)).astype(np.float32)
    inv16 = (500000.0 ** (-np.arange(0, 16, 2, dtype=np.float32) / 16)).astype(np.float32)
    invc = np.zeros((128, 5), np.float32)
    p = np.arange(128)
    invc[:, 0] = inv32[p // 8]
    invc[:, 1] = inv32[p % 16]
    invc[:, 2] = inv16[p // 16]
    invc[:, 3] = inv16[p % 8]
    invc[:, 4] = inv16[(p % 32) // 4]
    c["invc"] = invc
    k = np.arange(128)[:, None]
    q = np.arange(128)[None, :]
    m = np.zeros((128, 4, 128), np.float32)
    m[:, 0] = np.where(k <= q, 0.0, NEG)
    m[:, 1] = np.where(k > q, 0.0, NEG)
    m[:, 2] = np.where(k >= q, 0.0, NEG)
    m[:, 3] = np.eye(128, dtype=np.float32)
    c["cmask"] = m
    sel = np.zeros((128, 16, 128), np.float32)
    for j in range(16):
        for pp in range(128):
            sel[pp, j, 8 * j + pp % 8] = 1.0
    c["sel"] = sel
    zm = np.zeros((128, 128), np.float32)
    for mm in range(128):
        for pp in range(128):
            if mm % 8 == pp % 8:
                zm[mm, pp] = 1.0
    c["zmask"] = zm
    bs = np.zeros((128, 16), np.float32)
    for mm in range(128):
        bs[mm, mm // 8] = 1.0
    c["bsel"] = bs
    c["triq"] = np.where(q.T >= k.T, 0.0, NEG).astype(np.float32) if False else np.where(np.arange(128)[None, :] <= np.arange(128)[:, None], 0.0, NEG).astype(np.float32)
    return c


CONSTS = _consts()


class KB:
    def __init__(self, layers=(0, 1, 2, 3), final=True):
        self.layers = layers
        self.final = final
        nc = self.nc = bass.Bass("TRN2", target_bir_lowering=False)
        self.P = Prog(nc)
        self.inputs = {}
        self.arena = nc.alloc_sbuf_tensor("arena", [128, 206 * 1024 // 4], F32).ap()
        self.cap = 206 * 1024
        self.off = 0
        self.ps = [nc.alloc_psum_tensor("ps%d" % i, [128, 512], F32).ap() for i in range(8)]
        self.psb = [Buf("ps%d" % i) for i in range(8)]
        self.psi = 0

    def din(self, name, shape, dtype=F32):
        ap = self.nc.dram_tensor(name, list(shape), dtype, kind="ExternalInput").ap()
        self.inputs[name] = ap
        return ap

    def dscr(self, name, shape, dtype=BF16):
        return self.nc.dram_tensor(name, list(shape), dtype).ap()

    def tile(self, free, dtype=F32, parts=128):
        if isinstance(free, int):
            free = [free]
        n = int(np.prod(free))
        es = 2 if dtype == BF16 else 4
        nb = (n * es + 31) // 32 * 32
        off = self.off
        self.off += nb
        assert self.off <= self.cap, "SBUF arena overflow %d" % self.off
        v = self.arena[0:parts, off // 4:(off + nb) // 4]
        if dtype != F32:
            v = v.bitcast(dtype)
        v = v[:, 0:n]
        if len(free) == 2:
            v = v.rearrange("p (a b) -> p a b", b=free[1])
        elif len(free) == 3:
            v = v.rearrange("p (a b c) -> p a b c", b=free[1], c=free[2])
        return v

    def bank(self):
        i = self.psi
        self.psi = (self.psi + 1) % 8
        return self.ps[i], self.psb[i]

    def phase_end(self, mark):
        self.P.barrier()
        self.off = mark


def recip(P, out, in_, rbuf, wbuf):
    P.op("dve", lambda e: e.reciprocal(out=out, in_=in_), reads=rbuf, writes=wbuf)


def build_program(layers=(0, 1, 2, 3), final=True, taps=(), ffn=True):
    kb = KB(layers, final)
    nc, P = kb.nc, kb.P
    tile = kb.tile

    xin = kb.din("xT", [D, T])
    memT_d = kb.din("memT", [D, 256])
    pos_d = kb.din("pos", [1, T], I32)
    gv_d = kb.din("gvec", [128, 4 + 4 + 1 + 1, 8])
    invc_d = kb.din("invc", [128, 5])
    cmask_d = kb.din("cmask", [128, 4, 128])
    sel_d = kb.din("sel", [128, 16, 128])
    zmask_d = kb.din("zmask", [128, 128])
    bsel_d = kb.din("bsel", [128, 16])
    triq_d = kb.din("triq", [128, 128])
    w_in_d, w_out_d = {}, {}
    for i in layers:
        w_in_d[i] = kb.din("w_in%d" % i, [D, len(PERMS[i % 3][0])])
        w_out_d[i] = kb.din("w_out%d" % i, [1280, D])
    w_up_d = kb.din("w_up", [4, D, 5632] if ffn else [4, 128, 8])
    w_down_d = kb.din("w_down", [4, 2816, D] if ffn else [4, 128, 8])
    convw_d = kb.din("convw", [4, 128, 22, 3])
    convb_d = kb.din("convb", [4, 128, 22])
    wmem_d = kb.din("wmem", [4, D, 512])
    sinks_d = kb.din("sinks", [128, 8])
    kvn_d = kb.din("kvn", [2, 1, 256])
    wuk_d = kb.din("wukT", [2, 8, 96, 256])
    wuv_d = kb.din("wuv", [2, 8, 256, 128])
    out_d = nc.dram_tensor("outT", [D, T], F32, kind="ExternalOutput").ap()

    XS = [kb.dscr("xs0", [D, T], F32), kb.dscr("xs1", [D, T], F32)]
    MIXd = kb.dscr("mixd", [D, T])
    QMd = kb.dscr("qmd", [256, T])
    Qd = kb.dscr("qd", [D, T])
    Kd = kb.dscr("kd", [D, T])
    Vd = kb.dscr("vd", [T, D])
    ROPE = [(kb.dscr("cos%d" % k, [128, T], F32), kb.dscr("sin%d" % k, [128, T], F32)) for k in range(5)]

    def tsl(tt):
        return slice(tt * TT, (tt + 1) * TT)

    ones_f = tile([128]); b_ones_f = Buf()
    ident = tile([128], BF16); b_ident = Buf()
    ones_b = tile([128], BF16); b_ones_b = Buf()
    onz = tile([192], BF16); b_onz = Buf()
    maskb = tile([3, 512], BF16); b_maskb = Buf()
    gv = tile([10, 8]); b_gv = Buf()
    memn = tile([8, 256], BF16); b_memn = Buf()
    invc = tile([5]); b_invc = Buf()
    epsc = tile([1]); b_eps = Buf()
    P.op("dve", lambda e: e.memset(ones_f, 1.0), writes=[b_ones_f])
    P.op("dve", lambda e: e.memset(epsc, EPS), writes=[b_eps])
    P.op("dve", lambda e: e.memset(ones_b, 1.0), writes=[b_ones_b])
    P.op("dve", lambda e: e.memset(onz, 0.0), writes=[b_onz])
    P.op("dve", lambda e: e.memset(onz[:, 64:128], 1.0), writes=[b_onz])
    P.dma("sp", lambda e: e.dma_start(out=gv, in_=gv_d), writes=[b_gv])
    P.dma("sp", lambda e: e.dma_start(out=invc, in_=invc_d), writes=[b_invc])
    mark0 = kb.off
    cm = tile([4, 128]); b_cm = Buf()
    P.dma("sp", lambda e: e.dma_start(out=cm, in_=cmask_d), writes=[b_cm])
    P.op("dve", lambda e: e.tensor_copy(out=ident, in_=cm[:, 3, :]), reads=[b_cm], writes=[b_ident])
    for k in range(3):
        for r in range(4):
            P.op("dve", lambda e, k=k, r=r: e.tensor_copy(out=maskb[:, k, r * 128:(r + 1) * 128], in_=cm[:, k, :]),
                 reads=[b_cm], writes=[b_maskb], partial=True)

    posi = tile([T], I32); b_posi = Buf()
    posf = tile([T]); b_posf = Buf()
    P.dma("sp", lambda e: e.dma_start(out=posi, in_=pos_d.partition_broadcast(128)), writes=[b_posi])
    P.op("dve", lambda e: e.tensor_copy(out=posf, in_=posi), reads=[b_posi], writes=[b_posf])
    HALF = 2048
    ang = tile([HALF]); b_ang = Buf()
    yk = tile([HALF]); b_yk = Buf()
    ki = tile([HALF], I32); b_ki = Buf()
    rr = tile([HALF]); b_rr = Buf()
    mm_ = tile([HALF]); b_mm = Buf()
    tb = [tile([HALF]), tile([HALF])]; b_tb = [Buf(), Buf()]
    TWO_PI = float(2 * np.pi)
    tbi = 0
    for k in range(5):
        for half in range(2):
            hs = slice(half * HALF, (half + 1) * HALF)
            for which in range(2):
                shift = float(np.pi / 2) if which == 0 else 0.0
                P.op("dve", lambda e, k=k, hs=hs, shift=shift: e.tensor_scalar(out=ang, in0=posf[:, hs], scalar1=invc[:, k:k + 1], scalar2=shift, op0=ALU.mult, op1=ALU.add),
                     reads=[b_posf, b_invc], writes=[b_ang])
                P.op("dve", lambda e: e.tensor_scalar(out=yk, in0=ang, scalar1=1.0 / TWO_PI, scalar2=None, op0=ALU.mult), reads=[b_ang], writes=[b_yk])
                P.op("dve", lambda e: e.tensor_copy(out=ki, in_=yk), reads=[b_yk], writes=[b_ki])
                P.op("dve", lambda e: e.tensor_copy(out=yk, in_=ki), reads=[b_ki], writes=[b_yk])
                P.op("dve", lambda e: e.scalar_tensor_tensor(out=rr, in0=yk, scalar=-TWO_PI, in1=ang, op0=ALU.mult, op1=ALU.add), reads=[b_yk, b_ang], writes=[b_rr])
                P.op("dve", lambda e: e.tensor_scalar(out=mm_, in0=rr, scalar1=float(np.pi), scalar2=-TWO_PI, op0=ALU.is_gt, op1=ALU.mult), reads=[b_rr], writes=[b_mm])
                P.op("dve", lambda e: e.tensor_tensor(out=rr, in0=rr, in1=mm_, op=ALU.add), reads=[b_rr, b_mm], writes=[b_rr])
                P.op("dve", lambda e: e.tensor_scalar(out=mm_, in0=rr, scalar1=-float(np.pi), scalar2=TWO_PI, op0=ALU.is_lt, op1=ALU.mult), reads=[b_rr], writes=[b_mm])
                P.op("dve", lambda e: e.tensor_tensor(out=rr, in0=rr, in1=mm_, op=ALU.add), reads=[b_rr, b_mm], writes=[b_rr])
                P.op("dve", lambda e: e.tensor_scalar(out=rr, in0=rr, scalar1=3.14159, scalar2=-3.14159, op0=ALU.min, op1=ALU.max), reads=[b_rr], writes=[b_rr])
                t_, bt_ = tb[tbi % 2], b_tb[tbi % 2]
                tbi += 1
                P.op("act", lambda e, t_=t_: e.activation(out=t_, in_=rr, func=AF.Sin), reads=[b_rr], writes=[bt_])
                dst = ROPE[k][which]
                P.dma("sp", lambda e, t_=t_, dst=dst, hs=hs: e.dma_start(out=dst[:, hs], in_=t_), reads=[bt_])

    mt = tile([8, 256]); b_mt = Buf()
    msq = tile([8, 256]); b_msq = Buf()
    mrs = tile([256]); b_mrs = Buf()
    P.dma("sp", lambda e: e.dma_start(out=mt, in_=memT_d.rearrange("(kc p) m -> p kc m", p=128)), writes=[b_mt])
    P.op("act", lambda e: e.activation(out=msq, in_=mt, func=AF.Square), reads=[b_mt], writes=[b_msq])
    ps, pb = kb.bank()

    def f_(e, ps=ps):
        for kc in range(8):
            ins = e.matmul(ps[:, 0:256], lhsT=ones_f, rhs=msq[:, kc, :], start=(kc == 0), stop=(kc == 7))
        return ins
    P.op("pe", f_, reads=[b_msq, b_ones_f], writes=[pb])
    P.op("act", lambda e, ps=ps: e.activation(out=mrs, in_=ps[:, 0:256], func=AF.Sqrt, scale=1.0 / D, bias=epsc[:, 0:1]), reads=[pb, b_eps], writes=[b_mrs])
    recip(P, mrs, mrs, [b_mrs], [b_mrs])
    for kc in range(8):
        P.op("dve", lambda e, kc=kc: e.scalar_tensor_tensor(out=memn[:, kc, :], in0=mt[:, kc, :], scalar=gv[:, 8, kc:kc + 1], in1=mrs, op0=ALU.mult, op1=ALU.mult),
             reads=[b_mt, b_gv, b_mrs], writes=[b_memn], partial=True)
    kb.phase_end(mark0)
    PERSIST = kb.off

    def load_w(w_d, c0, ncols, dst, b_dst, kcs=8):
        src = w_d.rearrange("(kc p) m -> p kc m", p=128)
        c = 0
        while c < ncols:
            n = min(512, ncols - c)
            P.dma("pool", lambda e, c=c, n=n: e.dma_start(out=dst[:, :, c:c + n], in_=src[:, :, c0 + c:c0 + c + n]), writes=[b_dst], partial=True)
            c += n

    class NormCtx:
        pass

    def norm_setup():
        n = NormCtx()
        n.xt = [tile([8, TT]), tile([8, TT])]; n.b_xt = [Buf(), Buf()]
        n.sq = tile([8, TT]); n.b_sq = Buf()
        n.rs = tile([TT]); n.b_rs = Buf()
        n.hT = [tile([8, TT], BF16), tile([8, TT], BF16)]; n.b_hT = [Buf(), Buf()]
        return n

    def norm_tile(n, xsrc, tt, gidx, want_x=False):
        s = tt % 2
        xt, bx = n.xt[s], n.b_xt[s]
        P.dma("sp", lambda e: e.dma_start(out=xt, in_=xsrc.rearrange("(kc p) t -> p kc t", p=128)[:, :, tsl(tt)]), writes=[bx])
        P.op("act", lambda e: e.activation(out=n.sq, in_=xt, func=AF.Square), reads=[bx], writes=[n.b_sq])
        ps, pb = kb.bank()

        def f(e):
            for kc in range(8):
                ins = e.matmul(ps, lhsT=ones_f, rhs=n.sq[:, kc, :], start=(kc == 0), stop=(kc == 7))
            return ins
        P.op("pe", f, reads=[n.b_sq, b_ones_f], writes=[pb])
        P.op("act", lambda e: e.activation(out=n.rs, in_=ps, func=AF.Sqrt, scale=1.0 / D, bias=epsc[:, 0:1]), reads=[pb, b_eps], writes=[n.b_rs])
        recip(P, n.rs, n.rs, [n.b_rs], [n.b_rs])
        hT, bh = n.hT[s], n.b_hT[s]
        for kc in range(8):
            P.op("dve", lambda e, kc=kc: e.scalar_tensor_tensor(out=hT[:, kc, :], in0=xt[:, kc, :], scalar=gv[:, gidx, kc:kc + 1], in1=n.rs, op0=ALU.mult, op1=ALU.mult),
                 reads=[bx, b_gv, n.b_rs], writes=[bh], partial=(kc > 0))
        return hT, bh, xt, bx

    def proj(hT, bh, w_sb, b_w, c0, M, n0=0, nn=TT):
        ps, pb = kb.bank()

        def f(e):
            for kc in range(8):
                ins = e.matmul(ps[0:M, 0:nn], lhsT=w_sb[:, kc, c0:c0 + M], rhs=hT[:, kc, n0:n0 + nn], start=(kc == 0), stop=(kc == 7))
            return ins
        P.op("pe", f, reads=[bh, b_w], writes=[pb])
        return ps, pb

    def proj_tok(hT, bh, w_sb, b_w, c0, N, tb):
        ps, pb = kb.bank()

        def f(e):
            for kc in range(8):
                ins = e.matmul(ps[:, 0:N], lhsT=hT[:, kc, tb * 128:(tb + 1) * 128], rhs=w_sb[:, kc, c0:c0 + N], start=(kc == 0), stop=(kc == 7))
            return ins
        P.op("pe", f, reads=[bh, b_w], writes=[pb])
        return ps, pb

    class RopeCtx:
        pass

    def rope_setup():
        r = RopeCtx()
        r.t1 = tile([TT]); r.b1 = Buf()
        r.t2 = tile([TT]); r.b2 = Buf()
        return r

    def rope(r, psA, pbA, psB, pbB, M, cos, sin, b_cs, o1, o2, b_o, partial=True):
        A, B = psA[0:M, :], psB[0:M, :]
        C, S_ = cos[0:M, :], sin[0:M, :]
        t1, t2 = r.t1[0:M, :], r.t2[0:M, :]
        P.op("dve", lambda e: e.tensor_tensor(out=t1, in0=A, in1=C, op=ALU.mult), reads=[pbA, b_cs], writes=[r.b1])
        P.op("dve", lambda e: e.tensor_tensor(out=t2, in0=B, in1=S_, op=ALU.mult), reads=[pbB, b_cs], writes=[r.b2])
        P.op("dve", lambda e: e.tensor_tensor(out=o1, in0=t1, in1=t2, op=ALU.subtract), reads=[r.b1, r.b2], writes=[b_o], partial=partial)
        P.op("dve", lambda e: e.tensor_tensor(out=t1, in0=B, in1=C, op=ALU.mult), reads=[pbB, b_cs], writes=[r.b1])
        P.op("dve", lambda e: e.tensor_tensor(out=t2, in0=A, in1=S_, op=ALU.mult), reads=[pbA, b_cs], writes=[r.b2])
        P.op("dve", lambda e: e.tensor_tensor(out=o2, in0=t1, in1=t2, op=ALU.add), reads=[r.b1, r.b2], writes=[b_o], partial=partial)

    def load_cs(k, tt, cs, b_cs, rows=128):
        P.dma("sp", lambda e: e.dma_start(out=cs[0][0:rows, :], in_=ROPE[k][0][0:rows, tsl(tt)]), writes=[b_cs])
        P.dma("sp", lambda e: e.dma_start(out=cs[1][0:rows, :], in_=ROPE[k][1][0:rows, tsl(tt)]), writes=[b_cs], partial=True)

    def evac(ps, pb, M, out, b_out, partial=True, eng="act", nn=TT):
        if eng == "act":
            P.op("act", lambda e: e.activation(out=out, in_=ps[0:M, 0:nn], func=AF.Copy), reads=[pb], writes=[b_out], partial=partial)
        else:
            P.op("dve", lambda e: e.tensor_copy(out=out, in_=ps[0:M, 0:nn]), reads=[pb], writes=[b_out], partial=partial)

    def phase_out(i, xsrc, xdst):
        mark = kb.off
        wm_sb = tile([8, 512], BF16); b_wm = Buf()
        load_w(wmem_d[i], 0, 512, wm_sb, b_wm)
        wo_sb = tile([10, D], BF16); b_wo = Buf()
        load_w(w_out_d[i], 0, D, wo_sb, b_wo)
        kTm = tile([2, 256], BF16); b_kTm = Buf()
        VM = tile([2, 4, 192], BF16); b_VM = Buf()
        P.op("dve", lambda e: e.memset(VM, 0.0), writes=[b_VM])
        for cc in range(2):
            ps, pb = kb.bank()

            def f(e, ps=ps, cc=cc):
                for kc in range(8):
                    ins = e.matmul(ps[:, 0:256], lhsT=wm_sb[:, kc, cc * 128:(cc + 1) * 128], rhs=memn[:, kc, :], start=(kc == 0), stop=(kc == 7))
                return ins
            P.op("pe", f, reads=[b_wm, b_memn], writes=[pb])
            evac(ps, pb, 128, kTm[:, cc, :], b_kTm, nn=256)
        for mb in range(2):
            ps, pb = kb.bank()

            def f(e, ps=ps, mb=mb):
                for kc in range(8):
                    ins = e.matmul(ps[:, 0:256], lhsT=memn[:, kc, mb * 128:(mb + 1) * 128], rhs=wm_sb[:, kc, 256:512], start=(kc == 0), stop=(kc == 7))
                return ins
            P.op("pe", f, reads=[b_wm, b_memn], writes=[pb])
            P.op("act", lambda e, ps=ps, mb=mb: e.activation(out=VM[:, mb, :, 64:128], in_=ps[:, 0:256].rearrange("p (h c) -> p h c", c=64), func=AF.Copy),
                 reads=[pb], writes=[b_VM], partial=True)
        qm = [tile([2, TT], BF16), tile([2, TT], BF16)]; b_qm = [Buf(), Buf()]
        mix = [tile([8, TT], BF16), tile([8, TT], BF16)]; b_mix = [Buf(), Buf()]
        xt = [tile([8, TT]), tile([8, TT])]; b_xt = [Buf(), Buf()]
        memo = tile([2, TT], BF16); b_memo = Buf()
        PT = [tile([TT], BF16) for _ in range(4)]; b_PT = [Buf() for _ in range(4)]
        rL = tile([TT]); b_rL = Buf()
        pti = 0
        for tt in range(NTT):
            s = tt % 2
            P.dma("sp", lambda e, s=s, tt=tt: e.dma_start(out=qm[s], in_=QMd.rearrange("(c p) t -> p c t", p=128)[:, :, tsl(tt)]), writes=[b_qm[s]])
            P.dma("sp", lambda e, s=s, tt=tt: e.dma_start(out=mix[s], in_=MIXd.rearrange("(c p) t -> p c t", p=128)[:, :, tsl(tt)]), writes=[b_mix[s]])
            P.dma("sp", lambda e, s=s, tt=tt: e.dma_start(out=xt[s], in_=xsrc.rearrange("(c p) t -> p c t", p=128)[:, :, tsl(tt)]), writes=[b_xt[s]])
            for cc in range(2):
                psU, pbU = kb.bank()
                psL, pbL = kb.bank()
                first = True
                for hh in range(2):
                    head = 2 * cc + hh
                    rs_ = slice(64 * hh, 64 * hh + 64)
                    vs_ = slice(64, 192) if hh == 0 else slice(0, 128)
                    for mb in range(2):
                        ps, pb = kb.bank()
                        P.op("pe", lambda e, ps=ps, rs_=rs_, cc=cc, mb=mb, s=s: e.matmul(ps, lhsT=kTm[rs_, cc, mb * 128:(mb + 1) * 128], rhs=qm[s][rs_, cc, :], start=True, stop=True),
                             reads=[b_kTm, b_qm[s]], writes=[pb])
                        pt, bpt = PT[pti % 4], b_PT[pti % 4]
                        pti += 1
                        P.op("act", lambda e, ps=ps, pt=pt: e.activation(out=pt, in_=ps, func=AF.Exp, scale=0.125), reads=[pb], writes=[bpt])
                        last = (hh == 1 and mb == 1)

                        def f(e, pt=pt, mb=mb, head=head, vs_=vs_, first=first, last=last, psU=psU, psL=psL):
                            e.matmul(psU, lhsT=VM[:, mb, head, vs_], rhs=pt, start=first, stop=last, skip_group_check=True)
                            return e.matmul(psL, lhsT=onz[:, vs_], rhs=pt, start=first, stop=last, skip_group_check=True)
                        P.op("pe", f, reads=[bpt, b_VM, b_onz], writes=[pbU, pbL], partial=not first)
                        first = False
                recip(P, rL, psL, [pbL], [b_rL])
                P.op("dve", lambda e, psU=psU, cc=cc: e.tensor_tensor(out=memo[:, cc, :], in0=psU, in1=rL, op=ALU.mult), reads=[pbU, b_rL], writes=[b_memo], partial=(cc > 0))
            for m in range(8):
                ps, pb = kb.bank()

                def f(e, ps=ps, m=m, s=s):
                    for c in range(8):
                        e.matmul(ps, lhsT=wo_sb[:, c, m * 128:(m + 1) * 128], rhs=mix[s][:, c, :], start=(c == 0), stop=False)
                    e.matmul(ps, lhsT=wo_sb[:, 8, m * 128:(m + 1) * 128], rhs=memo[:, 0, :], start=False, stop=False)
                    return e.matmul(ps, lhsT=wo_sb[:, 9, m * 128:(m + 1) * 128], rhs=memo[:, 1, :], start=False, stop=True)
                P.op("pe", f, reads=[b_wo, b_mix[s], b_memo], writes=[pb])
                P.op("dve", lambda e, ps=ps, m=m, s=s: e.tensor_tensor(out=xt[s][:, m, :], in0=xt[s][:, m, :], in1=ps, op=ALU.add), reads=[pb, b_xt[s]], writes=[b_xt[s]], partial=True)
            P.dma("sp", lambda e, s=s, tt=tt: e.dma_start(out=xdst.rearrange("(c p) t -> p c t", p=128)[:, :, tsl(tt)], in_=xt[s]), reads=[b_xt[s]])
        kb.phase_end(mark)

    def phase_ffn(i, xsrc, xdst):
        mark = kb.off
        ST = 2048
        NS = ST // TT
        hA = tile([8, ST], BF16); b_hA = [Buf() for _ in range(NS)]
        g = tile([22, ST], BF16); b_g = [Buf() for _ in range(NS)]
        xt = tile([8, TT]); b_xt = Buf()
        sq = tile([8, TT], BF16); b_sq = Buf()
        rs = tile([TT]); b_rs = Buf()
        wa = [tile([8, 256], BF16), tile([8, 256], BF16)]; b_wa = [Buf(), Buf()]
        wb = [tile([8, 256], BF16), tile([8, 256], BF16)]; b_wb = [Buf(), Buf()]
        wdn = [tile([22, 128], BF16), tile([22, 128], BF16)]; b_wdn = [Buf(), Buf()]
        cw = tile([22, 3]); b_cw = Buf()
        cb = tile([22]); b_cb = Buf()
        halo = tile([22, 2]); b_halo = Buf()
        a_sb = [tile([TT + 2]), tile([TT + 2])]; b_a = [Buf(), Buf()]
        cv = [tile([TT]), tile([TT])]; b_cv = [Buf(), Buf()]
        sg = [tile([TT]), tile([TT])]; b_sg = [Buf(), Buf()]
        xr = [tile([TT]), tile([TT])]; b_xr = [Buf(), Buf()]
        P.dma("sp", lambda e: e.dma_start(out=cw, in_=convw_d[i]), writes=[b_cw])
        P.dma("sp", lambda e: e.dma_start(out=cb, in_=convb_d[i]), writes=[b_cb])
        P.op("dve", lambda e: e.memset(halo, 0.0), writes=[b_halo])
        wupr = w_up_d[i].rearrange("(kc p) m -> p kc m", p=128)
        wdnr = w_down_d[i].rearrange("(c p) m -> p c m", p=128)
        gi = 4 + i
        ai = 0
        for st in range(T // ST):
            for tl in range(NS):
                tt = st * NS + tl
                P.dma("sp", lambda e, tt=tt: e.dma_start(out=xt, in_=xsrc.rearrange("(kc p) t -> p kc t", p=128)[:, :, tsl(tt)]), writes=[b_xt])
                P.op("act", lambda e: e.activation(out=sq, in_=xt, func=AF.Square), reads=[b_xt], writes=[b_sq])
                ps, pb = kb.bank()

                def f(e, ps=ps):
                    for kc in range(8):
                        ins = e.matmul(ps, lhsT=ones_b, rhs=sq[:, kc, :], start=(kc == 0), stop=(kc == 7))
                    return ins
                P.op("pe", f, reads=[b_sq, b_ones_b], writes=[pb])
                P.op("act", lambda e, ps=ps: e.activation(out=rs, in_=ps, func=AF.Sqrt, scale=1.0 / D, bias=epsc[:, 0:1]), reads=[pb, b_eps], writes=[b_rs])
                recip(P, rs, rs, [b_rs], [b_rs])
                for kc in range(8):
                    P.op("dve", lambda e, kc=kc, tl=tl: e.scalar_tensor_tensor(out=hA[:, kc, tl * TT:(tl + 1) * TT], in0=xt[:, kc, :], scalar=gv[:, gi, kc:kc + 1], in1=rs, op0=ALU.mult, op1=ALU.mult),
                         reads=[b_xt, b_gv, b_rs], writes=[b_hA[tl]], partial=(kc > 0))
            for pc in range(11):
                s = pc % 2
                P.dma("pool", lambda e, s=s, pc=pc: e.dma_start(out=wa[s], in_=wupr[:, :, pc * 256:(pc + 1) * 256]), writes=[b_wa[s]])
                P.dma("pool", lambda e, s=s, pc=pc: e.dma_start(out=wb[s], in_=wupr[:, :, 2816 + pc * 256:2816 + (pc + 1) * 256]), writes=[b_wb[s]])
                for cl in range(2):
                    c = pc * 2 + cl
                    for tl in range(NS):
                        tt = st * NS + tl
                        psa, pba = kb.bank()
                        psb, pbb = kb.bank()

                        def f(e, psa=psa, psb=psb, s=s, cl=cl, tl=tl):
                            for kc in range(8):
                                e.matmul(psa, lhsT=wa[s][:, kc, cl * 128:(cl + 1) * 128], rhs=hA[:, kc, tl * TT:(tl + 1) * TT], start=(kc == 0), stop=(kc == 7))
                            for kc in range(8):
                                ins = e.matmul(psb, lhsT=wb[s][:, kc, cl * 128:(cl + 1) * 128], rhs=hA[:, kc, tl * TT:(tl + 1) * TT], start=(kc == 0), stop=(kc == 7))
                            return ins
                        P.op("pe", f, reads=[b_wa[s], b_wb[s], b_hA[tl]], writes=[pba, pbb])
                        a_, ba_ = a_sb[ai % 2], b_a[ai % 2]
                        cv_, bcv_ = cv[ai % 2], b_cv[ai % 2]
                        sg_, bsg_ = sg[ai % 2], b_sg[ai % 2]
                        ai += 1
                        P.op("act", lambda e, a_=a_, psa=psa: e.activation(out=a_[:, 2:TT + 2], in_=psa, func=AF.Copy), reads=[pba], writes=[ba_])
                        P.op("dve", lambda e, a_=a_, c=c: e.tensor_copy(out=a_[:, 0:2], in_=halo[:, c, :]), reads=[b_halo], writes=[ba_], partial=True)
                        P.op("dve", lambda e, a_=a_, cv_=cv_, c=c: e.tensor_scalar(out=cv_, in0=a_[:, 2:TT + 2], scalar1=cw[:, c, 2:3], scalar2=cb[:, c:c + 1], op0=ALU.mult, op1=ALU.add),
                             reads=[ba_, b_cw, b_cb], writes=[bcv_])
                        P.op("dve", lambda e, a_=a_, cv_=cv_, c=c: e.scalar_tensor_tensor(out=cv_, in0=a_[:, 1:TT + 1], scalar=cw[:, c, 1:2], in1=cv_, op0=ALU.mult, op1=ALU.add),
                             reads=[ba_, b_cw, bcv_], writes=[bcv_])
                        P.op("dve", lambda e, a_=a_, cv_=cv_, c=c: e.scalar_tensor_tensor(out=cv_, in0=a_[:, 0:TT], scalar=cw[:, c, 0:1], in1=cv_, op0=ALU.mult, op1=ALU.add),
                             reads=[ba_, b_cw, bcv_], writes=[bcv_])
                        P.op("dve", lambda e, a_=a_, c=c: e.tensor_copy(out=halo[:, c, :], in_=a_[:, TT:TT + 2]), reads=[ba_], writes=[b_halo], partial=True)
                        P.op("act", lambda e, cv_=cv_, sg_=sg_: e.activation(out=sg_, in_=cv_, func=AF.Silu), reads=[bcv_], writes=[bsg_])
                        P.op("dve", lambda e, sg_=sg_, psb=psb, c=c, tl=tl: e.tensor_tensor(out=g[:, c, tl * TT:(tl + 1) * TT], in0=sg_, in1=psb, op=ALU.mult),
                             reads=[bsg_, pbb], writes=[b_g[tl]], partial=True)
            xi = 0
            for m in range(8):
                s = m % 2
                P.dma("pool", lambda e, s=s, m=m: e.dma_start(out=wdn[s], in_=wdnr[:, :, m * 128:(m + 1) * 128]), writes=[b_wdn[s]])
                for tl in range(NS):
                    tt = st * NS + tl
                    xr_, bxr_ = xr[xi % 2], b_xr[xi % 2]
                    xi += 1
                    P.dma("sp", lambda e, xr_=xr_, m=m, tt=tt: e.dma_start(out=xr_, in_=xsrc[m * 128:(m + 1) * 128, tsl(tt)]), writes=[bxr_])
                    ps, pb = kb.bank()

                    def f(e, ps=ps, s=s, tl=tl):
                        for c in range(22):
                            ins = e.matmul(ps, lhsT=wdn[s][:, c, :], rhs=g[:, c, tl * TT:(tl + 1) * TT], start=(c == 0), stop=(c == 21))
                        return ins
                    P.op("pe", f, reads=[b_wdn[s], b_g[tl]], writes=[pb])
                    P.op("dve", lambda e, xr_=xr_, ps=ps: e.tensor_tensor(out=xr_, in0=xr_, in1=ps, op=ALU.add), reads=[pb, bxr_], writes=[bxr_])
                    P.dma("sp", lambda e, xr_=xr_, m=m, tt=tt: e.dma_start(out=xdst[m * 128:(m + 1) * 128, tsl(tt)], in_=xr_), reads=[bxr_])
        kb.phase_end(mark)

    def phase_final(xsrc):
        mark = kb.off
        n = norm_setup()
        ot = [tile([8, TT]), tile([8, TT])]; b_ot = [Buf(), Buf()]
        for tt in range(NTT):
            s = tt % 2
            xt, bx = n.xt[s], n.b_xt[s]
            P.dma("sp", lambda e, xt=xt, tt=tt: e.dma_start(out=xt, in_=xsrc.rearrange("(kc p) t -> p kc t", p=128)[:, :, tsl(tt)]), writes=[bx])
            P.op("act", lambda e, xt=xt: e.activation(out=n.sq, in_=xt, func=AF.Square), reads=[bx], writes=[n.b_sq])
            ps, pb = kb.bank()

            def f(e, ps=ps):
                for kc in range(8):
                    ins = e.matmul(ps, lhsT=ones_f, rhs=n.sq[:, kc, :], start=(kc == 0), stop=(kc == 7))
                return ins
            P.op("pe", f, reads=[n.b_sq, b_ones_f], writes=[pb])
            P.op("act", lambda e, ps=ps: e.activation(out=n.rs, in_=ps, func=AF.Sqrt, scale=1.0 / D, bias=epsc[:, 0:1]), reads=[pb, b_eps], writes=[n.b_rs])
            recip(P, n.rs, n.rs, [n.b_rs], [n.b_rs])
            for kc in range(8):
                P.op("dve", lambda e, kc=kc, xt=xt, s=s: e.scalar_tensor_tensor(out=ot[s][:, kc, :], in0=xt[:, kc, :], scalar=gv[:, 9, kc:kc + 1], in1=n.rs, op0=ALU.mult, op1=ALU.mult),
                     reads=[bx, b_gv, n.b_rs], writes=[b_ot[s]], partial=(kc > 0))
            P.dma("sp", lambda e, s=s, tt=tt: e.dma_start(out=out_d.rearrange("(kc p) t -> p kc t", p=128)[:, :, tsl(tt)], in_=ot[s]), reads=[b_ot[s]])
        kb.phase_end(mark)

    def p1_b(i, xsrc):
        mark = kb.off
        cols, G = PERMS[1]
        NC = len(cols)
        w_sb = tile([8, NC], BF16); b_w = Buf()
        load_w(w_in_d[i], 0, NC, w_sb, b_w)
        n = norm_setup()
        r = rope_setup()
        csq = [tile([TT]), tile([TT])]; b_csq = Buf()
        csk = [tile([TT]), tile([TT])]; b_csk = Buf()
        qst = [tile([8, TT], BF16), tile([8, TT], BF16)]; b_qst = [Buf(), Buf()]
        kst = [tile([4, TT], BF16), tile([4, TT], BF16)]; b_kst = [Buf(), Buf()]
        qmst = [tile([2, TT], BF16), tile([2, TT], BF16)]; b_qmst = [Buf(), Buf()]
        vst = [tile([4, 256], BF16), tile([4, 256], BF16)]; b_vst = [Buf(), Buf()]
        for tt in range(NTT):
            s = tt % 2
            hT, bh, _, _ = norm_tile(n, xsrc, tt, i)
            load_cs(2, tt, csq, b_csq)
            load_cs(4, tt, csk, b_csk, rows=32)
            pA, bA = proj(hT, bh, w_sb, b_w, G["QX1"][0], 128)
            pB, bB = proj(hT, bh, w_sb, b_w, G["QX2"][0], 128)
            rope(r, pA, bA, pB, bB, 128, csq[0], csq[1], b_csq, qst[s][:, 0, :], qst[s][:, 1, :], b_qst[s])
            for c in range(6):
                ps, pb = proj(hT, bh, w_sb, b_w, G["QN%d" % c][0], 128)
                evac(ps, pb, 128, qst[s][:, 2 + c, :], b_qst[s], eng=("act" if c % 2 == 0 else "dve"))
            P.dma("sp", lambda e, s=s, tt=tt: e.dma_start(out=Qd.rearrange("(c p) t -> p c t", p=128)[:, :, tsl(tt)], in_=qst[s]), reads=[b_qst[s]])
            pA, bA = proj(hT, bh, w_sb, b_w, G["KX1"][0], 32)
            pB, bB = proj(hT, bh, w_sb, b_w, G["KX2"][0], 32)
            rope(r, pA, bA, pB, bB, 32, csk[0], csk[1], b_csk, kst[s][0:32, 0, :], kst[s][0:32, 1, :], b_kst[s])
            ps, pb = proj(hT, bh, w_sb, b_w, G["KN0"][0], 128)
            evac(ps, pb, 128, kst[s][:, 2, :], b_kst[s])
            ps, pb = proj(hT, bh, w_sb, b_w, G["KN1"][0], 64)
            evac(ps, pb, 64, kst[s][0:64, 3, :], b_kst[s], eng="dve")
            for (r0, nr, ci) in ((0, 32, 0), (32, 32, 1), (64, 128, 2), (192, 64, 3)):
                P.dma("sp", lambda e, s=s, tt=tt, r0=r0, nr=nr, ci=ci: e.dma_start(out=Kd[r0:r0 + nr, tsl(tt)], in_=kst[s][0:nr, ci, :]), reads=[b_kst[s]])
            for c in range(2):
                ps, pb = proj(hT, bh, w_sb, b_w, G["QM%d" % c][0], 128)
                evac(ps, pb, 128, qmst[s][:, c, :], b_qmst[s], eng=("act" if c == 0 else "dve"))
            P.dma("sp", lambda e, s=s, tt=tt: e.dma_start(out=QMd.rearrange("(c p) t -> p c t", p=128)[:, :, tsl(tt)], in_=qmst[s]), reads=[b_qmst[s]])
            for tb_ in range(4):
                ps, pb = proj_tok(hT, bh, w_sb, b_w, G["V"][0], 256, tb_)
                evac(ps, pb, 128, vst[s][:, tb_, :], b_vst[s], nn=256, eng=("act" if tb_ % 2 == 0 else "dve"))
            P.dma("sp", lambda e, s=s, tt=tt: e.dma_start(out=Vd[:, 0:256].rearrange("(n p) c -> p n c", p=128)[:, tt * 4:tt * 4 + 4, :], in_=vst[s]), reads=[b_vst[s]])
        kb.phase_end(mark)

    def p2_b(i):
        mark = kb.off
        K_sb = tile([4, T], BF16, parts=64); b_K = Buf()
        P.dma("sp", lambda e: e.dma_start(out=K_sb, in_=Kd[0:256, :].rearrange("(d h) t -> d h t", h=4)), writes=[b_K])
        VZ = tile([32, 4, 192], BF16); b_VZ = Buf()
        P.op("dve", lambda e: e.memset(VZ, 0.0), writes=[b_VZ])
        for g in range(4):
            P.dma("sp", lambda e, g=g: e.dma_start(out=VZ[:, :, g, 64:128], in_=Vd[:, g * 64:(g + 1) * 64].rearrange("(n p) c -> p n c", p=128)), writes=[b_VZ], partial=True)
        esk = tile([8]); b_esk = Buf()
        P.dma("sp", lambda e: e.dma_start(out=esk, in_=sinks_d), writes=[b_esk])
        P.op("act", lambda e: e.activation(out=esk, in_=esk, func=AF.Exp), reads=[b_esk], writes=[b_esk])
        Q_sb = [tile([16, TT], BF16, parts=64), tile([16, TT], BF16, parts=64)]; b_Q = [Buf(), Buf()]
        mixst = [tile([8, TT], BF16), tile([8, TT], BF16)]; b_mixst = [Buf(), Buf()]
        NPT = 10
        PT = [tile([TT], BF16) for _ in range(NPT)]; b_PT = [Buf() for _ in range(NPT)]
        Ls = tile([TT]); b_Ls = Buf()
        pti = 0
        for tt in range(NTT):
            s = tt % 2
            P.dma("sp", lambda e, s=s, tt=tt: e.dma_start(out=Q_sb[s], in_=Qd.rearrange("(d h) t -> d h t", h=16)[:, :, tsl(tt)]), writes=[b_Q[s]])
            for g in range(4):
                pts = {}
                for qb in range(4):
                    nb = tt * 4 + qb
                    for w_, kb_ in enumerate((nb - 1, nb)):
                        if kb_ < 0:
                            continue
                        kind = 1 if w_ == 0 else 0
                        ps, pb = kb.bank()

                        def f(e, ps=ps, kind=kind, g=g, kb_=kb_, qb=qb, s=s):
                            e.matmul(ps, lhsT=ident, rhs=maskb[:, kind, :], start=True, stop=False)
                            return e.matmul(ps, lhsT=K_sb[:, g, kb_ * 128:(kb_ + 1) * 128], rhs=Q_sb[s][:, 4 * g:4 * g + 4, qb * 128:(qb + 1) * 128], start=False, stop=True)
                        P.op("pe", f, reads=[b_ident, b_maskb, b_K, b_Q[s]], writes=[pb])
                        pt, bpt = PT[pti % NPT], b_PT[pti % NPT]
                        pti += 1
                        P.op("act", lambda e, ps=ps, pt=pt: e.activation(out=pt, in_=ps, func=AF.Exp, scale=0.125), reads=[pb], writes=[bpt])
                        pts[(qb, w_)] = (pt, bpt, kb_)
                for cc in range(2):
                    psU, pbU = kb.bank()
                    psL, pbL = kb.bank()
                    items = sorted(pts.items())

                    def f(e, items=items, cc=cc, g=g, psU=psU, psL=psL):
                        first = True
                        for (qb, w_), (pt, bpt, kb_) in items:
                            qs = slice(qb * 128, (qb + 1) * 128)
                            for hh in range(2):
                                hq = 2 * cc + hh
                                vs_ = slice(64, 192) if hh == 0 else slice(0, 128)
                                e.matmul(psU[:, qs], lhsT=VZ[:, kb_, g, vs_], rhs=pt[:, hq * 128:(hq + 1) * 128], start=first, stop=False, skip_group_check=True)
                                first = False
                        first = True
                        for (qb, w_), (pt, bpt, kb_) in items:
                            qs = slice(qb * 128, (qb + 1) * 128)
                            for hh in range(2):
                                hq = 2 * cc + hh
                                vs_ = slice(64, 192) if hh == 0 else slice(0, 128)
                                ins = e.matmul(psL[:, qs], lhsT=onz[:, vs_], rhs=pt[:, hq * 128:(hq + 1) * 128], start=first, stop=False, skip_group_check=True)
                                first = False
                        return ins
                    P.op("pe", f, reads=[v[1] for v in pts.values()] + [b_VZ, b_onz], writes=[pbU, pbL])
                    ch = 2 * g + cc
                    P.op("dve", lambda e, psL=psL, ch=ch: e.tensor_scalar(out=Ls, in0=psL, scalar1=esk[:, ch:ch + 1], scalar2=None, op0=ALU.add), reads=[pbL, b_esk], writes=[b_Ls])
                    recip(P, Ls, Ls, [b_Ls], [b_Ls])
                    P.op("dve", lambda e, psU=psU, ch=ch, s=s: e.tensor_tensor(out=mixst[s][:, ch, :], in0=psU, in1=Ls, op=ALU.mult), reads=[pbU, b_Ls], writes=[b_mixst[s]], partial=True)
            P.dma("sp", lambda e, s=s, tt=tt: e.dma_start(out=MIXd.rearrange("(c p) t -> p c t", p=128)[:, :, tsl(tt)], in_=mixst[s]), reads=[b_mixst[s]])
        kb.phase_end(mark)

    def p1_c(i, xsrc):
        mark = kb.off
        cols, G = PERMS[2]
        NC = len(cols)
        w_sb = tile([8, NC], BF16); b_w = Buf()
        load_w(w_in_d[i], 0, NC, w_sb, b_w)
        n = norm_setup()
        r = rope_setup()
        cs = [tile([TT]), tile([TT])]; b_cs = Buf()
        qst = [tile([8, TT], BF16), tile([8, TT], BF16)]; b_qst = [Buf(), Buf()]
        kst = [tile([8, TT], BF16), tile([8, TT], BF16)]; b_kst = [Buf(), Buf()]
        qmst = [tile([2, TT], BF16), tile([2, TT], BF16)]; b_qmst = [Buf(), Buf()]
        vst = [tile([4, 512], BF16), tile([4, 512], BF16)]; b_vst = [Buf(), Buf()]
        vi = 0
        for tt in range(NTT):
            s = tt % 2
            hT, bh, _, _ = norm_tile(n, xsrc, tt, i)
            load_cs(0, tt, cs, b_cs)
            for pre, st_, bst_, dst in (("Q", qst, b_qst, Qd), ("K", kst, b_kst, Kd)):
                pA, bA = proj(hT, bh, w_sb, b_w, G[pre + "X1"][0], 128)
                pB, bB = proj(hT, bh, w_sb, b_w, G[pre + "X2"][0], 128)
                rope(r, pA, bA, pB, bB, 128, cs[0], cs[1], b_cs, st_[s][:, 0, :], st_[s][:, 1, :], bst_[s])
                for c in range(6):
                    ps, pb = proj(hT, bh, w_sb, b_w, G[pre + "N%d" % c][0], 128)
                    evac(ps, pb, 128, st_[s][:, 2 + c, :], bst_[s], eng=("act" if c % 2 == 0 else "dve"))
                P.dma("sp", lambda e, s=s, tt=tt, st_=st_, dst=dst: e.dma_start(out=dst.rearrange("(c p) t -> p c t", p=128)[:, :, tsl(tt)], in_=st_[s]), reads=[bst_[s]])
            for c in range(2):
                ps, pb = proj(hT, bh, w_sb, b_w, G["QM%d" % c][0], 128)
                evac(ps, pb, 128, qmst[s][:, c, :], b_qmst[s], eng=("act" if c == 0 else "dve"))
            P.dma("sp", lambda e, s=s, tt=tt: e.dma_start(out=QMd.rearrange("(c p) t -> p c t", p=128)[:, :, tsl(tt)], in_=qmst[s]), reads=[b_qmst[s]])
            for half in range(2):
                v_, bv_ = vst[vi % 2], b_vst[vi % 2]
                vi += 1
                for tb_ in range(4):
                    ps, pb = proj_tok(hT, bh, w_sb, b_w, G["V"][0] + half * 512, 512, tb_)
                    evac(ps, pb, 128, v_[:, tb_, :], bv_, nn=512, eng=("act" if tb_ % 2 == 0 else "dve"))
                P.dma("sp", lambda e, v_=v_, tt=tt, half=half: e.dma_start(out=Vd[:, half * 512:(half + 1) * 512].rearrange("(n p) c -> p n c", p=128)[:, tt * 4:tt * 4 + 4, :], in_=v_), reads=[bv_])
        kb.phase_end(mark)

    def p2_c(i):
        mark = kb.off
        maskPD = tile([512], BF16); b_mPD = Buf()
        for j in range(4):
            P.op("dve", lambda e, j=j: e.tensor_copy(out=maskPD[:, j * 128:(j + 1) * 128], in_=maskb[:, (2 if j % 2 == 0 else 0), 0:128]), reads=[b_maskb], writes=[b_mPD], partial=True)
        QT = [tile([T], BF16), tile([T], BF16)]; b_QT = [Buf(), Buf()]
        KT = [tile([T], BF16), tile([T], BF16)]; b_KT = [Buf(), Buf()]
        VD = [tile([32, 128], BF16), tile([32, 128], BF16)]; b_VD = [Buf(), Buf()]
        Uacc = tile([T]); b_U = Buf()
        Lacc = tile([T]); b_L = Buf()
        Ob = [tile([T], BF16), tile([T], BF16)]; b_Ob = [Buf(), Buf()]
        NPT = 6
        PT = [tile([512], BF16) for _ in range(NPT)]; b_PT = [Buf() for _ in range(NPT)]
        pti = 0
        vdi = 0
        SC = float(128 ** -0.5)
        for h in range(8):
            s = h % 2
            P.dma("sp", lambda e, s=s, h=h: e.dma_start(out=QT[s], in_=Qd.rearrange("(d h) t -> d h t", h=8)[:, h, :]), writes=[b_QT[s]])
            P.dma("sp", lambda e, s=s, h=h: e.dma_start(out=KT[s], in_=Kd.rearrange("(d h) t -> d h t", h=8)[:, h, :]), writes=[b_KT[s]])
            for dl in (1, 4, 16):
                nblk = 32 // dl
                vd, bvd = VD[vdi % 2], b_VD[vdi % 2]
                vdi += 1
                vsrc = Vd[:, h * 128:(h + 1) * 128].rearrange("(n p r) c -> p r n c", p=128, r=dl)
                for r in range(dl):
                    P.dma("sp", lambda e, vd=vd, vsrc=vsrc, r=r, nblk=nblk: e.dma_start(out=vd[:, r * nblk:(r + 1) * nblk, :], in_=vsrc[:, r, :, :]), writes=[bvd], partial=(r > 0))
                gsz = min(4, nblk)
                for r in range(dl):
                    for nq in range(0, nblk, gsz):
                        slots = {}
                        for jb in range(0, gsz, 2):
                            ps, pb = kb.bank()

                            def f(e, ps=ps, jb=jb, nq=nq, r=r, dl=dl, s=s, gsz=gsz):
                                ins = e.matmul(ps, lhsT=ident, rhs=maskPD, start=True, stop=False)
                                for jj in range(2):
                                    nb = nq + jb + jj
                                    if jb + jj >= gsz:
                                        continue
                                    qsl = slice(r + dl * 128 * nb, r + dl * 128 * nb + dl * 127 + 1, dl)
                                    for w_, kbn in enumerate((nb - 1, nb)):
                                        if kbn < 0:
                                            continue
                                        ksl = slice(r + dl * 128 * kbn, r + dl * 128 * kbn + dl * 127 + 1, dl)
                                        c0 = (2 * jj + w_) * 128
                                        ins = e.matmul(ps[:, c0:c0 + 128], lhsT=KT[s][:, ksl], rhs=QT[s][:, qsl], start=False, stop=False, skip_group_check=True)
                                return ins
                            P.op("pe", f, reads=[b_ident, b_mPD, b_KT[s], b_QT[s]], writes=[pb])
                            pt, bpt = PT[pti % NPT], b_PT[pti % NPT]
                            pti += 1
                            P.op("act", lambda e, ps=ps, pt=pt: e.activation(out=pt, in_=ps, func=AF.Exp, scale=SC), reads=[pb], writes=[bpt])
                            for jj in range(2):
                                if jb + jj < gsz:
                                    slots[jb + jj] = (pt, bpt, jj)
                        psU, pbU = kb.bank()
                        psL, pbL = kb.bank()

                        def f(e, slots=slots, nq=nq, r=r, nblk=nblk, vd=vd, psU=psU, psL=psL, gsz=gsz):
                            for (acc, use_v) in ((psU, True), (psL, False)):
                                first = True
                                for j in range(gsz):
                                    pt, bpt, jj = slots[j]
                                    nb = nq + j
                                    for w_, kbn in enumerate((nb - 1, nb)):
                                        if kbn < 0:
                                            continue
                                        c0 = (2 * jj + w_) * 128
                                        lhs = vd[:, r * nblk + kbn, :] if use_v else ones_b
                                        ins = e.matmul(acc[:, j * 128:(j + 1) * 128], lhsT=lhs, rhs=pt[:, c0:c0 + 128], start=first, stop=False, skip_group_check=True)
                                        first = False
                            return ins
                        P.op("pe", f, reads=[v[1] for v in slots.values()] + [bvd, b_ones_b], writes=[pbU, pbL])
                        asl = slice(r + dl * 128 * nq, r + dl * 128 * nq + dl * (gsz * 128 - 1) + 1, dl)
                        W = gsz * 128
                        if dl == 1:
                            P.op("act", lambda e, psU=psU, asl=asl, W=W: e.activation(out=Uacc[:, asl], in_=psU[:, 0:W], func=AF.Copy), reads=[pbU], writes=[b_U], partial=True)
                            P.op("dve", lambda e, psL=psL, asl=asl, W=W: e.tensor_copy(out=Lacc[:, asl], in_=psL[:, 0:W]), reads=[pbL], writes=[b_L], partial=True)
                        else:
                            P.op("dve", lambda e, psU=psU, asl=asl, W=W: e.tensor_tensor(out=Uacc[:, asl], in0=Uacc[:, asl], in1=psU[:, 0:W], op=ALU.add), reads=[pbU, b_U], writes=[b_U], partial=True)
                            P.op("dve", lambda e, psL=psL, asl=asl, W=W: e.tensor_tensor(out=Lacc[:, asl], in0=Lacc[:, asl], in1=psL[:, 0:W], op=ALU.add), reads=[pbL, b_L], writes=[b_L], partial=True)
            recip(P, Lacc, Lacc, [b_L], [b_L])
            P.op("dve", lambda e, s=s: e.tensor_tensor(out=Ob[s], in0=Uacc, in1=Lacc, op=ALU.mult), reads=[b_U, b_L], writes=[b_Ob[s]])
            P.dma("sp", lambda e, s=s, h=h: e.dma_start(out=MIXd[h * 128:(h + 1) * 128, :], in_=Ob[s]), reads=[b_Ob[s]])
        kb.phase_end(mark)

    QLd = kb.dscr("qld", [2048, T])
    QId = kb.dscr("qid", [D, T])
    WId = kb.dscr("wid", [T, 16], F32)

    def p1_a(i, xsrc):
        mark = kb.off
        j = i // 3
        cols, G = PERMS[0]
        NC = len(cols)
        w_sb = tile([8, NC], BF16); b_w = Buf()
        load_w(w_in_d[i], 0, NC, w_sb, b_w)
        wuk_sb = tile([8, 256], BF16, parts=96); b_wuk = Buf()
        P.dma("pool", lambda e: e.dma_start(out=wuk_sb, in_=wuk_d[j].rearrange("h n r -> n h r")), writes=[b_wuk])
        kvn_sb = tile([256]); b_kvn = Buf()
        P.dma("sp", lambda e: e.dma_start(out=kvn_sb, in_=kvn_d[j].partition_broadcast(128)), writes=[b_kvn])
        n = norm_setup()
        r = rope_setup()
        cs = [[tile([TT]), tile([TT])] for _ in range(4)]; b_cs = [Buf() for _ in range(4)]
        qrst = [tile([2, TT], BF16), tile([2, TT], BF16)]; b_qrst = [Buf(), Buf()]
        qn_sb = [tile([TT], BF16, parts=96), tile([TT], BF16, parts=96)]; b_qn = [Buf(), Buf()]
        qlst = [tile([2, TT], BF16), tile([2, TT], BF16)]; b_qlst = [Buf(), Buf()]
        krst = [tile([2, TT], BF16, parts=16), tile([2, TT], BF16, parts=16)]; b_krst = [Buf(), Buf()]
        qist = [tile([8, TT], BF16), tile([8, TT], BF16)]; b_qist = [Buf(), Buf()]
        kist = [tile([3, TT], BF16, parts=48), tile([3, TT], BF16, parts=48)]; b_kist = [Buf(), Buf()]
        qmst = [tile([2, TT], BF16), tile([2, TT], BF16)]; b_qmst = [Buf(), Buf()]
        ckst = [tile([4, 256], BF16), tile([4, 256], BF16)]; b_ckst = [Buf(), Buf()]
        ckT = [tile([2, TT], BF16), tile([2, TT], BF16)]; b_ckT = [Buf(), Buf()]
        wist = [tile([4, 16]), tile([4, 16])]; b_wist = [Buf(), Buf()]
        junk = tile([256]); b_junk = Buf()
        ssq = tile([1]); b_ssq = Buf()
        qi = 0
        for tt in range(NTT):
            s = tt % 2
            hT, bh, _, _ = norm_tile(n, xsrc, tt, i)
            load_cs(0, tt, cs[0], b_cs[0])
            load_cs(1, tt, cs[1], b_cs[1], rows=16)
            load_cs(2, tt, cs[2], b_cs[2])
            load_cs(3, tt, cs[3], b_cs[3], rows=8)
            pA, bA = proj(hT, bh, w_sb, b_w, G["QX1"][0], 128)
            pB, bB = proj(hT, bh, w_sb, b_w, G["QX2"][0], 128)
            rope(r, pA, bA, pB, bB, 128, cs[0][0], cs[0][1], b_cs[0], qrst[s][:, 0, :], qrst[s][:, 1, :], b_qrst[s])
            P.dma("sp", lambda e, s=s, tt=tt: e.dma_start(out=Qd[0:256, :].rearrange("(c p) t -> p c t", p=128)[:, :, tsl(tt)], in_=qrst[s]), reads=[b_qrst[s]])
            for h in range(8):
                ps, pb = proj(hT, bh, w_sb, b_w, G["QN%d" % h][0], 96)
                qn, bqn = qn_sb[qi % 2], b_qn[qi % 2]
                ql, bql = qlst[qi % 2], b_qlst[qi % 2]
                qi += 1
                evac(ps, pb, 96, qn, bqn, partial=False, eng=("act" if h % 2 == 0 else "dve"))
                for rc in range(2):
                    ps2, pb2 = kb.bank()
                    P.op("pe", lambda e, ps2=ps2, h=h, rc=rc, qn=qn: e.matmul(ps2, lhsT=wuk_sb[:, h, rc * 128:(rc + 1) * 128], rhs=qn, start=True, stop=True), reads=[b_wuk, bqn], writes=[pb2])
                    evac(ps2, pb2, 128, ql[:, rc, :], bql, eng=("act" if rc == 0 else "dve"))
                P.dma("sp", lambda e, ql=ql, h=h, tt=tt: e.dma_start(out=QLd[h * 256:(h + 1) * 256, :].rearrange("(c p) t -> p c t", p=128)[:, :, tsl(tt)], in_=ql), reads=[bql])
            pA, bA = proj(hT, bh, w_sb, b_w, G["KR1"][0], 16)
            pB, bB = proj(hT, bh, w_sb, b_w, G["KR2"][0], 16)
            rope(r, pA, bA, pB, bB, 16, cs[1][0], cs[1][1], b_cs[1], krst[s][:, 0, :], krst[s][:, 1, :], b_krst[s])
            for c in range(2):
                P.dma("sp", lambda e, s=s, tt=tt, c=c: e.dma_start(out=Kd[16 * c:16 * c + 16, tsl(tt)], in_=krst[s][:, c, :]), reads=[b_krst[s]])
            pA, bA = proj(hT, bh, w_sb, b_w, G["QIX1"][0], 128)
            pB, bB = proj(hT, bh, w_sb, b_w, G["QIX2"][0], 128)
            rope(r, pA, bA, pB, bB, 128, cs[2][0], cs[2][1], b_cs[2], qist[s][:, 0, :], qist[s][:, 1, :], b_qist[s])
            for c in range(6):
                ps, pb = proj(hT, bh, w_sb, b_w, G["QIN%d" % c][0], 128)
                evac(ps, pb, 128, qist[s][:, 2 + c, :], b_qist[s], eng=("act" if c % 2 == 0 else "dve"))
            P.dma("sp", lambda e, s=s, tt=tt: e.dma_start(out=QId.rearrange("(c p) t -> p c t", p=128)[:, :, tsl(tt)], in_=qist[s]), reads=[b_qist[s]])
            pA, bA = proj(hT, bh, w_sb, b_w, G["KIX1"][0], 8)
            pB, bB = proj(hT, bh, w_sb, b_w, G["KIX2"][0], 8)
            rope(r, pA, bA, pB, bB, 8, cs[3][0], cs[3][1], b_cs[3], kist[s][0:8, 0, :], kist[s][0:8, 1, :], b_kist[s])
            ps, pb = proj(hT, bh, w_sb, b_w, G["KIN"][0], 48)
            evac(ps, pb, 48, kist[s][:, 2, :], b_kist[s])
            for (r0, nr, ci) in ((64, 8, 0), (72, 8, 1), (80, 48, 2)):
                P.dma("sp", lambda e, s=s, tt=tt, r0=r0, nr=nr, ci=ci: e.dma_start(out=Kd[r0:r0 + nr, tsl(tt)], in_=kist[s][0:nr, ci, :]), reads=[b_kist[s]])
            for c in range(2):
                ps, pb = proj(hT, bh, w_sb, b_w, G["QM%d" % c][0], 128)
                evac(ps, pb, 128, qmst[s][:, c, :], b_qmst[s], eng=("act" if c == 0 else "dve"))
            P.dma("sp", lambda e, s=s, tt=tt: e.dma_start(out=QMd.rearrange("(c p) t -> p c t", p=128)[:, :, tsl(tt)], in_=qmst[s]), reads=[b_qmst[s]])
            for tb_ in range(4):
                ps, pb = proj_tok(hT, bh, w_sb, b_w, G["CKV"][0], 256, tb_)
                P.op("act", lambda e, ps=ps: e.activation(out=junk, in_=ps[:, 0:256], func=AF.Square, accum_out=ssq[:, 0:1]), reads=[pb], writes=[b_junk, b_ssq])
                P.op("act", lambda e: e.activation(out=ssq, in_=ssq, func=AF.Sqrt, scale=1.0 / 256, bias=epsc[:, 0:1]), reads=[b_ssq, b_eps], writes=[b_ssq])
                recip(P, ssq, ssq, [b_ssq], [b_ssq])
                P.op("dve", lambda e, ps=ps, s=s, tb_=tb_: e.scalar_tensor_tensor(out=ckst[s][:, tb_, :], in0=ps[:, 0:256], scalar=ssq[:, 0:1], in1=kvn_sb, op0=ALU.mult, op1=ALU.mult),
                     reads=[pb, b_ssq, b_kvn], writes=[b_ckst[s]], partial=True)
            P.dma("sp", lambda e, s=s, tt=tt: e.dma_start(out=Vd[:, 0:256].rearrange("(n p) c -> p n c", p=128)[:, tt * 4:tt * 4 + 4, :], in_=ckst[s]), reads=[b_ckst[s]])
            for rc in range(2):
                ps, pb = kb.bank()
                psb = ps.bitcast(BF16)

                def f(e, psb=psb, rc=rc, s=s):
                    for tb_ in range(4):
                        ins = e.transpose(psb[:, tb_ * 128:(tb_ + 1) * 128], ckst[s][:, tb_, rc * 128:(rc + 1) * 128], ident)
                    return ins
                P.op("pe", f, reads=[b_ckst[s], b_ident], writes=[pb])
                P.op("act", lambda e, psb=psb, rc=rc, s=s: e.activation(out=ckT[s][:, rc, :], in_=psb[:, 0:512], func=AF.Copy), reads=[pb], writes=[b_ckT[s]], partial=True)
            P.dma("sp", lambda e, s=s, tt=tt: e.dma_start(out=Kd[256:512, :].rearrange("(c p) t -> p c t", p=128)[:, :, tsl(tt)], in_=ckT[s]), reads=[b_ckT[s]])
            for tb_ in range(4):
                ps, pb = proj_tok(hT, bh, w_sb, b_w, G["WI"][0], 16, tb_)
                P.op("act", lambda e, ps=ps, s=s, tb_=tb_: e.activation(out=wist[s][:, tb_, :], in_=ps[:, 0:16], func=AF.Copy, scale=1.0 / 32.0), reads=[pb], writes=[b_wist[s]], partial=True)
            P.dma("sp", lambda e, s=s, tt=tt: e.dma_start(out=WId.rearrange("(n p) c -> p n c", p=128)[:, tt * 4:tt * 4 + 4, :], in_=wist[s]), reads=[b_wist[s]])
        kb.phase_end(mark)

    def p2_a(i, NIT=10):
        mark = kb.off
        j = i // 3
        SC = float(128 ** -0.5)
        KI_sb = tile([T], BF16, parts=64); b_KI = Buf()
        P.dma("sp", lambda e: e.dma_start(out=KI_sb, in_=Kd[64:128, :]), writes=[b_KI])
        CKT_sb = tile([2, T], BF16); b_CKT = Buf()
        P.dma("sp", lambda e: e.dma_start(out=CKT_sb, in_=Kd[256:512, :].rearrange("(c p) t -> p c t", p=128)), writes=[b_CKT])
        CK_sb = tile([32, 256], BF16); b_CK = Buf()
        P.dma("sp", lambda e: e.dma_start(out=CK_sb, in_=Vd[:, 0:256].rearrange("(n p) c -> p n c", p=128)), writes=[b_CK])
        KR_sb = tile([T], BF16, parts=32); b_KR = Buf()
        P.dma("sp", lambda e: e.dma_start(out=KR_sb, in_=Kd[0:32, :]), writes=[b_KR])
        sel_sb = tile([16, 128], BF16); b_sel = Buf()
        P.dma("pool", lambda e: e.dma_start(out=sel_sb, in_=sel_d), writes=[b_sel])
        zmask = tile([128]); b_zm = Buf()
        P.dma("sp", lambda e: e.dma_start(out=zmask, in_=zmask_d), writes=[b_zm])
        bsel = tile([16]); b_bsel = Buf()
        P.dma("sp", lambda e: e.dma_start(out=bsel, in_=bsel_d), writes=[b_bsel])
        triq = tile([128]); b_triq = Buf()
        P.dma("sp", lambda e: e.dma_start(out=triq, in_=triq_d), writes=[b_triq])
        wuv_sb = tile([8, 2, 128], BF16); b_wuv = Buf()
        for h in range(8):
            P.dma("pool", lambda e, h=h: e.dma_start(out=wuv_sb[:, h, :, :], in_=wuv_d[j, h].rearrange("(c p) v -> p c v", p=128)), writes=[b_wuv], partial=True)
        id30k = tile([128], BF16); b_id30k = Buf()
        P.op("dve", lambda e: e.tensor_scalar(out=id30k, in0=ident, scalar1=-NEG, scalar2=None, op0=ALU.mult), reads=[b_ident], writes=[b_id30k])
        halfc = tile([1]); b_half = Buf()
        P.op("dve", lambda e: e.memset(halfc, 0.5), writes=[b_half])
        QI_sb = tile([16, TT], BF16, parts=64); b_QI = Buf()
        QI2 = tile([64, 128], BF16, parts=64); b_QI2 = Buf()
        QL_sb = tile([8, 2, TT], BF16); b_QL = Buf()
        QR_sb = tile([8, TT], BF16, parts=32); b_QR = Buf()
        WI_sb = tile([4, 16]); b_WI = Buf()
        S = tile([T]); b_S = Buf()
        mqb = tile([T], BF16); b_mqb = Buf()
        junk, b_junk = mqb, b_mqb
        MT = tile([32, TT], BF16); b_MT = Buf()
        NR = 9
        Rt = [tile([TT], BF16) for _ in range(NR)]; b_Rt = [Buf() for _ in range(NR)]
        NPT = 4
        PT = [tile([TT], BF16) for _ in range(NPT)]; b_PT = [Buf() for _ in range(NPT)]
        olat = tile([2, TT], BF16); b_olat = Buf()
        rL = tile([TT]); b_rL = Buf()
        mixst1 = tile([8, TT], BF16); b_mixst1 = Buf()
        mixst = [mixst1, mixst1]; b_mixst = [b_mixst1, b_mixst1]
        Z = tile([128]); b_Z = Buf()
        wcol = tile([16]); b_wcol = Buf()
        pw2 = tile([NIT]); b_pw2 = Buf()
        for it in range(NIT):
            P.op("dve", lambda e, it=it: e.memset(pw2[:, it:it + 1], float(2.0 ** -(it + 1))), writes=[b_pw2], partial=True)
        Wst = tile([NIT]); b_Wst = Buf()
        lo = tile([1]); b_lo = Buf()
        hi = tile([1]); b_hi = Buf()
        mid = tile([1]); b_mid = Buf()
        cnt = tile([1]); b_cnt = Buf()
        ge = tile([1]); b_ge = Buf()
        dd = tile([1]); b_dd = Buf()
        ri_ = [0]
        pi_ = [0]
        dbank = [0]

        def bankof(lst, ctr):
            k = lst[ctr[0] % len(lst)]
            ctr[0] += 1
            return kb.ps[k], kb.psb[k]
        sacc = [0]
        tbank = [0]
        gcount = [0]
        for tt in range(NTT):
            P.dma("sp", lambda e, tt=tt: e.dma_start(out=QI_sb, in_=QId.rearrange("(d h) t -> d h t", h=16)[:, :, tsl(tt)]), writes=[b_QI])
            if "qi2" not in SKIP:
              P.op("pool", lambda e: e.tensor_copy(out=QI2.rearrange("d j (h q) -> d j h q", q=8), in_=QI_sb.rearrange("d h (j q) -> d j h q", q=8)), reads=[b_QI], writes=[b_QI2])
            for h in range(8):
                P.dma("sp", lambda e, tt=tt, h=h: e.dma_start(out=QL_sb[:, h, :, :], in_=QLd[h * 256:(h + 1) * 256, :].rearrange("(c p) t -> p c t", p=128)[:, :, tsl(tt)]), writes=[b_QL], partial=(h > 0))
            P.dma("sp", lambda e, tt=tt: e.dma_start(out=QR_sb, in_=Qd[0:256, :].rearrange("(i h) t -> i h t", h=8)[:, :, tsl(tt)]), writes=[b_QR])
            P.dma("sp", lambda e, tt=tt: e.dma_start(out=WI_sb, in_=WId.rearrange("(n p) c -> p n c", p=128)[:, tt * 4:tt * 4 + 4, :]), writes=[b_WI])
            P.op("dve", lambda e, tt=tt: e.memset(MT[:, 4 * tt:4 * tt + 4, :], -1.0), writes=[b_MT])
            for qb in range(4):
                if "idx" in SKIP:
                    continue
                nb = 4 * tt + qb
                nk = (nb + 1) * 128
                if "z" not in SKIP:
                    P.op("dve", lambda e, qb=qb: e.tensor_tensor(out=Z.rearrange("p (h q) -> p h q", q=8), in0=WI_sb[:, qb, :].unsqueeze(2).to_broadcast([128, 16, 8]),
                                                             in1=zmask.rearrange("p (h q) -> p h q", q=8), op=ALU.mult), reads=[b_WI, b_zm], writes=[b_Z])
                ps, pb = bankof([2, 3], tbank)
                if "wcol" not in SKIP:
                    P.op("pe", lambda e, ps=ps: e.matmul(ps[:, 0:16], lhsT=Z, rhs=bsel, start=True, stop=True), reads=[b_Z, b_bsel], writes=[pb])
                    P.op("act", lambda e, ps=ps: e.activation(out=wcol, in_=ps[:, 0:16], func=AF.Copy), reads=[pb], writes=[b_wcol])
                items = [(kc, jj) for kc in range(tt + 1) for jj in range(16)]
                groups = [items[k:k + 3] for k in range(0, len(items), 3)]
                pss_of = {}
                gpend = []
                for gidx in range(len(groups) + 1):
                    if gidx < len(groups):
                        grp = groups[gidx]
                        bset = [2, 3, 4] if (gcount[0] % 2 == 0) else [5, 6, 7]
                        gcount[0] += 1
                        recs = []
                        for ii, (kc, jj) in enumerate(grp):
                            ncol = 512 if kc < tt else (qb + 1) * 128
                            rt, brt = Rt[ri_[0] % NR], b_Rt[ri_[0] % NR]
                            ri_[0] += 1
                            recs.append((kc, jj, ncol, kb.ps[bset[ii]], kb.psb[bset[ii]], rt, brt))

                        def fd(e, recs=recs, qb=qb):
                            for (kc, jj, ncol, psd, pbd, rt, brt) in recs:
                                ins = e.matmul(psd[:, 0:ncol], lhsT=QI2[:, qb * 16 + jj, :], rhs=KI_sb[:, kc * 512:kc * 512 + ncol], start=True, stop=True)
                            return ins
                        P.op("pe", fd, reads=[b_QI2, b_KI], writes=[r_[4] for r_ in recs])
                        for (kc, jj, ncol, psd, pbd, rt, brt) in recs:
                            P.op("dve", lambda e, psd=psd, rt=rt, jj=jj, ncol=ncol: e.tensor_scalar(out=rt[:, 0:ncol], in0=psd[:, 0:ncol], scalar1=0.0, scalar2=wcol[:, jj:jj + 1], op0=ALU.max, op1=ALU.mult),
                                 reads=[pbd, b_wcol], writes=[brt])
                        gpend.append(recs)
                    if gidx >= 1:
                        recs = gpend[gidx - 1]
                        for (kc, jj, ncol, psd, pbd, rt, brt) in recs:
                            if jj == 0:
                                pss_of[kc] = bankof([0, 1], sacc)
                        wr = []
                        for (kc, jj, ncol, psd, pbd, rt, brt) in recs:
                            if pss_of[kc][1] not in wr:
                                wr.append(pss_of[kc][1])

                        def fs(e, recs=recs, pss_of=dict(pss_of)):
                            for (kc, jj, ncol, psd, pbd, rt, brt) in recs:
                                ins = e.matmul(pss_of[kc][0][:, 0:ncol], lhsT=sel_sb[:, jj, :], rhs=rt[:, 0:ncol], start=(jj == 0), stop=(jj == 15))
                            return ins
                        all_first = all(r_[1] == 0 for r_ in recs)
                        P.op("pe", fs, reads=[r_[6] for r_ in reversed(recs)] + [b_sel], writes=wr, partial=True)
                        for (kc, jj, ncol, psd, pbd, rt, brt) in recs:
                            if jj != 15:
                                continue
                            pss, pbs = pss_of[kc]
                            if kc < tt:
                                P.op("act", lambda e, pss=pss, kc=kc: e.activation(out=S[:, kc * 512:(kc + 1) * 512], in_=pss, func=AF.Copy), reads=[pbs], writes=[b_S], partial=True)
                            else:
                                if qb > 0:
                                    P.op("dve", lambda e, pss=pss, kc=kc, qb=qb: e.tensor_copy(out=S[:, kc * 512:kc * 512 + qb * 128], in_=pss[:, 0:qb * 128]), reads=[pbs], writes=[b_S], partial=True)
                                P.op("dve", lambda e, pss=pss, kc=kc, qb=qb: e.tensor_tensor(out=S[:, kc * 512 + qb * 128:kc * 512 + (qb + 1) * 128], in0=pss[:, qb * 128:(qb + 1) * 128], in1=triq, op=ALU.add),
                                     reads=[pbs, b_triq], writes=[b_S], partial=True)
                if nb >= 2 and "thr" not in SKIP:
                    P.op("dve", lambda e, nb=nb: e.tensor_reduce(out=lo, in_=S[:, 0:nb * 128], axis=AX.X, op=ALU.min), reads=[b_S], writes=[b_lo])
                    P.op("dve", lambda e, nk=nk: e.tensor_reduce(out=hi, in_=S[:, 0:nk], axis=AX.X, op=ALU.max), reads=[b_S], writes=[b_hi])
                    P.op("dve", lambda e: e.scalar_tensor_tensor(out=mid, in0=lo, scalar=hi[:, 0:1], in1=halfc, op0=ALU.add, op1=ALU.mult), reads=[b_lo, b_hi, b_half], writes=[b_mid])
                    P.op("dve", lambda e: e.tensor_tensor(out=dd, in0=hi, in1=lo, op=ALU.subtract), reads=[b_lo, b_hi], writes=[b_dd])
                    P.op("dve", lambda e: e.tensor_scalar(out=Wst, in0=pw2, scalar1=dd[:, 0:1], scalar2=None, op0=ALU.mult), reads=[b_dd, b_pw2], writes=[b_Wst])
                    for it in range(NIT):
                        P.op("dve", lambda e, nk=nk: e.tensor_scalar(out=junk[:, 0:nk], in0=S[:, 0:nk], scalar1=mid[:, 0:1], scalar2=None, op0=ALU.is_ge, op1=ALU.add, accum_out=cnt[:, 0:1]),
                             reads=[b_S, b_mid], writes=[b_junk, b_cnt])
                        P.op("dve", lambda e: e.tensor_scalar(out=ge, in0=cnt, scalar1=255.5, scalar2=0.5, op0=ALU.is_ge, op1=ALU.subtract), reads=[b_cnt], writes=[b_ge])
                        P.op("dve", lambda e, it=it: e.scalar_tensor_tensor(out=mid, in0=ge, scalar=Wst[:, it:it + 1], in1=mid, op0=ALU.mult, op1=ALU.add), reads=[b_ge, b_Wst, b_mid], writes=[b_mid])
                    P.op("dve", lambda e: e.scalar_tensor_tensor(out=lo, in0=Wst[:, NIT - 1:NIT], scalar=-0.5, in1=mid, op0=ALU.mult, op1=ALU.add), reads=[b_Wst, b_mid], writes=[b_lo])
                else:
                    P.op("dve", lambda e: e.memset(lo, -1.0e4), writes=[b_lo])
                P.op("dve", lambda e, nk=nk: e.tensor_scalar(out=mqb[:, 0:nk], in0=S[:, 0:nk], scalar1=lo[:, 0:1], scalar2=1.0, op0=ALU.is_ge, op1=ALU.subtract), reads=[b_S, b_lo], writes=[b_mqb])
                for kb0 in range(0, nb + 1, 4):
                    if "tr" in SKIP:
                        continue
                    gq = min(4, nb + 1 - kb0)
                    ps, pb = bankof([2, 3], tbank)
                    psb_ = ps.bitcast(BF16)

                    def f(e, psb_=psb_, kb0=kb0, gq=gq):
                        for t_i in range(gq):
                            ins = e.transpose(psb_[:, t_i * 128:(t_i + 1) * 128], mqb[:, (kb0 + t_i) * 128:(kb0 + t_i + 1) * 128], ident)
                        return ins
                    P.op("pe", f, reads=[b_mqb, b_ident], writes=[pb])
                    P.op("act", lambda e, psb_=psb_, kb0=kb0, gq=gq, qb=qb: e.activation(out=MT[:, kb0:kb0 + gq, qb * 128:(qb + 1) * 128], in_=psb_[:, 0:gq * 128].rearrange("p (g c) -> p g c", c=128), func=AF.Copy),
                         reads=[pb], writes=[b_MT], partial=True)
            nkb = 4 * (tt + 1)
            sm = tt % 2
            for h in range(8):
                if "att" in SKIP:
                    continue
                psO = [(kb.ps[0], kb.psb[0]), (kb.ps[1], kb.psb[1])]
                psL, pbL = kb.ps[2], kb.psb[2]
                pendp = []
                ADEPTH = 2
                for kidx in range(nkb + ADEPTH):
                    if kidx < nkb:
                        kbk = kidx
                        pss, pbs = bankof([4, 5, 6, 7], dbank)
                        ks = slice(kbk * 128, (kbk + 1) * 128)

                        def f(e, pss=pss, kbk=kbk, ks=ks, h=h):
                            e.matmul(pss, lhsT=id30k, rhs=MT[:, kbk, :], start=True, stop=False)
                            e.matmul(pss, lhsT=CKT_sb[:, 0, ks], rhs=QL_sb[:, h, 0, :], start=False, stop=False)
                            e.matmul(pss, lhsT=CKT_sb[:, 1, ks], rhs=QL_sb[:, h, 1, :], start=False, stop=False)
                            return e.matmul(pss, lhsT=KR_sb[:, ks], rhs=QR_sb[:, h, :], start=False, stop=True)
                        P.op("pe", f, reads=[b_id30k, b_MT, b_CKT, b_QL, b_KR, b_QR], writes=[pbs])
                        pt, bpt = PT[pi_[0] % NPT], b_PT[pi_[0] % NPT]
                        pi_[0] += 1
                        P.op("act", lambda e, pss=pss, pt=pt: e.activation(out=pt, in_=pss, func=AF.Exp, scale=SC), reads=[pbs], writes=[bpt])
                        pendp.append((kbk, pt, bpt))
                    if kidx >= ADEPTH:
                        kbk, pt, bpt = pendp[kidx - ADEPTH]

                        def f2(e, pt=pt, kbk=kbk, first=(kbk == 0), last=(kbk == nkb - 1)):
                            e.matmul(psO[0][0], lhsT=CK_sb[:, kbk, 0:128], rhs=pt, start=first, stop=last)
                            e.matmul(psO[1][0], lhsT=CK_sb[:, kbk, 128:256], rhs=pt, start=first, stop=last)
                            return e.matmul(psL, lhsT=ones_b, rhs=pt, start=first, stop=last)
                        P.op("pe", f2, reads=[bpt, b_CK, b_ones_b], writes=[psO[0][1], psO[1][1], pbL], partial=(kbk > 0))
                recip(P, rL, psL, [pbL], [b_rL])
                for rc in range(2):
                    P.op("dve", lambda e, rc=rc: e.tensor_tensor(out=olat[:, rc, :], in0=psO[rc][0], in1=rL, op=ALU.mult), reads=[psO[rc][1], b_rL], writes=[b_olat], partial=(rc > 0))
                pso, pbo = kb.ps[3], kb.psb[3]

                def f3(e, h=h, pso=pso):
                    e.matmul(pso, lhsT=wuv_sb[:, h, 0, :], rhs=olat[:, 0, :], start=True, stop=False)
                    return e.matmul(pso, lhsT=wuv_sb[:, h, 1, :], rhs=olat[:, 1, :], start=False, stop=True)
                P.op("pe", f3, reads=[b_wuv, b_olat], writes=[pbo])
                P.op("act", lambda e, pso=pso, h=h, sm=sm: e.activation(out=mixst[sm][:, h, :], in_=pso, func=AF.Copy), reads=[pbo], writes=[b_mixst[sm]], partial=True)
            P.dma("sp", lambda e, sm=sm, tt=tt: e.dma_start(out=MIXd.rearrange("(c p) t -> p c t", p=128)[:, :, tsl(tt)], in_=mixst[sm]), reads=[b_mixst[sm]])
        kb.phase_end(mark)

    def run_layers():
        xcur = xin
        for i in layers:
            kind = i % 3
            if kind == 1:
                p1_b(i, xcur)
                p2_b(i)
            elif kind == 2:
                p1_c(i, xcur)
                p2_c(i)
            else:
                p1_a(i, xcur)
                p2_a(i)
            phase_out(i, xcur, XS[0])
            phase_ffn(i, XS[0], XS[1])
            xcur = XS[1]
        if final:
            phase_final(xcur)
        else:
            mark = kb.off
            t_ = [tile([8, TT]), tile([8, TT])]; b_t = [Buf(), Buf()]
            for tt in range(NTT):
                s = tt % 2
                P.dma("sp", lambda e, s=s, tt=tt: e.dma_start(out=t_[s], in_=xcur.rearrange("(kc p) t -> p kc t", p=128)[:, :, tsl(tt)]), writes=[b_t[s]])
                P.dma("sp", lambda e, s=s, tt=tt: e.dma_start(out=out_d.rearrange("(kc p) t -> p kc t", p=128)[:, :, tsl(tt)], in_=t_[s]), reads=[b_t[s]])
            kb.phase_end(mark)
        if taps:
            mark = kb.off
            tp_ = [tile([8, TT], BF16), tile([8, TT], BF16)]; b_tp = [Buf(), Buf()]
            tapo = nc.dram_tensor("tapmix", [D, T], BF16, kind="ExternalOutput").ap()
            for tt in range(NTT):
                s = tt % 2
                P.dma("sp", lambda e, s=s, tt=tt: e.dma_start(out=tp_[s], in_=MIXd.rearrange("(kc p) t -> p kc t", p=128)[:, :, tsl(tt)]), writes=[b_tp[s]])
                P.dma("sp", lambda e, s=s, tt=tt: e.dma_start(out=tapo.rearrange("(kc p) t -> p kc t", p=128)[:, :, tsl(tt)], in_=tp_[s]), reads=[b_tp[s]])
            kb.phase_end(mark)
        P.barrier(engines=("sp",))

    kb_ctx = dict(locals())
    return kb, kb_ctx


def _host_inputs(inp, b, layers):
    f = np.float32
    m = {}
    m["xT"] = np.ascontiguousarray(inp["x"][b].T.astype(f))
    m["memT"] = np.ascontiguousarray(inp["mem"][b].T.astype(f))
    m["pos"] = np.ascontiguousarray(inp["positions"][b][None, :].astype(np.int32))
    gl = [inp["g_mix"][i] for i in range(4)] + [inp["g_ffn"][i] for i in range(4)] + [inp["g_mem"], inp["g_final"]]
    m["gvec"] = np.ascontiguousarray(np.stack([g.reshape(8, 128).T for g in gl], axis=1).astype(f))
    for k in ("invc", "cmask", "sel", "zmask", "bsel", "triq"):
        m[k] = CONSTS[k]
    for i in layers:
        kind, j = i % 3, i // 3
        w = (inp["a_w_in"], inp["b_w_in"], inp["c_w_in"])[kind][j]
        m["w_in%d" % i] = np.ascontiguousarray(w[:, PERMS[kind][0]].astype(f))
        m["w_out%d" % i] = np.ascontiguousarray((inp["a_w_out"], inp["b_w_out"], inp["c_w_out"])[kind][j].astype(f))
    m["w_up"] = np.ascontiguousarray(inp["f_w_up"].astype(f))
    m["w_down"] = np.ascontiguousarray(inp["f_w_down"].astype(f))
    m["convw"] = np.ascontiguousarray(inp["f_conv_w"].reshape(4, 3, 22, 128).transpose(0, 3, 2, 1).astype(f))
    m["convb"] = np.ascontiguousarray(inp["f_conv_b"].reshape(4, 22, 128).transpose(0, 2, 1).astype(f))
    m["wmem"] = np.ascontiguousarray(inp["w_mem_kv"].astype(f))
    sk = inp["b_sinks"][0]
    m["sinks"] = np.ascontiguousarray(np.stack([np.repeat(sk[2 * c:2 * c + 2], 64) for c in range(8)], axis=1).astype(f))
    m["kvn"] = np.ascontiguousarray(inp["a_kv_norm"][:, None, :].astype(f))
    m["wukT"] = np.ascontiguousarray(inp["a_w_uk"].transpose(0, 2, 3, 1).astype(f))
    m["wuv"] = np.ascontiguousarray(inp["a_w_uv"].transpose(0, 2, 1, 3).astype(f))
    return m


_CACHE = {}


def kernel(**inputs):
    inp = {k: np.asarray(v) for k, v in inputs.items()}
    layers = (0, 1, 2, 3)
    if "kb" not in _CACHE:
        kb, ctx = build_program(layers=layers, final=True)
        ctx["run_layers"]()
        kb.P.emit()
        _CACHE["kb"] = kb
    kb = _CACHE["kb"]
    n = 8
    maps = []
    for b in range(n):
        m = _host_inputs(inp, b, layers)
        maps.append({k: m[k] for k in kb.inputs})
    res = run_bass_kernel_spmd(kb.nc, maps, core_ids=list(range(n)))
    out = np.stack([np.ascontiguousarray(res.results[b]["outT"].T) for b in range(n)], axis=0)
    return out.astype(np.float32)
```
